# Optimizing a Trainium2 kernel written in Bass

```python
import math
import jax
import jax.numpy as jnp
from jax import lax
import numpy as np

D_MODEL = 1024
BATCH = 8
SEQ = 2048
DEPTH = 4

GRID_W = 64
CTX_LEN = 256
CHUNK = 64
NORM_EPS = 1e-6
F_TINY = 1e-30

DN_HEADS = 4
DN_DK = 128
DN_DV = 128
DN_QK = DN_HEADS * DN_DK
DN_V = DN_HEADS * DN_DV
CONV_K = 3

HG_HEADS = 4
HG_DK = 128
HG_DV = 128
HG_K = HG_HEADS * HG_DK
HG_V = HG_HEADS * HG_DV

N_GROUPS = 4
EXPERTS_PER_GROUP = 8
N_EXPERTS = N_GROUPS * EXPERTS_PER_GROUP
TOP_K_IN_GROUP = 2
D_EXPERT = D_MODEL // 2
MOE_BLOCK = 128

IN_SIZES = (2 * DN_QK + DN_V, DN_V, 2 * DN_HEADS, 2 * DN_HEADS, HG_K, 2 * HG_K, HG_V, HG_V, 2 * D_MODEL)
IN_SPLITS = tuple(sum(IN_SIZES[:i + 1]) for i in range(len(IN_SIZES) - 1))
N_IN = sum(IN_SIZES)

kernel_name = 'hybrid_deltanet_hgrn2_hmoe_dit'

F32 = jnp.float32


def _rmsnorm(x, g):
    xf = x.astype(F32)
    y = xf * lax.rsqrt(jnp.mean(xf * xf, axis=-1, keepdims=True) + NORM_EPS)
    return (y * g.astype(F32)).astype(x.dtype)


def _l2norm(x):
    xf = x.astype(F32)
    return xf * lax.rsqrt(jnp.sum(xf * xf, axis=-1, keepdims=True) + NORM_EPS)


def _masked_exp(diff, mask):
    return jnp.where(mask, jnp.exp(jnp.where(mask, diff, 0.0)), 0.0)


def _gated_rmsnorm(o, z, g):
    bsz, n, h, dv = o.shape
    y = o * lax.rsqrt(jnp.mean(o * o, axis=-1, keepdims=True) + NORM_EPS) * g.astype(F32)
    y = y * jax.nn.silu(z.astype(F32).reshape(bsz, n, h, dv))
    return y.reshape(bsz, n, h * dv).astype(z.dtype)


def _short_conv(x, w):
    pad = CONV_K // 2
    return lax.conv_general_dilated(x, w[:, None, :].astype(x.dtype), window_strides=(1,),
                                    padding=[(pad, pad)], dimension_numbers=('NWC', 'WIO', 'NWC'),
                                    feature_group_count=x.shape[-1])


def _to_col_major(t):
    bsz, n, f = t.shape
    rows = n // GRID_W
    return t.reshape(bsz, rows, GRID_W, f).transpose(0, 2, 1, 3).reshape(bsz, n, f)


def _from_col_major(t):
    bsz, n, f = t.shape
    rows = n // GRID_W
    return t.reshape(bsz, GRID_W, rows, f).transpose(0, 2, 1, 3).reshape(bsz, n, f)


def _rev(t, direction):
    return jnp.flip(t, axis=1) if direction == 1 else t


def _to_chunks(t):
    bsz, n, h = t.shape[:3]
    t = t.reshape(bsz, n // CHUNK, CHUNK, h, *t.shape[3:])
    return jnp.moveaxis(t, (1, 3), (0, 2))


def _from_chunks(t):
    t = jnp.moveaxis(t, (0, 2), (1, 3))
    return t.reshape(t.shape[0], t.shape[1] * t.shape[2], *t.shape[3:])


def gated_delta_chunked(q, k, v, g, beta, s0, return_out):
    q, k, v, g, beta = (_to_chunks(t.astype(F32)) for t in (q, k, v, g, beta))
    dv = v.shape[-1]
    incl = jnp.tril(jnp.ones((CHUNK, CHUNK), bool))
    strict = jnp.tril(jnp.ones((CHUNK, CHUNK), bool), -1)
    gcum = jnp.cumsum(g, axis=-1)
    decay = _masked_exp(gcum[..., :, None] - gcum[..., None, :], incl)
    kb = k * beta[..., None]
    a = jnp.where(strict, jnp.einsum('nbhik,nbhjk->nbhij', kb, k) * decay, 0.0)
    rhs = jnp.concatenate([v * beta[..., None], kb * jnp.exp(gcum)[..., None]], axis=-1)
    sol = lax.linalg.triangular_solve(a, rhs, left_side=True, lower=True, unit_diagonal=True)
    u, w = sol[..., :dv], sol[..., dv:]

    def step(s, inp):
        qc, kc, uc, wc, gc, dc = inp
        v_new = uc - jnp.einsum('bhck,bhkv->bhcv', wc, s)
        g_last = gc[..., -1]
        s_new = s * jnp.exp(g_last)[..., None, None] + jnp.einsum(
            'bhck,bhcv->bhkv', kc * jnp.exp(g_last[..., None] - gc)[..., None], v_new)
        if not return_out:
            return s_new, None
        o = jnp.einsum('bhck,bhkv->bhcv', qc * jnp.exp(gc)[..., None], s) + jnp.einsum(
            'bhij,bhjv->bhiv', jnp.einsum('bhik,bhjk->bhij', qc, kc) * dc, v_new)
        return s_new, o

    s_fin, o = lax.scan(step, s0.astype(F32), (q, k, u, w, gcum, decay))
    return (_from_chunks(o) if return_out else None), s_fin


def gla_chunked(q, k, v, log_f, s0, return_out):
    q, k, v, log_f = (_to_chunks(t.astype(F32)) for t in (q, k, v, log_f))
    gcum = jnp.cumsum(log_f, axis=3)
    incl = jnp.tril(jnp.ones((CHUNK, CHUNK), bool))[..., None]

    def step(s, inp):
        qc, kc, vc, gc = inp
        g_last = gc[:, :, -1]
        s_new = s * jnp.exp(g_last)[..., None] + jnp.einsum(
            'bhck,bhcv->bhkv', kc * jnp.exp(g_last[:, :, None] - gc), vc)
        if not return_out:
            return s_new, None
        rel = _masked_exp(gc[:, :, :, None] - gc[:, :, None], incl)
        att = jnp.einsum('bhik,bhjk,bhijk->bhij', qc, kc, rel)
        o = jnp.einsum('bhck,bhkv->bhcv', qc * jnp.exp(gc), s) + jnp.einsum('bhij,bhjv->bhiv', att, vc)
        return s_new, o

    s_fin, o = lax.scan(step, s0.astype(F32), (q, k, v, gcum))
    return (_from_chunks(o) if return_out else None), s_fin


def _dn_inputs(qkv, b, a, conv_w, a_log, dt_bias):
    bsz, n = qkv.shape[:2]
    qkv = jax.nn.silu(_short_conv(qkv, conv_w))
    q, k, v = jnp.split(qkv, [DN_QK, 2 * DN_QK], axis=-1)
    q = _l2norm(q.reshape(bsz, n, DN_HEADS, DN_DK)) * (DN_DK ** -0.5)
    k = _l2norm(k.reshape(bsz, n, DN_HEADS, DN_DK))
    v = v.reshape(bsz, n, DN_HEADS, DN_DV)
    beta = jax.nn.sigmoid(b.astype(F32).reshape(bsz, n, 2, DN_HEADS))
    g = -jnp.exp(a_log.astype(F32)) * jax.nn.softplus(
        a.astype(F32).reshape(bsz, n, 2, DN_HEADS) + dt_bias.astype(F32))
    return q, k, v, beta, g


def _deltanet_scans(lat, ctx, ctx_out):
    xq, xk, xv, xb, xg = lat
    cq, ck, cv, cb, cg = ctx
    o_x, o_c = 0.0, 0.0
    for d in range(2):
        s0 = jnp.zeros((xq.shape[0], DN_HEADS, DN_DK, DN_DV), F32)
        oc, s_ctx = gated_delta_chunked(*(_rev(t, d) for t in (cq, ck, cv, cg[:, :, d], cb[:, :, d])), s0, ctx_out)
        ox, _ = gated_delta_chunked(*(_rev(t, d) for t in (xq, xk, xv, xg[:, :, d], xb[:, :, d])), s_ctx, True)
        o_x = o_x + _rev(ox, d)
        if ctx_out:
            o_c = o_c + _rev(oc, d)
    return o_x, (o_c if ctx_out else None)


def _hg_inputs(q, f2, i, lb):
    bsz, n = q.shape[:2]
    q = jax.nn.silu(q.astype(F32)).reshape(bsz, n, HG_HEADS, HG_DK)
    z = f2.astype(F32).reshape(bsz, n, 2, HG_HEADS, HG_DK)
    f = lb + (1.0 - lb) * jax.nn.sigmoid(z)
    log_f = jnp.log(jnp.maximum(f, F_TINY))
    k = (1.0 - lb) * jax.nn.sigmoid(-z)
    v = i.astype(F32).reshape(bsz, n, HG_HEADS, HG_DV)
    return q, k, log_f, v


def _hgrn2_scans(lat, ctx, ctx_out):
    xq, xk, xlf, xv = lat
    cq, ck, clf, cv = ctx
    o_x, o_c = 0.0, 0.0
    for d in range(2):
        s0 = jnp.zeros((xq.shape[0], HG_HEADS, HG_DK, HG_DV), F32)
        oc, s_ctx = gla_chunked(*(_rev(t, d) for t in (cq, ck[:, :, d], cv, clf[:, :, d])), s0, ctx_out)
        ox, _ = gla_chunked(*(_rev(t, d) for t in (xq, xk[:, :, d], xv, xlf[:, :, d])), s_ctx, True)
        o_x = o_x + _rev(ox, d)
        if ctx_out:
            o_c = o_c + _rev(oc, d)
    return o_x, (o_c if ctx_out else None)


def hybrid_mixer(hx, hc, w_in, dn_conv, dn_a_log, dn_dt_bias, dn_norm_g, lb, hg_norm_g,
                 w_br_dn, w_br_hg, w_out, ctx_out):
    bsz, n_lat = hx.shape[:2]
    n_ctx = hc.shape[1]
    proj = jnp.concatenate([hc, hx], axis=1) @ w_in
    pc = jnp.split(proj[:, :n_ctx], IN_SPLITS, axis=-1)
    px = jnp.split(proj[:, n_ctx:], IN_SPLITS, axis=-1)
    odn_x, odn_c = _deltanet_scans(_dn_inputs(px[0], px[2], px[3], dn_conv, dn_a_log, dn_dt_bias),
                                   _dn_inputs(pc[0], pc[2], pc[3], dn_conv, dn_a_log, dn_dt_bias), ctx_out)
    ohg_x, ohg_c = _hgrn2_scans(_hg_inputs(*(_to_col_major(t) for t in (px[4], px[5], px[6])), lb),
                                _hg_inputs(pc[4], pc[5], pc[6], lb), ctx_out)
    ohg_x = _from_col_major(ohg_x.reshape(bsz, n_lat, HG_V)).reshape(bsz, n_lat, HG_HEADS, HG_DV)

    def merge(parts, odn, ohg):
        br_dn = _gated_rmsnorm(odn, parts[1], dn_norm_g) @ w_br_dn
        br_hg = _gated_rmsnorm(ohg, parts[7], hg_norm_g) @ w_br_hg
        gate_dn, gate_hg = jnp.split(parts[8], 2, axis=-1)
        return (jax.nn.sigmoid(gate_dn) * br_dn + jax.nn.sigmoid(gate_hg) * br_hg) @ w_out

    y_x = merge(px, odn_x, ohg_x)
    y_c = merge(pc, odn_c, ohg_c) if ctx_out else None
    return y_x, y_c


def hier_moe(h, w_rg, b_rg, w_re, b_re, w_gate, w_up, w_down):
    n_tok, d = h.shape
    grp_logits = (h @ w_rg + b_rg).astype(F32)
    grp_sel = jnp.argmax(grp_logits, axis=-1)
    grp_p = jnp.take_along_axis(jax.nn.softmax(grp_logits, axis=-1), grp_sel[:, None], axis=-1)
    exp_logits = (h @ w_re + b_re).astype(F32).reshape(n_tok, N_GROUPS, EXPERTS_PER_GROUP)
    in_logits = jnp.take_along_axis(exp_logits, grp_sel[:, None, None], axis=1)[:, 0]
    top_p, top_j = lax.top_k(jax.nn.softmax(in_logits, axis=-1), TOP_K_IN_GROUP)
    gate = grp_p * top_p / jnp.sum(top_p, axis=-1, keepdims=True)
    expert = grp_sel[:, None] * EXPERTS_PER_GROUP + top_j
    n_asg = n_tok * TOP_K_IN_GROUP
    e_flat = expert.reshape(n_asg)
    order = jnp.argsort(e_flat)
    e_sorted = e_flat[order]
    tok_sorted = order // TOP_K_IN_GROUP
    gate_sorted = gate.reshape(n_asg)[order]
    counts = jnp.bincount(e_flat, length=N_EXPERTS)
    padded = (counts + MOE_BLOCK - 1) // MOE_BLOCK * MOE_BLOCK
    pad_end = jnp.cumsum(padded)
    pad_start = pad_end - padded
    start = jnp.cumsum(counts) - counts
    dest = pad_start[e_sorted] + jnp.arange(n_asg) - start[e_sorted]
    n_blocks = (n_asg + N_EXPERTS * (MOE_BLOCK - 1) + MOE_BLOCK - 1) // MOE_BLOCK
    rows = jnp.zeros((n_blocks * MOE_BLOCK, d), h.dtype).at[dest].set(h[tok_sorted])
    block_expert = jnp.minimum(jnp.searchsorted(pad_end, jnp.arange(n_blocks) * MOE_BLOCK, side='right'),
                               N_EXPERTS - 1)

    def expert_block(args):
        xb, e = args
        return (jax.nn.silu(xb @ w_gate[e]) * (xb @ w_up[e])) @ w_down[e]

    out = lax.map(expert_block, (rows.reshape(n_blocks, MOE_BLOCK, d), block_expert)).reshape(-1, d)
    contrib = (out[dest] * gate_sorted[:, None]).astype(h.dtype)
    return jnp.zeros((n_tok, d), h.dtype).at[tok_sorted].add(contrib)


def setup_inputs(seed: int = 0) -> dict:
    key = jax.random.key(seed)
    ks = iter(jax.random.split(key, 26))
    D, L = D_MODEL, DEPTH

    def nrm(shape, scale):
        return jax.random.normal(next(ks), shape, F32) * scale

    x = nrm((BATCH, SEQ, D), 1.0)
    c = nrm((BATCH, D), 1.0)
    ctx = nrm((BATCH, CTX_LEN, D), 1.0)
    c_ctx = nrm((D,), 1.0)
    w_ada = nrm((L, D, 6 * D), 0.5 * D ** -0.5)
    b_ada = nrm((L, 6 * D), 0.02)
    g_mix = 1.0 + nrm((L, D), 0.02)
    g_ffn = 1.0 + nrm((L, D), 0.02)
    g_final = 1.0 + nrm((D,), 0.02)
    w_in = nrm((L, D, N_IN), D ** -0.5)
    dn_conv = nrm((L, CONV_K, 2 * DN_QK + DN_V), CONV_K ** -0.5)
    dn_a_log = jnp.log(jax.random.uniform(next(ks), (L, 2, DN_HEADS), F32, 1.0, 16.0))
    dt = jnp.exp(jax.random.uniform(next(ks), (L, 2, DN_HEADS), F32, math.log(1e-3), math.log(1e-1)))
    dn_dt_bias = dt + jnp.log(-jnp.expm1(-dt))
    dn_norm_g = 1.0 + nrm((L, DN_DV), 0.02)
    hg_lb_logits = nrm((L, HG_K), 0.5)
    hg_norm_g = 1.0 + nrm((L, HG_DV), 0.02)
    w_br_dn = nrm((L, DN_V, D), DN_V ** -0.5)
    w_br_hg = nrm((L, HG_V, D), HG_V ** -0.5)
    w_out = nrm((L, D, D), D ** -0.5)
    w_router_grp = nrm((L, D, N_GROUPS), D ** -0.5)
    b_router_grp = nrm((L, N_GROUPS), 0.01)
    w_router_exp = nrm((L, D, N_EXPERTS), D ** -0.5)
    b_router_exp = nrm((L, N_EXPERTS), 0.01)
    w_exp_gate = nrm((L, N_EXPERTS, D, D_EXPERT), D ** -0.5)
    w_exp_up = nrm((L, N_EXPERTS, D, D_EXPERT), D ** -0.5)
    w_exp_down = nrm((L, N_EXPERTS, D_EXPERT, D), D_EXPERT ** -0.5)
    return {'x': x, 'c': c, 'ctx': ctx, 'c_ctx': c_ctx, 'w_ada': w_ada, 'b_ada': b_ada,
            'g_mix': g_mix, 'g_ffn': g_ffn, 'g_final': g_final, 'w_in': w_in, 'dn_conv': dn_conv,
            'dn_a_log': dn_a_log, 'dn_dt_bias': dn_dt_bias, 'dn_norm_g': dn_norm_g,
            'hg_lb_logits': hg_lb_logits, 'hg_norm_g': hg_norm_g, 'w_br_dn': w_br_dn,
            'w_br_hg': w_br_hg, 'w_out': w_out, 'w_router_grp': w_router_grp,
            'b_router_grp': b_router_grp, 'w_router_exp': w_router_exp, 'b_router_exp': b_router_exp,
            'w_exp_gate': w_exp_gate, 'w_exp_up': w_exp_up, 'w_exp_down': w_exp_down}


def reference(x, c, ctx, c_ctx, w_ada, b_ada, g_mix, g_ffn, g_final, w_in, dn_conv, dn_a_log,
              dn_dt_bias, dn_norm_g, hg_lb_logits, hg_norm_g, w_br_dn, w_br_hg, w_out,
              w_router_grp, b_router_grp, w_router_exp, b_router_exp, w_exp_gate, w_exp_up,
              w_exp_down):
    bsz, n_lat, d = x.shape
    n_ctx = ctx.shape[1]
    lb_w = jax.nn.softmax(hg_lb_logits.astype(F32), axis=0)
    lower_bounds = jnp.cumsum(lb_w, axis=0) - lb_w[0]
    s_c = jax.nn.silu(c)
    s_cc = jax.nn.silu(c_ctx)
    h_ctx = ctx
    for layer in range(DEPTH):
        last = layer == DEPTH - 1
        mod_x = jnp.split((s_c @ w_ada[layer] + b_ada[layer])[:, None, :], 6, axis=-1)
        mod_c = jnp.split(s_cc @ w_ada[layer] + b_ada[layer], 6, axis=-1)
        hx = _rmsnorm(x, g_mix[layer]) * (1.0 + mod_x[1]) + mod_x[0]
        hc = _rmsnorm(h_ctx, g_mix[layer]) * (1.0 + mod_c[1]) + mod_c[0]
        y_x, y_c = hybrid_mixer(hx, hc, w_in[layer], dn_conv[layer], dn_a_log[layer], dn_dt_bias[layer],
                                dn_norm_g[layer], lower_bounds[layer].reshape(HG_HEADS, HG_DK),
                                hg_norm_g[layer], w_br_dn[layer], w_br_hg[layer], w_out[layer],
                                not last)
        x = x + mod_x[2] * y_x
        hx = (_rmsnorm(x, g_ffn[layer]) * (1.0 + mod_x[4]) + mod_x[3]).reshape(-1, d)
        moe_args = (w_router_grp[layer], b_router_grp[layer], w_router_exp[layer], b_router_exp[layer],
                    w_exp_gate[layer], w_exp_up[layer], w_exp_down[layer])
        if last:
            f_x = hier_moe(hx, *moe_args)
        else:
            h_ctx = h_ctx + mod_c[2] * y_c
            hc = (_rmsnorm(h_ctx, g_ffn[layer]) * (1.0 + mod_c[4]) + mod_c[3]).reshape(-1, d)
            f_all = hier_moe(jnp.concatenate([hc, hx], axis=0), *moe_args)
            h_ctx = h_ctx + mod_c[5] * f_all[:bsz * n_ctx].reshape(bsz, n_ctx, d)
            f_x = f_all[bsz * n_ctx:]
        x = x + mod_x[5] * f_x.reshape(bsz, n_lat, d)
    return _rmsnorm(x, g_final)
```

```python
import numpy as np
from contextlib import ExitStack
import concourse.bass as bass
import concourse.mybir as mybir
from concourse.bass_utils import run_bass_kernel_spmd

F32 = mybir.dt.float32
BF16 = mybir.dt.bfloat16
AF = mybir.ActivationFunctionType
ALU = mybir.AluOpType
AX = mybir.AxisListType

D = 1024
KC = 8
NCTX = 256
NLAT = 2048
NTOK = NCTX + NLAT
NT = NTOK // 128
DEPTH = 4
N_IN = 6672
OFF_DNQ, OFF_DNK, OFF_DNV, OFF_DNZ, OFF_DNB, OFF_DNA = 0, 512, 1024, 1536, 2048, 2056
OFF_HGQ, OFF_HGF, OFF_HGI, OFF_HGG, OFF_GDN, OFF_GHG = 2064, 2576, 3600, 4112, 4624, 5648
NE = 32
DEXP = 512
EPS = 1e-6


class Op:
    __slots__ = ("eng", "fn", "deps", "idx", "signal", "dma", "dsem", "dval", "cnt")

    def __init__(self, eng, fn, dma):
        self.eng, self.fn, self.dma = eng, fn, dma
        self.deps = []
        self.signal = False
        self.dsem = self.dval = None
        self.cnt = None


class _Rec:
    def __init__(self):
        self.call = None

    def __getattr__(self, name):
        def f(*a, **kw):
            assert self.call is None
            self.call = (name, a, kw)
            return self
        return f


class Plan:
    ENGS = ("pe", "act", "dve", "pool", "sp")
    NDSEM = 24
    SEM_WRAP = 30000

    def __init__(self, nc):
        self.nc = nc
        self.ops = {e: [] for e in self.ENGS}
        self.writer = {}
        self.readers = {}
        self.dcum = [0] * self.NDSEM
        self.drr = 0
        self.pending = {e: [] for e in self.ENGS}

    def op(self, eng, fn, reads=(), writes=(), dma=False):
        if fn is not None:
            rec = _Rec()
            fn(rec)
            call = rec.call
            fn = lambda e, call=call: getattr(e, call[0])(*call[1], **call[2])
        o = Op(eng, fn, dma)
        psk = [k for k in reads if isinstance(k, tuple) and k[0] == "ps"]
        if psk:
            reads = [k for k in reads if k not in psk]
            writes = list(writes) + psk
        deps = list(self.pending[eng])
        self.pending[eng] = []
        for k in reads:
            deps.extend(self.writer.get(k, ()))
        for k in writes:
            deps.extend(self.writer.get(k, ()))
            deps.extend(self.readers.get(k, ()))
        for d in deps:
            if d.dma:
                o.deps.append(("d", d.dsem, max(d.dval, self.dcum[d.dsem])))
            else:
                if d.eng == eng and (eng == "pe"):
                    continue
                o.deps.append(("e", d))
        if not dma:
            rset = set()
            for k in reads:
                for w in self.writer.get(k, ()):
                    rset.add(id(w))
            o.deps = [t for t in o.deps if not (t[0] == "e" and t[1].eng == eng and id(t[1]) not in rset)]
        if dma:
            k = self.drr
            self.drr = (self.drr + 1) % self.NDSEM
            self.dcum[k] += 16
            o.dsem, o.dval = k, self.dcum[k]
        for t in o.deps:
            if t[0] == "e":
                t[1].signal = True
        for k in reads:
            self.readers.setdefault(k, []).append(o)
        for k in writes:
            prev = self.writer.get(k)
            if dma and prev and all(p.dma for p in prev) and not self.readers.get(k):
                prev.append(o)
            else:
                self.writer[k] = [o]
            self.readers[k] = []
        o.idx = len(self.ops[eng])
        self.ops[eng].append(o)
        return o

    def barrier(self):
        toks = []
        for e in self.ENGS:
            if self.ops[e]:
                last = None
                for o in reversed(self.ops[e]):
                    if not o.dma:
                        last = o
                        break
                if last is not None:
                    toks.append(last)
        dtoks = [k for k in range(self.NDSEM) if self.dcum[k] > 0]
        for e in self.ENGS:
            self.pending[e] = [t for t in toks if t.eng != e or e != "pe"]
            for k in dtoks:
                p = Op("sp", None, True)
                p.dsem, p.dval = k, self.dcum[k]
                self.pending[e].append(p)
        self.writer.clear()
        self.readers.clear()

    def prepare(self, es):
        nc = self.nc
        nsem_needed = {}
        for e in self.ENGS:
            c = 0
            for o in self.ops[e]:
                if o.signal and not o.dma:
                    c += 1
                    o.cnt = c
            nsem_needed[e] = c // self.SEM_WRAP + 1
        esems = {e: [es.enter_context(nc.semaphore(f"s_{e}_{i}")) for i in range(nsem_needed[e])] for e in self.ENGS}
        dsems = [es.enter_context(nc.semaphore(f"s_dma_{i}")) for i in range(self.NDSEM)]
        self.esems, self.dsems = esems, dsems

    def emit(self, block):
        esems, dsems = self.esems, self.dsems
        W = self.SEM_WRAP

        def sem_of(o):
            i = (o.cnt - 1) // W
            return esems[o.eng][i], o.cnt - i * W, i

        def run(engname, eng):
            waited = {}
            for o in self.ops[engname]:
                for t in o.deps:
                    if t[0] == "d":
                        key = ("d", t[1])
                        if waited.get(key, 0) >= t[2]:
                            continue
                        waited[key] = t[2]
                        eng.wait_ge(dsems[t[1]], t[2])
                    else:
                        s, v, i = sem_of(t[1])
                        key = ("e", t[1].eng, i)
                        if waited.get(key, 0) >= v:
                            continue
                        waited[key] = v
                        eng.wait_ge(s, v)
                ins = o.fn(eng)
                if o.dma:
                    ins.then_inc(dsems[o.dsem], 16)
                elif o.signal:
                    s, v, i = sem_of(o)
                    ins.then_inc(s, 1)
            if engname == "sp":
                for k in range(self.NDSEM):
                    if self.dcum[k] > 0 and waited.get(("d", k), 0) < self.dcum[k]:
                        eng.wait_ge(dsems[k], self.dcum[k])

        @block.tensor
        def _(e):
            run("pe", e)

        @block.scalar
        def _(e):
            run("act", e)

        @block.vector
        def _(e):
            run("dve", e)

        @block.gpsimd
        def _(e):
            run("pool", e)

        @block.sync
        def _(e):
            run("sp", e)


def build_program(depth=DEPTH, taps=None, stop_after=None, ne_decl=NE, dbg_stage=99):
    nc = bass.Bass("TRN2", target_bir_lowering=False)
    es = ExitStack()
    P = Plan(nc)
    taps = taps or {}
    tap_out = {}

    def din(name, shape):
        return nc.dram_tensor(name, list(shape), F32, kind="ExternalInput").ap()

    x_in = din("x", [NLAT, D])
    ctx_in = din("ctx", [NCTX, D])
    c_in = din("c", [1, D])
    cctx_in = din("c_ctx", [1, D])
    w_ada = din("w_ada", [DEPTH, D, 6 * D])
    b_ada = din("b_ada", [DEPTH, 6 * D])
    g_mix = din("g_mix", [DEPTH, D])
    g_ffn = din("g_ffn", [DEPTH, D])
    g_final = din("g_final", [1, D])
    w_in = din("w_in", [DEPTH, D, N_IN])
    dn_conv = din("dn_conv", [DEPTH, 3, 1536])
    dn_a_log = din("dn_a_log", [DEPTH, 8])
    dn_dt_bias = din("dn_dt_bias", [DEPTH, 8])
    dn_norm_g = din("dn_norm_g", [DEPTH, 128])
    hg_lb_logits = din("hg_lb_logits", [DEPTH, 512])
    hg_norm_g = din("hg_norm_g", [DEPTH, 128])
    w_br_dn = din("w_br_dn", [DEPTH, 512, D])
    w_br_hg = din("w_br_hg", [DEPTH, 512, D])
    w_out = din("w_out", [DEPTH, D, D])
    w_rg = din("w_router_grp", [DEPTH, D, 4])
    b_rg = din("b_router_grp", [DEPTH, 4])
    w_re = din("w_router_exp", [DEPTH, D, 32])
    b_re = din("b_router_exp", [DEPTH, 32])
    w_eg = din("w_exp_gate", [DEPTH, ne_decl, D, DEXP])
    w_eu = din("w_exp_up", [DEPTH, ne_decl, D, DEXP])
    w_ed = din("w_exp_down", [DEPTH, ne_decl, DEXP, D])
    out = nc.dram_tensor("out", [NLAT, D], F32, kind="ExternalOutput").ap()
    for nm, shp in taps.items():
        if shp is None:
            continue
        tap_out[nm] = nc.dram_tensor("tap_" + nm, list(shp), F32, kind="ExternalOutput").ap()

    Xd = nc.dram_tensor("Xd", [NTOK, D], F32).ap()
    modrow_d = nc.dram_tensor("modrow_d", [2, 6 * D], F32).ap()

    def sb(name, shape, dt=F32):
        return es.enter_context(nc.sbuf_tensor(name, list(shape), dt))

    hT = sb("hT", [128, KC, NTOK], BF16)
    ident_f = sb("ident_f", [128, 128], F32)
    ident_b = sb("ident_b", [128, 128], BF16)
    ones_f = sb("ones_f", [128, 128], F32)
    eps_c = sb("eps_c", [128, 1], F32)
    sT = sb("sT", [128, KC, 2], F32)
    modfm = sb("modfm", [128, 2, 6, KC], F32)
    gmixT = sb("gmixT", [128, KC], F32)
    gffnT = sb("gffnT", [128, KC], F32)
    AB = sb("AB", [128, 2, 2, KC], F32)
    ARENA = 42400
    A = sb("A", [128, ARENA], F32)
    WORK0 = 16384
    YMIX0 = ARENA - 9216
    wst = A[:, 0:8192].rearrange("p (s n) -> p s n", s=2)
    work = A[:, WORK0:ARENA]
    ymix = A[:, YMIX0:ARENA].bitcast(BF16).rearrange("p (b h n) -> p b h n", b=2, h=4)
    U_f = sb("U_f", [128, 128], F32)
    U_b = sb("U_b", [128, 128], F32)
    SU_f = sb("SU_f", [128, 128], F32)
    SU_b = sb("SU_b", [128, 128], F32)
    UBf = sb("UBf", [128, 128], F32)
    UBb = sb("UBb", [128, 128], F32)
    DBf = sb("DBf", [128, 128], F32)
    DBb = sb("DBb", [128, 128], F32)
    _consts = {"UBf": UBf, "UBb": UBb, "DBf": DBf, "DBb": DBb}

    def sb_const(n):
        return _consts[n]
    lgG = A[:, 41600:41672].rearrange("p (t n) -> p t n", t=NT)
    lgE = A[:, 41672:42248].rearrange("p (t n) -> p t n", t=NT)
    one_c = sb("one_c", [128, 1], F32)
    eps128_c = sb("eps128_c", [128, 1], F32)
    psum = es.enter_context(nc.psum_tensor("psum", [128, 8, 512], F32))

    V, S_, G, T_, SPQ = "dve", "act", "pool", "pe", "sp"

    P.op(G, lambda e: e.memset(ident_f[:], 0.0), writes=["ident_f"])
    P.op(G, lambda e: e.affine_select(out=ident_f[:], in_=ident_f[:], pattern=[[-1, 128]], compare_op=ALU.not_equal,
                                      fill=1.0, base=0, channel_multiplier=1), reads=["ident_f"], writes=["ident_f"])
    P.op(G, lambda e: e.tensor_copy(out=ident_b[:], in_=ident_f[:]), reads=["ident_f"], writes=["ident_b"])
    P.op(G, lambda e: e.memset(ones_f[:], 1.0), writes=["ones_f"])
    P.op(G, lambda e: e.memset(eps_c[:], EPS), writes=["eps_c"])
    P.op(G, lambda e: e.memset(one_c[:], 1.0), writes=["one_c"])
    P.op(G, lambda e: e.memset(eps128_c[:], 128.0 * EPS), writes=["eps128_c"])
    for (m_, cm, op_, nm) in ((U_f, -1, ALU.is_ge, "U_f"), (U_b, 1, ALU.is_ge, "U_b"), (SU_f, -1, ALU.is_gt, "SU_f"), (SU_b, 1, ALU.is_gt, "SU_b")):
        P.op(G, lambda e, m_=m_: e.memset(m_[:], 1.0), writes=[nm])
        P.op(G, lambda e, m_=m_, cm=cm, op_=op_: e.affine_select(out=m_[:], in_=m_[:], pattern=[[-cm, 128]], compare_op=op_, fill=0.0,
                                                               base=0, channel_multiplier=cm), reads=[nm], writes=[nm])

    for (dst_, src_, nm) in ((UBf, U_f, "U_f"), (UBb, U_b, "U_b"), (DBf, SU_b, "SU_b"), (DBb, SU_f, "SU_f")):
        P.op(G, lambda e, dst_=dst_, src_=src_: e.tensor_copy(out=dst_[:], in_=src_[:]), reads=[nm], writes=[nm + "B"])
        P.op(G, lambda e, dst_=dst_: e.memset(dst_[0:64, 64:128], 0.0), reads=[nm + "B"], writes=[nm + "B"])
        P.op(G, lambda e, dst_=dst_: e.memset(dst_[64:128, 0:64], 0.0), reads=[nm + "B"], writes=[nm + "B"])
    crow = work[0:2, 0:D]
    P.op(SPQ, lambda e: e.dma_start(out=work[0:1, 0:D], in_=c_in[:, :]), writes=["crow0"], dma=True)
    P.op(SPQ, lambda e: e.dma_start(out=work[1:2, 0:D], in_=cctx_in[:, :]), writes=["crow1"], dma=True)
    P.op(S_, lambda e: e.activation(out=crow, in_=crow, func=AF.Silu), reads=["crow0", "crow1"], writes=["crow"])
    for k in range(KC):
        P.op(T_, lambda e, k=k: e.transpose(psum[:, 0, 2 * k:2 * k + 2], crow[:, k * 128:(k + 1) * 128], ident_f[0:2, 0:2]),
             reads=["crow", "ident_f"], writes=[("ps", 0)])
    P.op(V, lambda e: e.tensor_copy(out=sT[:].rearrange("p k v -> p (k v)"), in_=psum[:, 0, 0:2 * KC]), reads=[("ps", 0)], writes=["sT"])

    def tap(name, ap_sb, key, dst=None):
        if name in tap_out:
            d = tap_out[name] if dst is None else dst
            P.op(SPQ, lambda e: e.dma_start(out=d, in_=ap_sb), reads=[key], writes=["tap_" + name], dma=True)

    def phase_mod(l):
        modrow = work[0:2, 0:6 * D]
        brow = work[0:2, 6 * D:12 * D]
        P.op(SPQ, lambda e: e.dma_start(out=brow, in_=b_ada[l:l + 1, :].partition_broadcast(2)), writes=["brow"], dma=True)
        NB = 24
        for nb in range(NB):
            slot = nb % 2
            P.op(SPQ, lambda e, nb=nb, slot=slot: e.dma_start(
                out=wst[:, slot, 0:KC * 256].rearrange("p (k n) -> p k n", k=KC),
                in_=w_ada[l, :, nb * 256:(nb + 1) * 256].rearrange("(k p) n -> p k n", p=128)),
                writes=[("wst", slot)], dma=True)
            pb = psum[0:2, nb % 2, 0:256]
            for k in range(KC):
                P.op(T_, lambda e, k=k, slot=slot, pb=pb: e.matmul(pb, lhsT=sT[:, k, :], rhs=wst[:, slot, k * 256:(k + 1) * 256],
                                                                  start=(k == 0), stop=(k == KC - 1)),
                     reads=[("wst", slot), "sT"], writes=[("ps", nb % 2)])
            P.op(V, lambda e, nb=nb, pb=pb: e.tensor_tensor(out=modrow[:, nb * 256:(nb + 1) * 256], in0=pb,
                                                            in1=brow[:, nb * 256:(nb + 1) * 256], op=ALU.add),
                 reads=[("ps", nb % 2), "brow"], writes=["modrow"])
        P.op(SPQ, lambda e: e.dma_start(out=modrow_d[:, :], in_=modrow), reads=["modrow"], writes=["modrow_d"], dma=True)
        tap(f"modrow{l}", modrow, "modrow")
        for j in range(48):
            P.op(T_, lambda e, j=j: e.transpose(psum[:, 2, 2 * j:2 * j + 2], modrow[:, j * 128:(j + 1) * 128], ident_f[0:2, 0:2]),
                 reads=["modrow", "ident_f"], writes=[("ps", 2)])
        P.op(V, lambda e: e.tensor_copy(out=modfm[:].rearrange("p v w k -> p v (w k)"),
                                        in_=psum[:, 2, 0:96].rearrange("p (j v) -> p v j", v=2)),
             reads=[("ps", 2)], writes=[("modfm", 0), ("modfm", 1)])
        grow = work[0:2, 12 * D:13 * D]
        P.op(SPQ, lambda e: e.dma_start(out=work[0:1, 12 * D:13 * D], in_=g_mix[l:l + 1, :]), writes=["grow0"], dma=True)
        P.op(SPQ, lambda e: e.dma_start(out=work[1:2, 12 * D:13 * D], in_=g_ffn[l:l + 1, :]), writes=["grow1"], dma=True)
        for k in range(KC):
            P.op(T_, lambda e, k=k: e.transpose(psum[:, 3, 2 * k:2 * k + 2], grow[:, k * 128:(k + 1) * 128], ident_f[0:2, 0:2]),
                 reads=["grow0", "grow1", "ident_f"], writes=[("ps", 3)])
        P.op(V, lambda e: e.tensor_copy(out=gmixT[:], in_=psum[:, 3, 0:2 * KC].rearrange("p (k v) -> p v k", v=2)[:, 0, :]),
             reads=[("ps", 3)], writes=["gmixT"])
        P.op(V, lambda e: e.tensor_copy(out=gffnT[:], in_=psum[:, 3, 0:2 * KC].rearrange("p (k v) -> p v k", v=2)[:, 1, :]),
             reads=[("ps", 3)], writes=["gffnT"])

    def set_AB(which):
        sh, sc = (0, 1) if which == 0 else (3, 4)
        g = gmixT if which == 0 else gffnT
        gk = "gmixT" if which == 0 else "gffnT"
        for v in range(2):
            P.op(V, lambda e, v=v: e.scalar_tensor_tensor(out=AB[:, v, 0, :], in0=modfm[:, v, sc, :], scalar=1.0, in1=g[:],
                                                          op0=ALU.add, op1=ALU.mult),
                 reads=[("modfm", v), gk], writes=[("AB", v)])
            P.op(V, lambda e, v=v: e.tensor_copy(out=AB[:, v, 1, :], in_=modfm[:, v, sh, :]),
                 reads=[("modfm", v)], writes=[("AB", v)])

    def x_tile_src(l, first_read, t):
        if l == 0 and first_read:
            return ctx_in[t * 128:(t + 1) * 128, :] if t < 2 else x_in[(t - 2) * 128:(t - 1) * 128, :]
        return Xd[t * 128:(t + 1) * 128, :]

    def phase_norm(l, which, tiles):
        set_AB(which)
        xt = [work[:, j * D:(j + 1) * D] for j in range(4)]
        xn = [work[:, 4 * D:5 * D], work[:, 5 * D:6 * D]]
        t32 = [work[:, 6 * D:7 * D], work[:, 7 * D:8 * D]]
        lo = [work[:, 8 * D:8 * D + D // 2].bitcast(BF16), work[:, 8 * D + D // 2:9 * D].bitcast(BF16)]
        junk = work[:, 9 * D:9 * D + D // 2].bitcast(BF16)
        st = work[:, 10 * D:10 * D + 64]
        wr_f = work[:, 11 * D:11 * D + 288]
        wr_hi = work[:, 11 * D + 288:11 * D + 432].bitcast(BF16)
        wr_lo = work[:, 11 * D + 432:11 * D + 576].bitcast(BF16)
        wr_t = work[:, 11 * D + 576:11 * D + 864]
        brow = work[:, 11 * D + 864:11 * D + 900]
        if which == 1:
            P.op(SPQ, lambda e: e.dma_start(out=wr_f.rearrange("p (k n) -> p k n", k=KC)[:, :, 0:4],
                                            in_=w_rg[l].rearrange("(k p) n -> p k n", p=128)), writes=["wr_f0"], dma=True)
            P.op(SPQ, lambda e: e.dma_start(out=wr_f.rearrange("p (k n) -> p k n", k=KC)[:, :, 4:36],
                                            in_=w_re[l].rearrange("(k p) n -> p k n", p=128)), writes=["wr_f1"], dma=True)
            P.op(SPQ, lambda e: e.dma_start(out=brow[:, 0:4], in_=b_rg[l:l + 1, :].partition_broadcast(128)), writes=["brow0"], dma=True)
            P.op(SPQ, lambda e: e.dma_start(out=brow[:, 4:36], in_=b_re[l:l + 1, :].partition_broadcast(128)), writes=["brow1"], dma=True)
            P.op(V, lambda e: e.tensor_copy(out=wr_hi, in_=wr_f), reads=["wr_f0", "wr_f1"], writes=["wr_hi"])
            P.op(V, lambda e: e.tensor_tensor(out=wr_t, in0=wr_f, in1=wr_hi, op=ALU.subtract), reads=["wr_hi", "wr_f0", "wr_f1"], writes=["wr_t"])
            P.op(V, lambda e: e.tensor_copy(out=wr_lo, in_=wr_t), reads=["wr_t"], writes=["wr_lo"])
        def stage1(i, t):
            b = i % 2
            xb = i % 4
            v = 1 if t < 2 else 0
            src = x_tile_src(l, which == 0, t)
            P.op(SPQ, lambda e, xb=xb, src=src: e.dma_start(out=xt[xb], in_=src), reads=[("Xd", t)],
                 writes=[("xt", xb)], dma=True)
            P.op(S_, lambda e, b=b, xb=xb: e.activation(out=junk, in_=xt[xb], func=AF.Square, accum_out=st[:, 2 * b:2 * b + 1]),
                 reads=[("xt", xb)], writes=["junk", ("st", b)])
            P.op(S_, lambda e, b=b: e.activation(out=st[:, 2 * b + 1:2 * b + 2], in_=st[:, 2 * b:2 * b + 1], func=AF.Ln, scale=1.0 / D, bias=eps_c[:]),
                 reads=[("st", b), "eps_c"], writes=[("st2", b)])
            P.op(S_, lambda e, b=b: e.activation(out=st[:, 2 * b + 1:2 * b + 2], in_=st[:, 2 * b + 1:2 * b + 2], func=AF.Exp, scale=-0.5),
                 reads=[("st2", b)], writes=[("st2", b)])
            P.op(G, lambda e, b=b, xb=xb: e.tensor_scalar(out=xn[b], in0=xt[xb], scalar1=st[:, 2 * b + 1:2 * b + 2], scalar2=None, op0=ALU.mult),
                 reads=[("xt", xb), ("st2", b)], writes=[("xn", b)])

        def stage2(i, t):
            b = i % 2
            v = 1 if t < 2 else 0
            banks = (2 * b, 2 * b + 1)
            for k in range(KC):
                bk = banks[k // 4]
                P.op(T_, lambda e, b=b, k=k, bk=bk: e.transpose(psum[:, bk, (k % 4) * 128:(k % 4 + 1) * 128], xn[b][:, k * 128:(k + 1) * 128], ident_f[:]),
                     reads=[("xn", b), "ident_f"], writes=[("ps", bk)])
            for k in range(KC):
                bk = banks[k // 4]
                src_ps = psum[:, bk, (k % 4) * 128:(k % 4 + 1) * 128]
                dst = t32[b][:, k * 128:(k + 1) * 128] if which == 1 else hT[:, k, t * 128:(t + 1) * 128]
                wk = [("t32", b, k // 4)] if which == 1 else [("hT", k, t)]
                if k < 4:
                    P.op(S_, lambda e, k=k, v=v, src_ps=src_ps, dst=dst: e.activation(
                        out=dst, in_=src_ps, func=AF.Identity, scale=AB[:, v, 0, k:k + 1], bias=AB[:, v, 1, k:k + 1]),
                        reads=[("ps", bk), ("AB", v)], writes=wk)
                else:
                    P.op(V, lambda e, k=k, v=v, src_ps=src_ps, dst=dst: e.tensor_scalar(
                        out=dst, in0=src_ps, scalar1=AB[:, v, 0, k:k + 1], scalar2=AB[:, v, 1, k:k + 1], op0=ALU.mult, op1=ALU.add),
                        reads=[("ps", bk), ("AB", v)], writes=wk)
            if which == 1:
                t3 = t32[b].rearrange("p (k n) -> p k n", k=KC)
                l3 = lo[b].rearrange("p (k n) -> p k n", k=KC)
                hview = hT[:, :, t * 128:(t + 1) * 128]
                P.op(G, lambda e, t3=t3, hview=hview: e.tensor_copy(out=hview, in_=t3), reads=[("t32", b, 0), ("t32", b, 1)],
                     writes=[("hT", k, t) for k in range(KC)])
                P.op(V, lambda e, t3=t3, l3=l3, hview=hview: e.tensor_tensor(out=l3, in0=t3, in1=hview, op=ALU.subtract),
                     reads=[("t32", b, 0), ("t32", b, 1)] + [("hT", k, t) for k in range(KC)], writes=[("lo", b)])
                rb = 4 + b
                n_mm = 3 * KC
                j = 0
                for k in range(KC):
                    for (lh, rw, kk) in ((hT[:, k, t * 128:(t + 1) * 128], wr_hi, ("hT", k, t)), (lo[b][:, k * 128:(k + 1) * 128], wr_hi, ("lo", b)),
                                         (hT[:, k, t * 128:(t + 1) * 128], wr_lo, ("hT", k, t))):
                        P.op(T_, lambda e, lh=lh, rw=rw, k=k, j=j, rb=rb: e.matmul(psum[:, rb, 0:36], lhsT=lh, rhs=rw[:, k * 36:(k + 1) * 36],
                                                                                  start=(j == 0), stop=(j == n_mm - 1)),
                             reads=[kk, "wr_hi", "wr_lo"], writes=[("ps", rb)])
                        j += 1
                P.op(V, lambda e, t=t, rb=rb: e.tensor_tensor(out=lgG[:, t, :], in0=psum[:, rb, 0:4], in1=brow[:, 0:4], op=ALU.add),
                     reads=[("ps", rb), "brow0"], writes=[("lgG", t)])
                P.op(V, lambda e, t=t, rb=rb: e.tensor_tensor(out=lgE[:, t, :], in0=psum[:, rb, 4:36], in1=brow[:, 4:36], op=ALU.add),
                     reads=[("ps", rb), "brow1"], writes=[("lgE", t)])

        for i in range(len(tiles) + 1):
            if i < len(tiles):
                stage1(i, tiles[i])
            if i >= 1:
                stage2(i - 1, tiles[i - 1])

    PADW = 2307
    TBLK = [(0, 256)] + [(256 + 512 * i, 512) for i in range(4)]

    def pcol(n):
        return n + 1 if n < NCTX else n + 2

    bank_rr = [0]

    def next_bank():
        b = bank_rr[0]
        bank_rr[0] = (b + 1) % 8
        return b

    evac_rr = [0]

    def evac_eng():
        evac_rr[0] ^= 1
        return S_ if evac_rr[0] else V

    def copy_op(eng, out, in_, reads, writes):
        if eng == S_:
            P.op(S_, lambda e: e.activation(out=out, in_=in_, func=AF.Copy), reads=reads, writes=writes)
        else:
            P.op(eng, lambda e: e.tensor_copy(out=out, in_=in_), reads=reads, writes=writes)

    def load_w_cols(l, slot, groups, key):
        res = []
        off = 0
        for (c0, n) in groups:
            P.op(SPQ, lambda e, c0=c0, n=n, off=off: e.dma_start(
                out=wst[:, slot, off:off + KC * n].rearrange("p (k n) -> p k n", k=KC),
                in_=w_in[l, :, c0:c0 + n].rearrange("(k p) n -> p k n", p=128)),
                writes=[("wst", slot)], dma=True)
            res.append((off, n))
            off += KC * n
        return res

    def interleave(gens):
        active = []
        pending = list(gens)
        while active or pending:
            while pending and len(active) < 4:
                active.append(pending.pop(0))
            nxt = []
            for g in active:
                try:
                    next(g)
                    nxt.append(g)
                except StopIteration:
                    pass
            active = nxt

    def phase_dn(l, ctx_out):
        W0 = 10240
        wbf_dn = A[:, 8192:10240].bitcast(BF16)
        pre = A[:, W0:W0 + PADW]
        yq = A[:, W0 + 2308:W0 + 2308 + PADW]
        yk = A[:, W0 + 4616:W0 + 4616 + PADW]
        yv = A[:, W0 + 6924:W0 + 6924 + PADW]
        o = W0 + 9232
        ktok = A[:, o:o + 2304].rearrange("p (t n) -> p t n", t=NT); o += 2304
        vtok = A[:, o:o + 2304].rearrange("p (t n) -> p t n", t=NT); o += 2304
        oacc = A[:, o:o + 2304].rearrange("p (t n) -> p t n", t=NT); o += 2304
        zs = A[:, o:o + 1152].bitcast(BF16).rearrange("p (t n) -> p t n", t=NT); o += 1152
        ba_raw = A[:, o:o + 288].rearrange("p (t n) -> p t n", t=NT); o += 288
        beta = A[:, o:o + 144].rearrange("p (d t h) -> p d t h", d=2, t=NT); o += 144
        gg = A[:, o:o + 144].rearrange("p (d t h) -> p d t h", d=2, t=NT); o += 144
        gc = A[:, o:o + 144].rearrange("p (d t h) -> p d t h", d=2, t=NT); o += 144
        egc = A[:, o:o + 144].rearrange("p (d t h) -> p d t h", d=2, t=NT); o += 144
        dl = A[:, o:o + 144].rearrange("p (d t h) -> p d t h", d=2, t=NT); o += 144
        egl = A[:, o:o + 144].rearrange("p (d t h) -> p d t h", d=2, t=NT); o += 144
        negA = A[:, o:o + 8]; o += 8
        dtb = A[:, o:o + 8]; o += 8
        cw = A[:, o:o + 36].rearrange("p (c k) -> p c k", k=3); o += 36
        gnb = A[:, o:o + 128]; o += 128
        sq = A[:, o:o + 512]; o += 512
        rn = A[:, o:o + 512]; o += 512
        stat = A[:, o:o + 64]; o += 64
        wba_f = A[:, o:o + 128]; o += 128
        wba_b = A[:, o:o + 64].bitcast(BF16); o += 64
        Sbuf = [A[:, o + 128 * i:o + 128 * (i + 1)] for i in range(4)]; o += 512
        xslots_extra = o
        o += 16 * 128
        assert o <= YMIX0, (o, YMIX0)
        NSLOT = 13
        def uslot(u, i):
            if u >= 4:
                base = (u - 4) * NSLOT * 128
                return A[:, base + 128 * i:base + 128 * (i + 1)]
            if u < 2:
                base = W0 + (0 if u == 0 else 6924)
            else:
                base = xslots_extra if u == 2 else None
            if u == 3:
                if i < 3:
                    base = xslots_extra + 13 * 128
                    return A[:, base + 128 * i:base + 128 * (i + 1)]
                j = i - 3
                if j < 5:
                    base = W0 + 13 * 128
                    return A[:, base + 128 * j:base + 128 * (j + 1)]
                base = W0 + 6924 + 13 * 128
                return A[:, base + 128 * (j - 5):base + 128 * (j - 4)]
            return A[:, base + 128 * i:base + 128 * (i + 1)]

        def uslot2(u, i):
            a = uslot(u, i)
            b = uslot(u, i + 1)
            return a, b

        cwrow = A[0:3, W0:W0 + 1536]
        P.op(SPQ, lambda e: e.dma_start(out=cwrow, in_=dn_conv[l]), writes=["cwrow"], dma=True)
        bk = next_bank()
        for c in range(12):
            P.op(T_, lambda e, c=c, bk=bk: e.transpose(psum[:, bk, 3 * c:3 * c + 3], cwrow[:, c * 128:(c + 1) * 128], ident_f[0:3, 0:3]),
                 reads=["cwrow", "ident_f"], writes=[("ps", bk)])
        P.op(V, lambda e, bk=bk: e.tensor_copy(out=cw[:].rearrange("p c k -> p (c k)"), in_=psum[:, bk, 0:36]), reads=[("ps", bk)], writes=["cw"])
        P.op(SPQ, lambda e: e.dma_start(out=negA, in_=dn_a_log[l:l + 1, :].partition_broadcast(128)), writes=["negA"], dma=True)
        P.op(SPQ, lambda e: e.dma_start(out=dtb, in_=dn_dt_bias[l:l + 1, :].partition_broadcast(128)), writes=["dtb"], dma=True)
        P.op(SPQ, lambda e: e.dma_start(out=gnb, in_=dn_norm_g[l:l + 1, :].partition_broadcast(128)), writes=["gnb"], dma=True)
        P.op(S_, lambda e: e.activation(out=negA, in_=negA, func=AF.Exp), reads=["negA"], writes=["negA"])
        P.op(V, lambda e: e.tensor_scalar(out=negA, in0=negA, scalar1=-1.0, scalar2=None, op0=ALU.mult), reads=["negA"], writes=["negA"])
        P.op(SPQ, lambda e: e.dma_start(out=wba_f.rearrange("p (k n) -> p k n", k=KC),
                                        in_=w_in[l, :, OFF_DNB:OFF_DNB + 16].rearrange("(k p) n -> p k n", p=128)),
             writes=["wba_f"], dma=True)
        P.op(V, lambda e: e.tensor_copy(out=wba_b, in_=wba_f), reads=["wba_f"], writes=["wba_b"])
        bk = next_bank()
        pba = psum[:, bk, 0:288].rearrange("p (t n) -> p t n", t=NT)
        for t in range(NT):
            for k in range(KC):
                P.op(T_, lambda e, t=t, k=k: e.matmul(pba[:, t, :], lhsT=hT[:, k, t * 128:(t + 1) * 128], rhs=wba_b[:, k * 16:(k + 1) * 16],
                                                      start=(k == 0), stop=(k == KC - 1)),
                     reads=["wba_b"] + [("hT", k, t)], writes=[("ps", bk)])
        P.op(V, lambda e: e.tensor_copy(out=ba_raw, in_=pba), reads=[("ps", bk)], writes=["ba_raw"])
        for d in range(2):
            P.op(S_, lambda e, d=d: e.activation(out=beta[:, d, :, :], in_=ba_raw[:, :, 4 * d:4 * d + 4], func=AF.Sigmoid),
                 reads=["ba_raw"], writes=[("beta", d)])
            P.op(V, lambda e, d=d: e.tensor_tensor(out=gg[:, d, :, :], in0=ba_raw[:, :, 8 + 4 * d:12 + 4 * d],
                                                   in1=dtb[:, 4 * d:4 * d + 4].unsqueeze(1).to_broadcast([128, NT, 4]), op=ALU.add),
                 reads=["ba_raw", "dtb"], writes=[("gg", d)])
            P.op(S_, lambda e, d=d: e.activation(out=gg[:, d, :, :], in_=gg[:, d, :, :], func=AF.Exp), reads=[("gg", d)], writes=[("gg", d)])
            P.op(S_, lambda e, d=d: e.activation(out=gg[:, d, :, :], in_=gg[:, d, :, :], func=AF.Ln, bias=one_c[:]), reads=[("gg", d), "one_c"],
                 writes=[("gg", d)])
            P.op(V, lambda e, d=d: e.tensor_tensor(out=gg[:, d, :, :], in0=gg[:, d, :, :],
                                                   in1=negA[:, 4 * d:4 * d + 4].unsqueeze(1).to_broadcast([128, NT, 4]), op=ALU.mult),
                 reads=[("gg", d), "negA"], writes=[("gg", d)])
        bk = next_bank()
        for d in range(2):
            P.op(T_, lambda e, d=d: e.matmul(psum[:, bk, 72 * d:72 * d + 72], lhsT=(U_f if d == 0 else U_b)[:],
                                             rhs=gg[:, d, :, :].rearrange("p t h -> p (t h)"), start=True, stop=True),
                 reads=[("gg", d), "U_f", "U_b"], writes=[("ps", bk)])
        P.op(T_, lambda e: e.matmul(psum[:, bk, 144:288], lhsT=ones_f[:], rhs=gg[:].rearrange("p d t h -> p (d t h)"), start=True, stop=True),
             reads=[("gg", 0), ("gg", 1), "ones_f"], writes=[("ps", bk)])
        flat = lambda a: a[:].rearrange("p d t h -> p (d t h)")
        P.op(V, lambda e: e.tensor_copy(out=flat(gc), in_=psum[:, bk, 0:144]), reads=[("ps", bk)], writes=["gc"])
        P.op(V, lambda e: e.tensor_tensor(out=flat(dl), in0=psum[:, bk, 144:288], in1=flat(gc), op=ALU.subtract), reads=[("ps", bk), "gc"],
             writes=["dl"])
        P.op(S_, lambda e: e.activation(out=flat(egl), in_=psum[:, bk, 144:288], func=AF.Exp), reads=[("ps", bk)], writes=["egl"])
        P.op(S_, lambda e: e.activation(out=flat(dl), in_=flat(dl), func=AF.Exp), reads=["dl"], writes=["dl"])
        P.op(S_, lambda e: e.activation(out=flat(egc), in_=flat(gc), func=AF.Exp), reads=["gc"], writes=["egc"])
        if "dn_gc" in tap_out:
            tap("dn_gc", flat(gc), "gc")
            tap("dn_beta", flat(beta), ("beta", 0))

        tiles_f = list(range(NT))
        tiles_b = [1, 0] + list(range(NT - 1, 1, -1))

        for h in range(4):
            P.barrier()
            ws = h % 2
            grp = load_w_cols(l, ws, [(OFF_DNQ + 128 * h, 128), (OFF_DNK + 128 * h, 128), (OFF_DNV + 128 * h, 128), (OFF_DNZ + 128 * h, 128)],
                              "dnw")
            wb = wbf_dn[:, 0:4096]
            if h == 0 and "dn_wst" in tap_out:
                tap("dn_wst", wst[:, ws, 0:4096], ("wst", ws))
            for j in range(4):
                eng = (G, V, G, V)[j]
                P.op(eng, lambda e, j=j: e.tensor_copy(out=wb[:, j * 1024:(j + 1) * 1024], in_=wst[:, ws, j * 1024:(j + 1) * 1024]),
                     reads=[("wst", ws)], writes=[("wb", ws, j)])
            for pc in (0, 257, 2306):
                P.op(G, lambda e, pc=pc: e.memset(pre[:, pc:pc + 1], 0.0), writes=[("prepad", pc)])
            PREK = [("pre", t0) for (t0, tn) in TBLK] + [("prepad", pc) for pc in (0, 257, 2306)]
            ydst = (yq, yk, yv)
            for j in range(3):
                for (t0, tn) in TBLK:
                    bk = next_bank()
                    for k in range(KC):
                        P.op(T_, lambda e, j=j, k=k, t0=t0, tn=tn, bk=bk: e.matmul(
                            psum[:, bk, 0:tn], lhsT=wb[:, j * 1024 + k * 128:j * 1024 + (k + 1) * 128], rhs=hT[:, k, t0:t0 + tn],
                            start=(k == 0), stop=(k == KC - 1)),
                            reads=[("wb", ws, j)] + [("hT", k, t) for t in range(t0 // 128, (t0 + tn) // 128)], writes=[("ps", bk)])
                    c0 = pcol(t0)
                    copy_op(evac_eng(), pre[:, c0:c0 + tn], psum[:, bk, 0:tn], [("ps", bk)], [("pre", t0)])
                y = ydst[j]
                cj = 4 * j + h
                if h == 0 and j == 2 and "dn_pre" in tap_out:
                    tap("dn_pre", pre[:, 0:PADW], ("pre", 0))
                    tap("dn_cw", cw[:].rearrange("p c k -> p (c k)"), "cw")
                P.op(S_, lambda e, y=y, cj=cj: e.activation(out=y[:, 1:2306], in_=pre[:, 1:2306], func=AF.Copy, scale=cw[:, cj, 1:2]),
                     reads=PREK + ["cw"], writes=[("y", j)])
                P.op(V, lambda e, y=y, cj=cj: e.scalar_tensor_tensor(out=y[:, 1:2306], in0=pre[:, 0:2305], scalar=cw[:, cj, 0:1], in1=y[:, 1:2306],
                                                                     op0=ALU.mult, op1=ALU.add), reads=PREK + ["cw", ("y", j)], writes=[("y", j)])
                P.op(V, lambda e, y=y, cj=cj: e.scalar_tensor_tensor(out=y[:, 1:2306], in0=pre[:, 2:2307], scalar=cw[:, cj, 2:3], in1=y[:, 1:2306],
                                                                     op0=ALU.mult, op1=ALU.add), reads=PREK + ["cw", ("y", j)], writes=[("y", j)])
                P.op(S_, lambda e, y=y: e.activation(out=y[:, 1:2306], in_=y[:, 1:2306], func=AF.Silu), reads=[("y", j)], writes=[("y", j)])
                if j < 2:
                    for (t0, tn) in TBLK:
                        c0 = pcol(t0)
                        bk = next_bank()
                        P.op(S_, lambda e, y=y, c0=c0, tn=tn: e.activation(out=sq[:, 0:tn], in_=y[:, c0:c0 + tn], func=AF.Square),
                             reads=[("y", j)], writes=["sq"])
                        P.op(T_, lambda e, bk=bk, tn=tn: e.matmul(psum[:, bk, 0:tn], lhsT=ones_f[:], rhs=sq[:, 0:tn], start=True, stop=True),
                             reads=["sq", "ones_f"], writes=[("ps", bk)])
                        if j == 0:
                            P.op(S_, lambda e, bk=bk, tn=tn: e.activation(out=rn[:, 0:tn], in_=psum[:, bk, 0:tn], func=AF.Ln, scale=128.0,
                                                                          bias=eps128_c[:]), reads=[("ps", bk), "eps128_c"], writes=["rn"])
                        else:
                            P.op(S_, lambda e, bk=bk, tn=tn: e.activation(out=rn[:, 0:tn], in_=psum[:, bk, 0:tn], func=AF.Ln, bias=eps_c[:]),
                                 reads=[("ps", bk), "eps_c"], writes=["rn"])
                        P.op(S_, lambda e, tn=tn: e.activation(out=rn[:, 0:tn], in_=rn[:, 0:tn], func=AF.Exp, scale=-0.5), reads=["rn"],
                             writes=["rn"])
                        P.op(V, lambda e, y=y, c0=c0, tn=tn: e.tensor_tensor(out=y[:, c0:c0 + tn], in0=y[:, c0:c0 + tn], in1=rn[:, 0:tn],
                                                                             op=ALU.mult), reads=["rn", ("y", j)], writes=[("y", j)])
            for t4 in range(0, NT, 4):
                nt4 = min(4, NT - t4)
                bk = next_bank()
                for ti in range(nt4):
                    t = t4 + ti
                    for k in range(KC):
                        P.op(T_, lambda e, t=t, ti=ti, k=k, bk=bk: e.matmul(
                            psum[:, bk, ti * 128:(ti + 1) * 128], lhsT=hT[:, k, t * 128:(t + 1) * 128],
                            rhs=wb[:, 3 * 1024 + k * 128:3 * 1024 + (k + 1) * 128], start=(k == 0), stop=(k == KC - 1)),
                            reads=[("wb", ws, 3), ("hT", k, t)], writes=[("ps", bk)])
                P.op(S_, lambda e, t4=t4, nt4=nt4, bk=bk: e.activation(out=sq[:, 0:nt4 * 128], in_=psum[:, bk, 0:nt4 * 128], func=AF.Silu),
                     reads=[("ps", bk)], writes=["sq"])
                P.op(G, lambda e, t4=t4, nt4=nt4: e.tensor_tensor(out=zs[:, t4:t4 + nt4, :], in0=sq[:, 0:nt4 * 128].rearrange("p (t n) -> p t n", n=128),
                                                                  in1=gnb.unsqueeze(1).to_broadcast([128, nt4, 128]), op=ALU.mult),
                     reads=["sq", "gnb"], writes=[("zs", t4)])
            for (src, dst, nm, j) in ((yk, ktok, "ktok", 1), (yv, vtok, "vtok", 2)):
                for t4 in range(0, NT, 4):
                    nt4 = min(4, NT - t4)
                    bk = next_bank()
                    for ti in range(nt4):
                        t = t4 + ti
                        c0 = pcol(t * 128)
                        P.op(T_, lambda e, src=src, c0=c0, ti=ti, bk=bk: e.transpose(psum[:, bk, ti * 128:(ti + 1) * 128], src[:, c0:c0 + 128],
                                                                                    ident_f[:]),
                             reads=[("y", j), "ident_f"], writes=[("ps", bk)])
                    copy_op(evac_eng(), dst[:, t4:t4 + nt4, :], psum[:, bk, 0:nt4 * 128].rearrange("p (t n) -> p t n", n=128),
                            [("ps", bk)], [(nm, t4)])
            if h == 0 and "dn_qT" in tap_out:
                tap("dn_qT", yq[:, 0:PADW], ("y", 0))
                tap("dn_kT", yk[:, 0:PADW], ("y", 1))
                tap("dn_vtok", vtok[:].rearrange("p t n -> p (t n)"), ("vtok", 0))
            P.barrier()
            for d in range(2):
                P.op(G, lambda e, d=d: e.memset(Sbuf[2 * d][:], 0.0), writes=[("S", d, 0)])

            oacc_written = set()

            def unit(ui, us, d, t, first, pp):
                kq = ("u", us)
                bA = bB = us
                sl = lambda i: uslot(us, i)
                K = lambda n: ("u", us, n)
                c0 = pcol(t * 128)
                Um = U_f if d == 0 else U_b
                MI = U_f if d == 0 else U_b
                MS = SU_f if d == 0 else SU_b
                col = lambda a: a[:, d, t, h:h + 1]
                gb, E, Ei, M, PTt, MT, Xa, Xb, EG, qgT, kg, kdec, nwT = [sl(i) for i in (0, 1, 2, 3, 4, 5, 6, 7, 8, 9, 10, 11, 12)]
                P.op(G, lambda e: e.tensor_scalar(out=gb, in0=ones_f[:], scalar1=col(gg), scalar2=None, op0=ALU.mult),
                     reads=[("gg", d), "ones_f"], writes=[K("gb")])
                yield
                P.op(T_, lambda e: e.matmul(psum[:, bA, 0:128], lhsT=gb, rhs=Um[:], start=True, stop=True), reads=[K("gb")], writes=[("ps", bA)])
                P.op(T_, lambda e: e.matmul(psum[:, bA, 128:256], lhsT=yk[:, c0:c0 + 128], rhs=yk[:, c0:c0 + 128], start=True, stop=True),
                     reads=[("y", 1)], writes=[("ps", bA)])
                P.op(T_, lambda e: e.matmul(psum[:, bA, 256:384], lhsT=yk[:, c0:c0 + 128], rhs=yq[:, c0:c0 + 128], start=True, stop=True),
                     reads=[("y", 1), ("y", 0)], writes=[("ps", bA)])
                yield
                P.op(V, lambda e: e.tensor_scalar(out=E, in0=psum[:, bA, 0:128], scalar1=col(gc), scalar2=0.0, op0=ALU.subtract, op1=ALU.min),
                     reads=[("ps", bA), "gc"], writes=[K("E")])
                yield
                P.op(S_, lambda e: e.activation(out=E, in_=E, func=AF.Exp), reads=[K("E")], writes=[K("E")])
                P.op(S_, lambda e: e.activation(out=EG, in_=psum[:, bA, 0:128], func=AF.Exp), reads=[("ps", bA)], writes=[K("EG")])
                yield
                P.op(G, lambda e: e.tensor_tensor(out=Ei, in0=E, in1=MI[:], op=ALU.mult), reads=[K("E")], writes=[K("Ei")])
                P.op(G, lambda e: e.tensor_tensor(out=qgT, in0=yq[:, c0:c0 + 128], in1=EG, op=ALU.mult), reads=[K("EG"), ("y", 0)], writes=[K("qgT")])
                yield
                P.op(V, lambda e: e.tensor_tensor(out=PTt, in0=psum[:, bA, 256:384], in1=Ei, op=ALU.mult), reads=[("ps", bA), K("Ei")], writes=[K("PT")])
                P.op(V, lambda e: e.scalar_tensor_tensor(out=M, in0=psum[:, bA, 128:256], scalar=col(beta), in1=Ei, op0=ALU.mult, op1=ALU.mult),
                     reads=[("ps", bA), K("Ei"), ("beta", d)], writes=[K("M")])
                yield
                P.op(G, lambda e: e.tensor_tensor(out=M, in0=M, in1=MS[:], op=ALU.mult), reads=[K("M")], writes=[K("M")])
                P.op(G, lambda e: e.tensor_tensor(out=Xa, in0=ident_f[:], in1=M, op=ALU.subtract), reads=[K("M")], writes=[K("Xa")])
                P.op(S_, lambda e: e.activation(out=kg, in_=ktok[:, t, :], func=AF.Copy, scale=col(egc)), reads=["egc", ("ktok", (t // 4) * 4)],
                     writes=[K("kg")])
                P.op(S_, lambda e: e.activation(out=kdec, in_=ktok[:, t, :], func=AF.Copy, scale=col(dl)), reads=["dl", ("ktok", (t // 4) * 4)],
                     writes=[K("kdec")])
                yield
                P.op(T_, lambda e: e.transpose(psum[:, bB, 0:128], M, ident_f[:]), reads=[K("M")], writes=[("ps", bB)])
                yield
                P.op(S_, lambda e: e.activation(out=MT, in_=psum[:, bB, 0:128], func=AF.Copy), reads=[("ps", bB)], writes=[K("MT")])
                yield
                Pprev, PTprev, kP, kPT = M, MT, K("M"), K("MT")
                Xcur, Xnxt, kX, kXn = Xa, Xb, K("Xa"), K("Xb")
                for st in range(1, 7):
                    pa, pb_ = (sl(0), sl(1)) if st % 2 == 1 else (sl(2), sl(3))
                    kpa, kpb = (K("gb"), K("E")) if st % 2 == 1 else (K("Ei"), K("M"))
                    if st < 6:
                        P.op(T_, lambda e, PTprev=PTprev, Pprev=Pprev: e.matmul(psum[:, bB, 0:128], lhsT=PTprev, rhs=Pprev, start=True, stop=True),
                             reads=[kP, kPT], writes=[("ps", bB)])
                    P.op(T_, lambda e, PTprev=PTprev, Pprev=Pprev: e.matmul(psum[:, bB, 128:256], lhsT=Pprev, rhs=PTprev, start=True, stop=True),
                         reads=[kP, kPT], writes=[("ps", bB)])
                    yield
                    eng = S_ if st % 2 == 1 else V
                    if st < 6:
                        copy_op(eng, pa, psum[:, bB, 0:128], [("ps", bB)], [kpa])
                    copy_op(eng, pb_, psum[:, bB, 128:256], [("ps", bB)], [kpb])
                    yield
                    P.op(T_, lambda e, pb_=pb_, Xcur=Xcur: e.matmul(psum[:, bB, 256:384], lhsT=pb_, rhs=Xcur, start=True, stop=True),
                         reads=[kpb, kX], writes=[("ps", bB)])
                    yield
                    P.op(V, lambda e, Xcur=Xcur, Xnxt=Xnxt: e.tensor_tensor(out=Xnxt, in0=psum[:, bB, 256:384], in1=Xcur, op=ALU.add),
                         reads=[("ps", bB), kX], writes=[kXn])
                    yield
                    Pprev, PTprev, kP, kPT = pa, pb_, kpa, kpb
                    Xcur, Xnxt, kX, kXn = Xnxt, Xcur, kXn, kX
                X, kXf = Xcur, kX
                dbg = h == 0 and ((d == 0 and t == 0) or (d == 1 and t == 1))
                pf = "u%d_" % d
                if dbg and (pf + "X") in tap_out:
                    tap(pf + "X", X, kXf)
                    tap(pf + "PT", PTt, K("PT"))
                    tap(pf + "MT", MT, K("MT"))
                    tap(pf + "kg", kg, K("kg"))
                    tap(pf + "qgT", qgT, K("qgT"))
                    tap(pf + "kdec", kdec, K("kdec"))
                P.op(T_, lambda e: e.matmul(psum[:, bB, 384:512], lhsT=kg, rhs=X, start=True, stop=True), reads=[K("kg"), kXf], writes=[("ps", bB)])
                yield
                P.op(S_, lambda e: e.activation(out=nwT, in_=psum[:, bB, 384:512], func=AF.Copy, scale=-1.0), reads=[("ps", bB)], writes=[K("nwT")])
                yield
                yield "B"
                Sin, Sout = Sbuf[2 * d + pp], Sbuf[2 * d + (1 - pp)]
                kSin, kSout = ("S", d, pp), ("S", d, 1 - pp)
                vnew = EG
                P.op(T_, lambda e: e.matmul(psum[:, bA, 0:128], lhsT=X, rhs=vtok[:, t, :], start=True, stop=False),
                     reads=[kXf, ("vtok", (t // 4) * 4)], writes=[("ps", bA)])
                P.op(T_, lambda e: e.matmul(psum[:, bA, 0:128], lhsT=nwT, rhs=Sin, start=False, stop=True), reads=[K("nwT"), kSin], writes=[("ps", bA)])
                yield
                P.op(S_, lambda e: e.activation(out=vnew, in_=psum[:, bA, 0:128], func=AF.Copy, scale=col(beta)), reads=[("ps", bA), ("beta", d)],
                     writes=[K("EG")])
                yield
                need_o = ctx_out or t >= 2
                if dbg and (pf + "vnew") in tap_out:
                    tap(pf + "vnew", vnew, K("EG"))
                if need_o:
                    P.op(T_, lambda e: e.matmul(psum[:, bA, 128:256], lhsT=qgT, rhs=Sin, start=True, stop=False), reads=[K("qgT"), kSin],
                         writes=[("ps", bA)])
                    P.op(T_, lambda e: e.matmul(psum[:, bA, 128:256], lhsT=PTt, rhs=vnew, start=False, stop=True), reads=[K("PT"), K("EG")],
                         writes=[("ps", bA)])
                P.op(T_, lambda e: e.matmul(psum[:, bA, 256:384], lhsT=kdec, rhs=vnew, start=True, stop=True), reads=[K("kdec"), K("EG")],
                     writes=[("ps", bA)])
                yield
                if need_o:
                    if t not in oacc_written:
                        oacc_written.add(t)
                        P.op(V, lambda e: e.tensor_copy(out=oacc[:, t, :], in_=psum[:, bA, 128:256]), reads=[("ps", bA)], writes=[("oacc", t)])
                    else:
                        P.op(V, lambda e: e.tensor_tensor(out=oacc[:, t, :], in0=psum[:, bA, 128:256], in1=oacc[:, t, :], op=ALU.add),
                             reads=[("ps", bA), ("oacc", t)], writes=[("oacc", t)])
                P.op(V, lambda e: e.scalar_tensor_tensor(out=Sout, in0=Sin, scalar=col(egl), in1=psum[:, bA, 256:384], op0=ALU.mult, op1=ALU.add),
                     reads=[("ps", bA), kSin, "egl"], writes=[kSout])
                if h == 0 and d == 0 and t in (0, 1) and ("S%d" % t) in tap_out:
                    tap("S%d" % t, Sout, kSout)
                if h == 0 and d == 0 and t == 1 and "vnew1" in tap_out:
                    tap("vnew1", vnew, K("EG"))
                yield

            def mixed(b_gens, a_gens):
                act = [(g_, "B") for g_ in b_gens] + [(g_, "A") for g_ in a_gens]
                while act:
                    nxt = []
                    for g_, m_ in act:
                        try:
                            r_ = next(g_)
                        except StopIteration:
                            continue
                        if m_ == "A" and r_ == "B":
                            continue
                        nxt.append((g_, m_))
                    act = nxt

            dirs = [(0, tiles_f), (1, tiles_b)]
            if "dn_only_fwd" in taps:
                dirs = dirs[:1]
            if "dn_only_bwd" in taps:
                dirs = dirs[1:]
            def seq_pairs(pairs):
                for pair in pairs:
                    act_ = list(pair)
                    while act_:
                        nxt_ = []
                        for g_ in act_:
                            try:
                                next(g_)
                                nxt_.append(g_)
                            except StopIteration:
                                pass
                        act_ = nxt_
                        yield

            groups = []
            for gi in range(NT):
                groups.append([unit(0, 4 * ((gi // 2) % 2) + 2 * (gi % 2) + d, d, tl[gi], gi == 0, gi % 2) for d, tl in dirs])
            batches = [groups[i:i + 2] for i in range(0, NT, 2)]
            prev = []
            for bt in batches:
                mixed([seq_pairs(prev)] if prev else [], [g_ for grp in bt for g_ in grp])
                prev = bt
            mixed([seq_pairs(prev)], [])
            P.barrier()
            if h == 0 and "dn_oacc" in tap_out:
                tap("dn_oacc", oacc[:].rearrange("p t n -> p (t n)"), ("oacc", 0))
                P.barrier()
            t_start = 0 if ctx_out else 2
            for t in range(t_start, NT):
                b2 = t % 2
                junk = sq[:, 0:128]
                yb = rn[:, 64 * b2:64 * b2 + 64].bitcast(BF16)
                P.op(S_, lambda e, t=t, b2=b2: e.activation(out=junk, in_=oacc[:, t, :], func=AF.Square, accum_out=stat[:, 2 * b2:2 * b2 + 1]),
                     writes=["junk", ("stat", b2)])
                P.op(S_, lambda e, b2=b2: e.activation(out=stat[:, 2 * b2 + 1:2 * b2 + 2], in_=stat[:, 2 * b2:2 * b2 + 1], func=AF.Ln, scale=1.0 / 128,
                                                       bias=eps_c[:]), reads=[("stat", b2)], writes=[("stat2", b2)])
                P.op(S_, lambda e, b2=b2: e.activation(out=stat[:, 2 * b2 + 1:2 * b2 + 2], in_=stat[:, 2 * b2 + 1:2 * b2 + 2], func=AF.Exp, scale=-0.5),
                     reads=[("stat2", b2)], writes=[("stat2", b2)])
                P.op(V, lambda e, t=t, b2=b2, yb=yb: e.scalar_tensor_tensor(out=yb, in0=oacc[:, t, :], scalar=stat[:, 2 * b2 + 1:2 * b2 + 2],
                                                                            in1=zs[:, t, :], op0=ALU.mult, op1=ALU.mult),
                     reads=[("stat2", b2)], writes=[("yb", b2)])
                bk = next_bank()
                pt = psum[:, bk, 0:64].bitcast(BF16)
                P.op(T_, lambda e, yb=yb, pt=pt: e.transpose(pt, yb, ident_b[:]), reads=[("yb", b2)], writes=[("ps", bk)])
                copy_op(evac_eng(), ymix[:, 0, h, t * 128:(t + 1) * 128], pt, [("ps", bk)], [("ymix", 0, h, t)])
        P.barrier()


    def phase_hg(l, ctx_out):
        W0 = 10240
        wst_hg = A[:, 0:5120]
        wb = A[:, 5120:7680].bitcast(BF16)
        o = W0
        qT = A[:, o:o + NTOK]; o += NTOK
        vtok = A[:, o:o + 1152].bitcast(BF16).rearrange("p (t n) -> p t n", t=NT); o += 1152
        oT = A[:, o:o + NTOK]; o += NTOK
        lbb = A[:, o:o + 128]; o += 128
        omlb = A[:, o:o + 128]; o += 128
        gcol = A[:, o:o + 1]; o += 4
        lbT = A[:, o:o + 4]; o += 4
        lbl = A[0:4, o:o + 512]; o += 512
        Sb = [A[:, o + 128 * i:o + 128 * (i + 1)] for i in range(4)]; o += 512
        sq = A[:, o:o + 512]; o += 512
        rn = A[:, o:o + 512]; o += 512
        NS = 12
        slots0 = o
        o += 4 * NS * 128
        hTs = A[:, o:o + 8192].bitcast(BF16).rearrange("p (k n) -> p k n", k=KC)
        o += 8192
        assert o <= YMIX0, (o, YMIX0)
        for k in range(KC):
            P.op((V, G, S_)[k % 3] if False else (V, G)[k % 2], lambda e, k=k: e.tensor_copy(
                out=hTs[:, k, :].rearrange("p (c r) -> p c r", r=32),
                in_=hT[:, k, NCTX:NTOK].rearrange("p (r c) -> p c r", c=64)), writes=[("hTs", k)])

        def hT_s(k, st):
            if st < 2:
                return hT[:, k, st * 128:(st + 1) * 128]
            return hTs[:, k, (st - 2) * 128:(st - 1) * 128]

        def sl(us, i):
            if us >= 4:
                b = ((us - 4) * NS * 128 if us < 7 else 7680) + i * 128
                return A[:, b:b + 128]
            b = slots0 + (us * NS + i) * 128
            return A[:, b:b + 128]

        UbF = A[:, o - 128:o] if False else None
        P.op(SPQ, lambda e: e.dma_start(out=lbl, in_=hg_lb_logits[:, :]), writes=["lbl"], dma=True)
        bk = next_bank()
        for hc in range(4):
            P.op(T_, lambda e, hc=hc: e.transpose(psum[:, bk, 4 * hc:4 * hc + 4], lbl[:, hc * 128:(hc + 1) * 128], ident_f[0:4, 0:4]),
                 reads=["lbl", "ident_f"], writes=[("ps", bk)])
        lgt = sq[:, 0:16].rearrange("p (h l) -> p h l", l=4)
        P.op(S_, lambda e: e.activation(out=sq[:, 0:16], in_=psum[:, bk, 0:16], func=AF.Exp), reads=[("ps", bk)], writes=["lgt"])
        P.op(V, lambda e: e.tensor_reduce(out=sq[:, 16:20], in_=lgt, axis=AX.X, op=ALU.add), reads=["lgt"], writes=["lsum"])
        P.op(V, lambda e: e.reciprocal(out=sq[:, 16:20], in_=sq[:, 16:20]), reads=["lsum"], writes=["lsum"])
        if l == 0:
            P.op(V, lambda e: e.memset(lbT, 0.0), writes=["lbT"])
        else:
            P.op(V, lambda e: e.tensor_reduce(out=sq[:, 20:24], in_=lgt[:, :, 1:l + 1], axis=AX.X, op=ALU.add), reads=["lgt"], writes=["lpart"])
            P.op(V, lambda e: e.tensor_tensor(out=lbT, in0=sq[:, 20:24], in1=sq[:, 16:20], op=ALU.mult), reads=["lpart", "lsum"], writes=["lbT"])
        P.op(SPQ, lambda e: e.dma_start(out=gcol, in_=hg_norm_g[l:l + 1, :].rearrange("o p -> p o"), allow_slow_non_contiguous=True),
             writes=["gcol"], dma=True)
        UB = [sb_const("UBf"), sb_const("UBb"), sb_const("DBf"), sb_const("DBb")]
        tiles_f = list(range(NT))
        tiles_b = [1, 0] + list(range(NT - 1, 1, -1))

        for h in range(4):
            P.barrier()
            P.op(V, lambda e: e.tensor_scalar(out=sq[:, 0:128], in0=ones_f[:], scalar1=lbT[:, h:h + 1], scalar2=None, op0=ALU.mult),
                 reads=["lbT"], writes=["lbc"])
            bk = next_bank()
            P.op(T_, lambda e: e.transpose(psum[:, bk, 0:128], sq[:, 0:128], ident_f[:]), reads=["lbc"], writes=[("ps", bk)])
            P.op(V, lambda e: e.tensor_copy(out=lbb, in_=psum[:, bk, 0:128]), reads=[("ps", bk)], writes=["lbb"])
            P.op(V, lambda e: e.tensor_scalar(out=omlb, in0=lbb, scalar1=-1.0, scalar2=1.0, op0=ALU.mult, op1=ALU.add), reads=["lbb"], writes=["omlb"])
            cols = [OFF_HGQ + 128 * h, OFF_HGF + 128 * h, OFF_HGF + 512 + 128 * h, OFF_HGI + 128 * h, OFF_HGG + 128 * h]
            for j, c0 in enumerate(cols):
                P.op(SPQ, lambda e, j=j, c0=c0: e.dma_start(out=wst_hg[:, j * 1024:(j + 1) * 1024].rearrange("p (k n) -> p k n", k=KC),
                                                             in_=w_in[l, :, c0:c0 + 128].rearrange("(k p) n -> p k n", p=128)),
                     writes=[("wsth", j)], dma=True)
                P.op((G, V)[j % 2], lambda e, j=j: e.tensor_copy(out=wb[:, j * 1024:(j + 1) * 1024], in_=wst_hg[:, j * 1024:(j + 1) * 1024]),
                     reads=[("wsth", j)], writes=[("wbh", j)])
            wq = lambda k: wb[:, 0 * 1024 + k * 128:0 * 1024 + (k + 1) * 128]
            wf = lambda d, k: wb[:, (1 + d) * 1024 + k * 128:(1 + d) * 1024 + (k + 1) * 128]
            wi = lambda k: wb[:, 3 * 1024 + k * 128:3 * 1024 + (k + 1) * 128]
            wg = lambda k: wb[:, 4 * 1024 + k * 128:4 * 1024 + (k + 1) * 128]
            for st in range(NT):
                bk = next_bank()
                for k in range(KC):
                    P.op(T_, lambda e, st=st, k=k, bk=bk: e.matmul(psum[:, bk, 0:128], lhsT=hT_s(k, st), rhs=wi(k), start=(k == 0), stop=(k == KC - 1)),
                         reads=[("wbh", 3)], writes=[("ps", bk)])
                for k in range(KC):
                    P.op(T_, lambda e, st=st, k=k, bk=bk: e.matmul(psum[:, bk, 128:256], lhsT=wq(k), rhs=hT_s(k, st), start=(k == 0), stop=(k == KC - 1)),
                         reads=[("wbh", 0)], writes=[("ps", bk)])
                P.op(V, lambda e, st=st, bk=bk: e.tensor_copy(out=vtok[:, st, :], in_=psum[:, bk, 0:128]), reads=[("ps", bk)], writes=[("vtok", st)])
                P.op(S_, lambda e, st=st, bk=bk: e.activation(out=qT[:, st * 128:(st + 1) * 128], in_=psum[:, bk, 128:256], func=AF.Silu),
                     reads=[("ps", bk)], writes=[("qT", st)])
            for d in range(2):
                P.op(G, lambda e, d=d: e.memset(Sb[2 * d][:], 0.0), writes=[("S", d, 0)])
            oT_written = set()

            P.barrier()

            def unit(us, d, st, gi):
                bA = bB = us
                K = lambda n: ("u", us, n)
                F, KT, GC, EK, EQ, QG, EGq, ATT, EXD = [sl(us, i) for i in range(9)]
                KTt = sl(us, 9).bitcast(BF16)[:, 0:128]
                QTt = sl(us, 9).bitcast(BF16)[:, 128:256]
                KDEC = sl(us, 10).bitcast(BF16)[:, 0:128]
                ATTb = sl(us, 10).bitcast(BF16)[:, 128:256]
                NEGM = sl(us, 11)[:, 0:2]
                cs = slice(st * 128, (st + 1) * 128)
                for k in range(KC):
                    P.op(T_, lambda e, k=k: e.matmul(psum[:, bA, 0:128], lhsT=hT_s(k, st), rhs=wf(d, k), start=(k == 0), stop=(k == KC - 1)),
                         reads=[("wbh", 1 + d)], writes=[("ps", bA)])
                yield
                P.op(S_, lambda e: e.activation(out=F, in_=psum[:, bA, 0:128], func=AF.Sigmoid), reads=[("ps", bA)], writes=[K("F")])
                yield
                P.op(V, lambda e: e.tensor_tensor(out=F, in0=F, in1=omlb, op=ALU.mult), reads=[K("F"), "omlb"], writes=[K("F")])
                P.op(V, lambda e: e.tensor_tensor(out=F, in0=F, in1=lbb, op=ALU.add), reads=[K("F"), "lbb"], writes=[K("F")])
                yield
                P.op(G, lambda e: e.tensor_scalar(out=F, in0=F, scalar1=1e-30, scalar2=None, op0=ALU.max), reads=[K("F")], writes=[K("F")])
                P.op(G, lambda e: e.tensor_scalar(out=KT, in0=F, scalar1=-1.0, scalar2=1.0, op0=ALU.mult, op1=ALU.add), reads=[K("F")], writes=[K("KT")])
                yield
                P.op(S_, lambda e: e.activation(out=F, in_=F, func=AF.Ln), reads=[K("F")], writes=[K("F")])
                yield
                P.op(T_, lambda e: e.matmul(psum[:, bA, 128:256], lhsT=F, rhs=UB[d][:], start=True, stop=True), reads=[K("F")], writes=[("ps", bA)])
                P.op(T_, lambda e: e.matmul(psum[:, bA, 256:384], lhsT=UB[2 + d][:], rhs=F, start=True, stop=True), reads=[K("F")], writes=[("ps", bA)])
                P.op(T_, lambda e: e.transpose(psum[:, bA, 384:512], KT, ident_f[:]), reads=[K("KT")], writes=[("ps", bA)])
                yield
                P.op(S_, lambda e: e.activation(out=GC, in_=psum[:, bA, 128:256], func=AF.Copy), reads=[("ps", bA)], writes=[K("GC")])
                P.op(S_, lambda e: e.activation(out=EXD, in_=psum[:, bA, 256:384], func=AF.Exp), reads=[("ps", bA)], writes=[K("EXD")])
                yield
                mid = GC.rearrange("p (c n) -> p c n", n=64)[:, :, 32]
                P.op(G, lambda e: e.tensor_scalar(out=NEGM, in0=mid, scalar1=-1.0, scalar2=None, op0=ALU.mult), reads=[K("GC")], writes=[K("NEGM")])
                P.op(S_, lambda e: e.activation(out=EGq, in_=GC, func=AF.Exp), reads=[K("GC")], writes=[K("EGq")])
                yield
                for ch in range(2):
                    c64 = slice(ch * 64, (ch + 1) * 64)
                    P.op(S_, lambda e, ch=ch, c64=c64: e.activation(out=EK[:, c64], in_=GC[:, c64], func=AF.Exp, scale=-1.0, bias=GC[:, ch * 64 + 32:ch * 64 + 33]),
                         reads=[K("GC")], writes=[K("EK")])
                    P.op(S_, lambda e, ch=ch, c64=c64: e.activation(out=EQ[:, c64], in_=GC[:, c64], func=AF.Exp, bias=NEGM[:, ch:ch + 1]),
                         reads=[K("GC"), K("NEGM")], writes=[K("EQ")])
                yield
                P.op(V, lambda e: e.tensor_tensor(out=KTt, in0=psum[:, bA, 384:512], in1=EK, op=ALU.mult), reads=[("ps", bA), K("EK")], writes=[K("KTt")])
                P.op(G, lambda e: e.tensor_tensor(out=QTt, in0=qT[:, cs], in1=EQ, op=ALU.mult), reads=[K("EQ"), ("qT", st)], writes=[K("QTt")])
                P.op(G, lambda e: e.tensor_tensor(out=QG, in0=qT[:, cs], in1=EGq, op=ALU.mult), reads=[K("EGq"), ("qT", st)], writes=[K("QG")])
                P.op(V, lambda e: e.tensor_tensor(out=KDEC, in0=KT, in1=EXD, op=ALU.mult), reads=[K("KT"), K("EXD")], writes=[K("KDEC")])
                yield
                P.op(T_, lambda e: e.matmul(psum[:, bB, 0:128], lhsT=KTt, rhs=QTt, start=True, stop=True), reads=[K("KTt"), K("QTt")], writes=[("ps", bB)])
                yield
                P.op(V, lambda e: e.tensor_copy(out=ATT, in_=psum[:, bB, 0:128]), reads=[("ps", bB)], writes=[K("ATT")])
                yield
                for hf in range(2):
                    c64 = slice(64 * hf, 64 * hf + 64)
                    if d == 0:
                        P.op(G, lambda e, c64=c64, hf=hf: e.affine_select(out=ATTb[:, c64], in_=ATT[:, c64], pattern=[[1, 64]], compare_op=ALU.is_ge,
                                                                          fill=0.0, base=64 * hf, channel_multiplier=-1),
                             reads=[K("ATT")], writes=[K("ATTb%d" % hf)])
                    else:
                        P.op(G, lambda e, c64=c64, hf=hf: e.affine_select(out=ATTb[:, c64], in_=ATT[:, c64], pattern=[[-1, 64]], compare_op=ALU.is_ge,
                                                                          fill=0.0, base=-64 * hf, channel_multiplier=1),
                             reads=[K("ATT")], writes=[K("ATTb%d" % hf)])
                yield "B"
                for ci, ch in enumerate((0, 1) if d == 0 else (1, 0)):
                    pp = (2 * gi + ci) % 2
                    Sin, Sout = Sb[2 * d + pp], Sb[2 * d + 1 - pp]
                    kSin, kSout = ("S", d, pp), ("S", d, 1 - pp)
                    r64 = slice(ch * 64, (ch + 1) * 64)
                    need_o = ctx_out or st >= 2
                    pso = psum[:, bB, 128 + 64 * ch:128 + 64 * (ch + 1)]
                    if need_o:
                        P.op(T_, lambda e, r64=r64, Sin=Sin, pso=pso: e.matmul(pso, lhsT=Sin, rhs=QG[:, r64], start=True, stop=False),
                             reads=[kSin, K("QG")], writes=[("ps", bB)])
                        P.op(T_, lambda e, r64=r64, pso=pso: e.matmul(pso, lhsT=vtok[r64, st, :], rhs=ATTb[r64, r64], start=False, stop=True),
                             reads=[("vtok", st), K("ATTb0"), K("ATTb1")], writes=[("ps", bB)])
                    psS = psum[:, bB, 256 + 128 * ci:256 + 128 * (ci + 1)]
                    P.op(T_, lambda e, r64=r64, psS=psS: e.matmul(psS, lhsT=KDEC[r64, :], rhs=vtok[r64, st, :], start=True, stop=True),
                         reads=[K("KDEC"), ("vtok", st)], writes=[("ps", bB)])
                    yield
                    oc = slice(st * 128 + ch * 64, st * 128 + (ch + 1) * 64)
                    if need_o:
                        key = (st, ch)
                        if key not in oT_written:
                            oT_written.add(key)
                            P.op(V, lambda e, oc=oc, pso=pso: e.tensor_copy(out=oT[:, oc], in_=pso), reads=[("ps", bB)], writes=[("oT", st, ch)])
                        else:
                            P.op(V, lambda e, oc=oc, pso=pso: e.tensor_tensor(out=oT[:, oc], in0=pso, in1=oT[:, oc], op=ALU.add),
                                 reads=[("ps", bB), ("oT", st, ch)], writes=[("oT", st, ch)])
                    gl_col = ch * 64 + (63 if d == 0 else 0)
                    P.op(V, lambda e, Sin=Sin, Sout=Sout, psS=psS, gl_col=gl_col: e.scalar_tensor_tensor(
                        out=Sout, in0=Sin, scalar=EGq[:, gl_col:gl_col + 1], in1=psS, op0=ALU.mult, op1=ALU.add),
                        reads=[("ps", bB), kSin, K("EGq")], writes=[kSout])
                    yield

            def mixed(b_gens, a_gens):
                act = [(g_, "B") for g_ in b_gens] + [(g_, "A") for g_ in a_gens]
                while act:
                    nxt = []
                    for g_, m_ in act:
                        try:
                            r_ = next(g_)
                        except StopIteration:
                            continue
                        if m_ == "A" and r_ == "B":
                            continue
                        nxt.append((g_, m_))
                    act = nxt

            def seq_pairs(pairs):
                for pair in pairs:
                    act_ = list(pair)
                    while act_:
                        nxt_ = []
                        for g_ in act_:
                            try:
                                next(g_)
                                nxt_.append(g_)
                            except StopIteration:
                                pass
                        act_ = nxt_
                        yield

            dirs = [(0, tiles_f), (1, tiles_b)]
            groups = [[unit(4 * ((gi // 2) % 2) + 2 * (gi % 2) + d, d, tl[gi], gi) for d, tl in dirs] for gi in range(NT)]
            batches = [groups[i:i + 2] for i in range(0, NT, 2)]
            prev = []
            for bt in batches:
                mixed([seq_pairs(prev)] if prev else [], [g_ for grp in bt for g_ in grp])
                prev = bt
            mixed([seq_pairs(prev)], [])
            P.barrier()
            if h == 0 and "hg_oT" in tap_out:
                tap("hg_oT", oT[:, 0:NTOK], ("oT", 0, 0))
                P.barrier()
            blocks = TBLK if ctx_out else TBLK[1:]
            for (t0, tn) in blocks:
                bk = next_bank()
                P.op(S_, lambda e, t0=t0, tn=tn: e.activation(out=sq[:, 0:tn], in_=oT[:, t0:t0 + tn], func=AF.Square), writes=["sq"])
                P.op(T_, lambda e, bk=bk, tn=tn: e.matmul(psum[:, bk, 0:tn], lhsT=ones_f[:], rhs=sq[:, 0:tn], start=True, stop=True),
                     reads=["sq"], writes=[("ps", bk)])
                P.op(S_, lambda e, bk=bk, tn=tn: e.activation(out=rn[:, 0:tn], in_=psum[:, bk, 0:tn], func=AF.Ln, scale=1.0 / 128, bias=eps_c[:]),
                     reads=[("ps", bk)], writes=["rn"])
                P.op(S_, lambda e, tn=tn: e.activation(out=rn[:, 0:tn], in_=rn[:, 0:tn], func=AF.Exp, scale=-0.5), reads=["rn"], writes=["rn"])
                P.op(V, lambda e, t0=t0, tn=tn: e.scalar_tensor_tensor(out=oT[:, t0:t0 + tn], in0=oT[:, t0:t0 + tn], scalar=gcol[:, 0:1], in1=rn[:, 0:tn],
                                                                       op0=ALU.mult, op1=ALU.mult), reads=["rn", "gcol"], writes=[("oTn", t0)])
            for bi, (t0, tn) in enumerate(TBLK):
                if not ctx_out and bi == 0:
                    continue
                bk = next_bank()
                for k in range(KC):
                    P.op(T_, lambda e, k=k, bk=bk, t0=t0, tn=tn: e.matmul(psum[:, bk, 0:tn], lhsT=wg(k), rhs=hT[:, k, t0:t0 + tn],
                                                                         start=(k == 0), stop=(k == KC - 1)), reads=[("wbh", 4)], writes=[("ps", bk)])
                P.op(S_, lambda e, bk=bk, tn=tn: e.activation(out=sq[:, 0:tn], in_=psum[:, bk, 0:tn], func=AF.Silu), reads=[("ps", bk)], writes=["sq"])
                if bi == 0:
                    src = oT[:, 0:NCTX]
                    dst = ymix[:, 1, h, 0:NCTX]
                    s2 = sq[:, 0:tn]
                else:
                    b4 = bi - 1
                    src = oT[:, NCTX:NTOK].rearrange("p (c r) -> p r c", r=32)[:, 8 * b4:8 * b4 + 8, :]
                    dst = ymix[:, 1, h, t0:t0 + tn].rearrange("p (r c) -> p r c", c=64)
                    s2 = sq[:, 0:tn].rearrange("p (r c) -> p r c", c=64)
                P.op(V, lambda e, src=src, dst=dst, s2=s2: e.tensor_tensor(out=dst, in0=src, in1=s2, op=ALU.mult),
                     reads=["sq"] + [("oTn", x[0]) for x in TBLK], writes=[("ymix", 1, h, bi)])
        P.barrier()


    def phase_merge(l, ctx_out):
        mergedT = A[:, 10240:19456].bitcast(BF16).rearrange("p (k n) -> p k n", k=KC)
        wbr = A[:, 19456:23552].bitcast(BF16).rearrange("p (b h n) -> p b h n", b=2, h=4)
        wout_b = A[:, 23552:27648].bitcast(BF16).rearrange("p (k n) -> p k n", k=KC)
        wg_b = A[:, 27648:28672].bitcast(BF16)
        sg = [A[:, 28672:29184], A[:, 29184:29696]]
        gateb = A[:, 29696:31744].rearrange("p (v n) -> p v n", v=2)
        xt = [A[:, 31744:32768], A[:, 8192:9216]]
        tmp = A[:, 9216:10240]
        assert 32768 <= YMIX0
        for b, src in enumerate((w_br_dn, w_br_hg)):
            P.op(SPQ, lambda e, b=b, src=src: e.dma_start(out=wst[:, b, :].rearrange("p (h n) -> p h n", h=4),
                                                         in_=src[l].rearrange("(h p) n -> p h n", p=128)), writes=[("wst", b)], dma=True)
            P.op((G, V)[b], lambda e, b=b: e.tensor_copy(out=wbr[:, b, :, :], in_=wst[:, b, :].rearrange("p (h n) -> p h n", h=4)),
                 reads=[("wst", b)], writes=[("wbr", b)])
        for v in range(2):
            P.op(SPQ, lambda e, v=v: e.dma_start(out=gateb[:, v, :], in_=modrow_d[v:v + 1, 2 * D:3 * D].partition_broadcast(128)),
                 writes=[("gateb", v)], dma=True)
        blocks = list(enumerate(TBLK)) if ctx_out else list(enumerate(TBLK))[1:]
        for dc in range(KC):
            slot = dc % 2
            for gi_, c0 in enumerate((OFF_GDN + dc * 128, OFF_GHG + dc * 128)):
                P.op(SPQ, lambda e, gi_=gi_, c0=c0, slot=slot: e.dma_start(
                    out=wst[:, slot, gi_ * 1024:(gi_ + 1) * 1024].rearrange("p (k n) -> p k n", k=KC),
                    in_=w_in[l, :, c0:c0 + 128].rearrange("(k p) n -> p k n", p=128)), writes=[("wst", slot)], dma=True)
            P.op(G, lambda e, slot=slot: e.tensor_copy(out=wg_b, in_=wst[:, slot, 0:2048]), reads=[("wst", slot)], writes=["wg_b"])
            for bi, (t0, tn) in blocks:
                bks = [next_bank() for _ in range(4)]
                for br in range(2):
                    for hc in range(4):
                        P.op(T_, lambda e, br=br, hc=hc, t0=t0, tn=tn, bks=bks: e.matmul(
                            psum[:, bks[br], 0:tn], lhsT=wbr[:, br, hc, dc * 128:(dc + 1) * 128], rhs=ymix[:, br, hc, t0:t0 + tn],
                            start=(hc == 0), stop=(hc == 3)), reads=[("wbr", br)], writes=[("ps", bks[br])])
                    for k in range(KC):
                        P.op(T_, lambda e, br=br, k=k, t0=t0, tn=tn, bks=bks: e.matmul(
                            psum[:, bks[2 + br], 0:tn], lhsT=wg_b[:, br * 1024 + k * 128:br * 1024 + (k + 1) * 128], rhs=hT[:, k, t0:t0 + tn],
                            start=(k == 0), stop=(k == KC - 1)), reads=["wg_b"], writes=[("ps", bks[2 + br])])
                for br in range(2):
                    P.op(S_, lambda e, br=br, tn=tn, bks=bks: e.activation(out=sg[br][:, 0:tn], in_=psum[:, bks[2 + br], 0:tn], func=AF.Sigmoid),
                         reads=[("ps", bks[2 + br])], writes=[("sg", br)])
                    P.op(V, lambda e, br=br, tn=tn, bks=bks: e.tensor_tensor(out=sg[br][:, 0:tn], in0=psum[:, bks[br], 0:tn], in1=sg[br][:, 0:tn], op=ALU.mult),
                         reads=[("ps", bks[br]), ("sg", br)], writes=[("sg", br)])
                P.op(G, lambda e, t0=t0, tn=tn: e.tensor_tensor(out=mergedT[:, dc, t0:t0 + tn], in0=sg[0][:, 0:tn], in1=sg[1][:, 0:tn], op=ALU.add),
                     reads=[("sg", 0), ("sg", 1)], writes=[("mergedT", dc, bi)])
        for half in range(2):
            P.op(SPQ, lambda e, half=half: e.dma_start(out=wst[:, half, :].rearrange("p (k n) -> p k n", k=4),
                                                       in_=w_out[l, half * 512:(half + 1) * 512, :].rearrange("(k p) n -> p k n", p=128)),
                 writes=[("wst", half)], dma=True)
            P.op((G, V)[half], lambda e, half=half: e.tensor_copy(out=wout_b[:, 4 * half:4 * half + 4, :],
                                                                 in_=wst[:, half, :].rearrange("p (k n) -> p k n", k=4)),
                 reads=[("wst", half)], writes=[("wout_b", half)])
        tiles = list(range(NT)) if ctx_out else list(range(2, NT))
        for i, t in enumerate(tiles):
            b = i % 2
            v = 1 if t < 2 else 0
            src = x_tile_src(l, True, t)
            P.op(SPQ, lambda e, b=b, src=src: e.dma_start(out=xt[b], in_=src), writes=[("xt", b)], dma=True)
            for half in range(2):
                bk = next_bank()
                for dc in range(KC):
                    P.op(T_, lambda e, dc=dc, t=t, half=half, bk=bk: e.matmul(
                        psum[:, bk, :], lhsT=mergedT[:, dc, t * 128:(t + 1) * 128], rhs=wout_b[:, dc, half * 512:(half + 1) * 512],
                        start=(dc == 0), stop=(dc == KC - 1)),
                        reads=[("wout_b", 0), ("wout_b", 1)] + [("mergedT", dc, bi) for bi in range(5)], writes=[("ps", bk)])
                hs = slice(half * 512, (half + 1) * 512)
                P.op(V, lambda e, bk=bk, v=v, hs=hs: e.tensor_tensor(out=tmp[:, hs], in0=psum[:, bk, :], in1=gateb[:, v, hs], op=ALU.mult),
                     reads=[("ps", bk), ("gateb", v)], writes=[("tmp", half)])
                P.op(G, lambda e, b=b, hs=hs: e.tensor_tensor(out=xt[b][:, hs], in0=xt[b][:, hs], in1=tmp[:, hs], op=ALU.add),
                     reads=[("tmp", half), ("xt", b)], writes=[("xt", b)])
            P.op(SPQ, lambda e, b=b, t=t: e.dma_start(out=Xd[t * 128:(t + 1) * 128, :], in_=xt[b]), reads=[("xt", b)], writes=[("Xd", t)], dma=True)
        if not ctx_out:
            pass
        P.barrier()
        for nm_ in ("x1", "xmid%d" % l):
            if nm_ in tap_out:
                P.op(SPQ, lambda e, nm_=nm_: e.dma_start(out=tap_out[nm_], in_=Xd[:, :]), writes=["tap_" + nm_], dma=True)
                P.barrier()

    def phase_moe(l, last):
        o = 8192
        wgb = A[:, o:o + 2048].bitcast(BF16).rearrange("p (k n) -> p k n", k=KC); o += 2048
        wub = A[:, o:o + 2048].bitcast(BF16).rearrange("p (k n) -> p k n", k=KC); o += 2048
        wdb = A[:, o:o + 2048].bitcast(BF16).rearrange("p (k n) -> p k n", k=4); o += 2048
        acc = A[:, o:o + 18432].rearrange("p (t n) -> p t n", t=NT); o += 18432
        actT = A[:, o:o + 4608].bitcast(BF16).rearrange("p (f n) -> p f n", f=4); o += 4608
        gw = A[:, o:o + 576].rearrange("p (t n) -> p t n", t=NT); o += 576
        gsel = A[:, o:o + 72].rearrange("p (t n) -> p t n", t=NT); o += 72
        pen = A[:, o:o + 72].rearrange("p (t n) -> p t n", t=NT); o += 72
        oh1 = A[:, o:o + 576].rearrange("p (t n) -> p t n", t=NT); o += 576
        oh2 = A[:, o:o + 576].rearrange("p (t n) -> p t n", t=NT); o += 576
        sm = A[:, o:o + 18 * 8].rearrange("p (j t) -> p j t", j=8); o += 144
        brow = A[:, o:o + 36]; o += 36
        wr_f = A[:, o:o + 288]; o += 288
        wr_b = A[:, o:o + 144].bitcast(BF16); o += 144
        sgs = [A[:, o:o + 512], A[:, o + 512:o + 1024]]; o += 1024
        assert o <= ARENA, (o, ARENA)
        tiles = list(range(NT)) if not last else list(range(2, NT))
        blocks = TBLK if not last else TBLK[1:]
        T0 = tiles[0]
        NTl = len(tiles)
        ts = slice(T0, NT)
        gmax, gsum, m1, m2, p2, g1, g2 = [sm[:, j, ts] for j in range(7)]
        bc = lambda a, n: a.unsqueeze(2).to_broadcast([128, NTl, n])
        ops = []
        Vop = lambda f, **kw: P.op(V, f, reads=["moe_chain"], writes=["moe_chain"])
        Sop = lambda f, **kw: P.op(S_, f, reads=["moe_chain"], writes=["moe_chain"])
        lG, lE = lgG[:, ts, :], lgE[:, ts, :]
        Vop(lambda e: e.tensor_reduce(out=gmax, in_=lG, axis=AX.X, op=ALU.max))
        Vop(lambda e: e.tensor_tensor(out=gsel[:, ts, :], in0=lG, in1=bc(gmax, 4), op=ALU.is_equal))
        Vop(lambda e: e.tensor_tensor(out=pen[:, ts, :], in0=lG, in1=bc(gmax, 4), op=ALU.subtract))
        Sop(lambda e: e.activation(out=pen[:, ts, :], in_=pen[:, ts, :], func=AF.Exp))
        Vop(lambda e: e.tensor_reduce(out=gsum, in_=pen[:, ts, :], axis=AX.X, op=ALU.add))
        Vop(lambda e: e.reciprocal(out=gsum, in_=gsum))
        Vop(lambda e: e.tensor_scalar(out=pen[:, ts, :], in0=gsel[:, ts, :], scalar1=-1.0, scalar2=1e30, op0=ALU.add, op1=ALU.mult))
        lE4 = lgE[:, ts, :].rearrange("p t (g j) -> p t g j", g=4)
        Vop(lambda e: e.tensor_tensor(out=lE4, in0=lE4, in1=gsel[:, ts, :].unsqueeze(3).to_broadcast([128, NTl, 4, 8]), op=ALU.mult))
        Vop(lambda e: e.tensor_tensor(out=lE4, in0=lE4, in1=pen[:, ts, :].unsqueeze(3).to_broadcast([128, NTl, 4, 8]), op=ALU.add))
        Vop(lambda e: e.tensor_reduce(out=m1, in_=lE, axis=AX.X, op=ALU.max))
        Vop(lambda e: e.tensor_tensor(out=oh1[:, ts, :], in0=lE, in1=bc(m1, 32), op=ALU.is_equal))
        Vop(lambda e: e.scalar_tensor_tensor(out=lE, in0=oh1[:, ts, :], scalar=-1e30, in1=lE, op0=ALU.mult, op1=ALU.add))
        Vop(lambda e: e.tensor_reduce(out=m2, in_=lE, axis=AX.X, op=ALU.max))
        Vop(lambda e: e.tensor_tensor(out=oh2[:, ts, :], in0=lE, in1=bc(m2, 32), op=ALU.is_equal))
        Vop(lambda e: e.tensor_tensor(out=p2, in0=m2, in1=m1, op=ALU.subtract))
        Sop(lambda e: e.activation(out=p2, in_=p2, func=AF.Exp))
        Vop(lambda e: e.tensor_scalar(out=g1, in0=p2, scalar1=1.0, scalar2=None, op0=ALU.add))
        Vop(lambda e: e.reciprocal(out=g1, in_=g1))
        Vop(lambda e: e.tensor_tensor(out=g1, in0=g1, in1=gsum, op=ALU.mult))
        Vop(lambda e: e.tensor_tensor(out=g2, in0=g1, in1=p2, op=ALU.mult))
        Vop(lambda e: e.tensor_tensor(out=gw[:, ts, :], in0=oh1[:, ts, :], in1=bc(g1, 32), op=ALU.mult))
        Vop(lambda e: e.tensor_tensor(out=oh2[:, ts, :], in0=oh2[:, ts, :], in1=bc(g2, 32), op=ALU.mult))
        Vop(lambda e: e.tensor_tensor(out=gw[:, ts, :], in0=gw[:, ts, :], in1=oh2[:, ts, :], op=ALU.add))
        P.barrier()
        if "gw" in tap_out:
            tap("gw", gw[:].rearrange("p t n -> p (t n)"), "moe_chain")
            P.barrier()
        ne_run = min(NE, ne_decl) if "moe_ne" not in taps else taps["moe_ne_n"]
        for ex in range(ne_run):
            srcs = (w_eg[l, ex].rearrange("(k p) n -> p k n", p=128), w_eu[l, ex].rearrange("(k p) n -> p k n", p=128),
                    w_ed[l, ex].rearrange("(k p) n -> p k n", p=128))
            dsts = (wgb, wub, wdb)
            for j in range(3):
                slot = (3 * ex + j) % 2
                kk = KC if j < 2 else 4
                P.op(SPQ, lambda e, j=j, slot=slot, kk=kk: e.dma_start(out=wst[:, slot, :].rearrange("p (k n) -> p k n", k=kk), in_=srcs[j]),
                     writes=[("wst", slot)], dma=True)
                copy_op((G, S_, G)[j], dsts[j][:], wst[:, slot, :].rearrange("p (k n) -> p k n", k=kk), [("wst", slot)], [("wexp", j)])
            for bi, (t0, tn) in enumerate(blocks):
                for fc in range(4):
                    bg, bu = next_bank(), next_bank()
                    for k in range(KC):
                        P.op(T_, lambda e, k=k, fc=fc, t0=t0, tn=tn, bg=bg: e.matmul(psum[:, bg, 0:tn], lhsT=wgb[:, k, fc * 128:(fc + 1) * 128],
                                                                                   rhs=hT[:, k, t0:t0 + tn], start=(k == 0), stop=(k == KC - 1)),
                             reads=[("wexp", 0)], writes=[("ps", bg)])
                    for k in range(KC):
                        P.op(T_, lambda e, k=k, fc=fc, t0=t0, tn=tn, bu=bu: e.matmul(psum[:, bu, 0:tn], lhsT=wub[:, k, fc * 128:(fc + 1) * 128],
                                                                                   rhs=hT[:, k, t0:t0 + tn], start=(k == 0), stop=(k == KC - 1)),
                             reads=[("wexp", 1)], writes=[("ps", bu)])
                    sb_ = (bi * 4 + fc) % 2
                    P.op(S_, lambda e, tn=tn, bg=bg, sb_=sb_: e.activation(out=sgs[sb_][:, 0:tn], in_=psum[:, bg, 0:tn], func=AF.Silu),
                         reads=[("ps", bg)], writes=[("sgs", sb_)])
                    P.op(V, lambda e, fc=fc, t0=t0, tn=tn, bu=bu, sb_=sb_: e.tensor_tensor(out=actT[:, fc, t0:t0 + tn], in0=psum[:, bu, 0:tn],
                                                                                          in1=sgs[sb_][:, 0:tn], op=ALU.mult),
                         reads=[("ps", bu), ("sgs", sb_)], writes=[("actT", fc, bi)])
            for t in tiles:
                bi = 0 if t < 2 else (t - 2) // 4 + 1
                if last:
                    bi = (t - 2) // 4
                for half in range(2):
                    bk = next_bank()
                    for fc in range(4):
                        P.op(T_, lambda e, fc=fc, t=t, half=half, bk=bk: e.matmul(psum[:, bk, :], lhsT=actT[:, fc, t * 128:(t + 1) * 128],
                                                                                 rhs=wdb[:, fc, half * 512:(half + 1) * 512], start=(fc == 0), stop=(fc == 3)),
                             reads=[("wexp", 2)] + [("actT", fc, bi)], writes=[("ps", bk)])
                    hs = slice(half * 512, (half + 1) * 512)
                    if ex == 0:
                        P.op(V, lambda e, t=t, hs=hs, bk=bk: e.tensor_scalar(out=acc[:, t, hs], in0=psum[:, bk, :], scalar1=gw[:, t, ex:ex + 1], scalar2=None,
                                                                            op0=ALU.mult), reads=[("ps", bk)], writes=[("acc", t, half)])
                    else:
                        P.op(V, lambda e, t=t, hs=hs, bk=bk, ex=ex: e.scalar_tensor_tensor(out=acc[:, t, hs], in0=psum[:, bk, :], scalar=gw[:, t, ex:ex + 1],
                                                                                          in1=acc[:, t, hs], op0=ALU.mult, op1=ALU.add),
                             reads=[("ps", bk), ("acc", t, half)], writes=[("acc", t, half)])
        P.barrier()
        gate5 = A[:, 0:2048].rearrange("p (v n) -> p v n", v=2)
        xt = [A[:, 2048:3072], A[:, 3072:4096]]
        gfin = A[:, 4096:5120]
        junk = A[:, 5120:5632].bitcast(BF16)
        st = A[:, 5632:5696]
        for v in range(2):
            P.op(SPQ, lambda e, v=v: e.dma_start(out=gate5[:, v, :], in_=modrow_d[v:v + 1, 5 * D:6 * D].partition_broadcast(128)),
                 writes=[("gate5", v)], dma=True)
        if last:
            P.op(SPQ, lambda e: e.dma_start(out=gfin, in_=g_final[0:1, :].partition_broadcast(128)), writes=["gfin"], dma=True)
        for i, t in enumerate(tiles):
            b = i % 2
            v = 1 if t < 2 else 0
            P.op(SPQ, lambda e, b=b, t=t: e.dma_start(out=xt[b], in_=Xd[t * 128:(t + 1) * 128, :]), writes=[("xt", b)], dma=True)
            P.op(G, lambda e, t=t, v=v: e.tensor_tensor(out=acc[:, t, :], in0=acc[:, t, :], in1=gate5[:, v, :], op=ALU.mult),
                 reads=[("gate5", v)], writes=[("accg", t)])
            P.op(V, lambda e, b=b, t=t: e.tensor_tensor(out=xt[b], in0=xt[b], in1=acc[:, t, :], op=ALU.add), reads=[("xt", b), ("accg", t)],
                 writes=[("xt", b)])
            if not last:
                P.op(SPQ, lambda e, b=b, t=t: e.dma_start(out=Xd[t * 128:(t + 1) * 128, :], in_=xt[b]), reads=[("xt", b)], writes=[("Xd", t)], dma=True)
            else:
                P.op(S_, lambda e, b=b: e.activation(out=junk, in_=xt[b], func=AF.Square, accum_out=st[:, 2 * b:2 * b + 1]), reads=[("xt", b)],
                     writes=["junk", ("st", b)])
                P.op(S_, lambda e, b=b: e.activation(out=st[:, 2 * b + 1:2 * b + 2], in_=st[:, 2 * b:2 * b + 1], func=AF.Ln, scale=1.0 / D, bias=eps_c[:]),
                     reads=[("st", b)], writes=[("st2", b)])
                P.op(S_, lambda e, b=b: e.activation(out=st[:, 2 * b + 1:2 * b + 2], in_=st[:, 2 * b + 1:2 * b + 2], func=AF.Exp, scale=-0.5),
                     reads=[("st2", b)], writes=[("st2", b)])
                P.op(V, lambda e, b=b: e.scalar_tensor_tensor(out=xt[b], in0=xt[b], scalar=st[:, 2 * b + 1:2 * b + 2], in1=gfin, op0=ALU.mult, op1=ALU.mult),
                     reads=[("xt", b), ("st2", b), "gfin"], writes=[("xt", b)])
                P.op(SPQ, lambda e, b=b, t=t: e.dma_start(out=out[(t - 2) * 128:(t - 1) * 128, :], in_=xt[b]), reads=[("xt", b)], writes=[("out", t)],
                     dma=True)
        P.barrier()
        if ("xend%d" % l) in tap_out:
            P.op(SPQ, lambda e: e.dma_start(out=tap_out["xend%d" % l], in_=Xd[:, :]), writes=["tap_xend"], dma=True)
            P.barrier()

    for l in range(depth):
        phase_mod(l)
        P.barrier()
        if stop_after == "mod":
            break
        last = (l == depth - 1)
        phase_norm(l, 0, list(range(NT)))
        P.barrier()
        if stop_after == "norm0":
            break
        if "skip_dn" not in taps:
            phase_dn(l, not last)
        if stop_after == "dn":
            break
        phase_hg(l, not last)
        if stop_after == "hg":
            break
        phase_merge(l, not last)
        if stop_after == "merge":
            break
        phase_norm(l, 1, list(range(NT)) if not last else list(range(2, NT)))
        P.barrier()
        phase_moe(l, last)

    if "hT" in tap_out:
        for k in range(KC):
            stg = work[:, 0:NTOK]
            P.op(V, lambda e, k=k: e.tensor_copy(out=stg, in_=hT[:, k, :]), reads=[("hT", k, t) for t in range(NT)], writes=["stg"])
            P.op(SPQ, lambda e, k=k: e.dma_start(out=tap_out["hT"][k * 128:(k + 1) * 128, :], in_=stg), reads=["stg"],
                 writes=[("tap_hT", k)], dma=True)

    P.prepare(es)
    with es, nc.Block() as block:
        P.emit(block)
    return nc


_NC_CACHE = {}


def kernel(**inputs):
    f32 = lambda a: np.ascontiguousarray(np.asarray(a, dtype=np.float32))
    shared = {k: f32(v) for k, v in inputs.items() if k not in ("x", "c", "ctx", "c_ctx", "g_final", "dn_a_log", "dn_dt_bias")}
    shared["c_ctx"] = f32(inputs["c_ctx"]).reshape(1, D)
    shared["g_final"] = f32(inputs["g_final"]).reshape(1, D)
    shared["dn_a_log"] = f32(inputs["dn_a_log"]).reshape(DEPTH, 8)
    shared["dn_dt_bias"] = f32(inputs["dn_dt_bias"]).reshape(DEPTH, 8)
    x = f32(inputs["x"])
    c = f32(inputs["c"])
    ctx = f32(inputs["ctx"])
    nb = x.shape[0]
    if "nc" not in _NC_CACHE:
        _NC_CACHE["nc"] = build_program()
    nc = _NC_CACHE["nc"]
    in_maps = []
    for b in range(nb):
        m = dict(shared)
        m["x"] = np.ascontiguousarray(x[b])
        m["ctx"] = np.ascontiguousarray(ctx[b])
        m["c"] = np.ascontiguousarray(c[b:b + 1])
        in_maps.append(m)
    res = run_bass_kernel_spmd(nc, in_maps, core_ids=list(range(nb)))
    return np.stack([np.asarray(r["out"], dtype=np.float32) for r in res.results], axis=0)
```

```python
import numpy as np
from contextlib import ExitStack
import concourse.bass as bass
import concourse.mybir as mybir
from concourse.bass_utils import run_bass_kernel_spmd

F32 = mybir.dt.float32
BF16 = mybir.dt.bfloat16
AF = mybir.ActivationFunctionType
ALU = mybir.AluOpType
AX = mybir.AxisListType

D = 1024
KC = 8
NCTX = 256
NLAT = 2048
NTOK = NCTX + NLAT
NT = NTOK // 128
DEPTH = 4
N_IN = 6672
OFF_DNQ, OFF_DNK, OFF_DNV, OFF_DNZ, OFF_DNB, OFF_DNA = 0, 512, 1024, 1536, 2048, 2056
OFF_HGQ, OFF_HGF, OFF_HGI, OFF_HGG, OFF_GDN, OFF_GHG = 2064, 2576, 3600, 4112, 4624, 5648
NE = 32
DEXP = 512
EPS = 1e-6


class Op:
    __slots__ = ("eng", "fn", "deps", "idx", "signal", "dma", "dsem", "dval", "cnt")

    def __init__(self, eng, fn, dma):
        self.eng, self.fn, self.dma = eng, fn, dma
        self.deps = []
        self.signal = False
        self.dsem = self.dval = None
        self.cnt = None


class _Rec:
    def __init__(self):
        self.call = None

    def __getattr__(self, name):
        def f(*a, **kw):
            assert self.call is None
            self.call = (name, a, kw)
            return self
        return f


class Plan:
    ENGS = ("pe", "act", "dve", "pool", "sp")
    NDSEM = 24
    SEM_WRAP = 30000

    def __init__(self, nc):
        self.nc = nc
        self.ops = {e: [] for e in self.ENGS}
        self.writer = {}
        self.readers = {}
        self.dcum = [0] * self.NDSEM
        self.drr = 0
        self.pending = {e: [] for e in self.ENGS}

    def op(self, eng, fn, reads=(), writes=(), dma=False):
        if fn is not None:
            rec = _Rec()
            fn(rec)
            call = rec.call
            fn = lambda e, call=call: getattr(e, call[0])(*call[1], **call[2])
        o = Op(eng, fn, dma)
        psk = [k for k in reads if isinstance(k, tuple) and k[0] == "ps"]
        if psk:
            reads = [k for k in reads if k not in psk]
            writes = list(writes) + psk
        deps = list(self.pending[eng])
        keep = set(id(d_) for d_ in deps)
        self.pending[eng] = []
        for k in reads:
            deps.extend(self.writer.get(k, ()))
        for k in writes:
            deps.extend(self.writer.get(k, ()))
            deps.extend(self.readers.get(k, ()))
        for d in deps:
            if d.dma:
                o.deps.append(("d", d.dsem, max(d.dval, self.dcum[d.dsem])))
            else:
                if d.eng == eng and (eng == "pe"):
                    continue
                o.deps.append(("e", d))
        if not dma:
            rset = set()
            for k in reads:
                for w in self.writer.get(k, ()):
                    rset.add(id(w))
            o.deps = [t for t in o.deps if not (t[0] == "e" and t[1].eng == eng and id(t[1]) not in rset and id(t[1]) not in keep)]
        if dma:
            k = self.drr
            self.drr = (self.drr + 1) % self.NDSEM
            self.dcum[k] += 16
            o.dsem, o.dval = k, self.dcum[k]
        for t in o.deps:
            if t[0] == "e":
                t[1].signal = True
        for k in reads:
            self.readers.setdefault(k, []).append(o)
        for k in writes:
            prev = self.writer.get(k)
            if dma and prev and all(p.dma for p in prev) and not self.readers.get(k):
                prev.append(o)
            else:
                self.writer[k] = [o]
            self.readers[k] = []
        o.idx = len(self.ops[eng])
        self.ops[eng].append(o)
        return o

    def barrier(self):
        toks = []
        for e in self.ENGS:
            if self.ops[e]:
                last = None
                for o in reversed(self.ops[e]):
                    if not o.dma:
                        last = o
                        break
                if last is not None:
                    toks.append(last)
        dtoks = [k for k in range(self.NDSEM) if self.dcum[k] > 0]
        for e in self.ENGS:
            self.pending[e] = [t for t in toks if t.eng != e or e != "pe"]
            for k in dtoks:
                p = Op("sp", None, True)
                p.dsem, p.dval = k, self.dcum[k]
                self.pending[e].append(p)
        self.writer.clear()
        self.readers.clear()

    def prepare(self, es):
        nc = self.nc
        nsem_needed = {}
        for e in self.ENGS:
            c = 0
            for o in self.ops[e]:
                if o.signal and not o.dma:
                    c += 1
                    o.cnt = c
            nsem_needed[e] = c // self.SEM_WRAP + 1
        esems = {e: [es.enter_context(nc.semaphore(f"s_{e}_{i}")) for i in range(nsem_needed[e])] for e in self.ENGS}
        dsems = [es.enter_context(nc.semaphore(f"s_dma_{i}")) for i in range(self.NDSEM)]
        self.esems, self.dsems = esems, dsems

    def emit(self, block):
        esems, dsems = self.esems, self.dsems
        W = self.SEM_WRAP

        def sem_of(o):
            i = (o.cnt - 1) // W
            return esems[o.eng][i], o.cnt - i * W, i

        def run(engname, eng):
            waited = {}
            for o in self.ops[engname]:
                for t in o.deps:
                    if t[0] == "d":
                        key = ("d", t[1])
                        if waited.get(key, 0) >= t[2]:
                            continue
                        waited[key] = t[2]
                        eng.wait_ge(dsems[t[1]], t[2])
                    else:
                        s, v, i = sem_of(t[1])
                        key = ("e", t[1].eng, i)
                        if waited.get(key, 0) >= v:
                            continue
                        waited[key] = v
                        eng.wait_ge(s, v)
                ins = o.fn(eng)
                if o.dma:
                    ins.then_inc(dsems[o.dsem], 16)
                elif o.signal:
                    s, v, i = sem_of(o)
                    ins.then_inc(s, 1)
            if engname == "sp":
                for k in range(self.NDSEM):
                    if self.dcum[k] > 0 and waited.get(("d", k), 0) < self.dcum[k]:
                        eng.wait_ge(dsems[k], self.dcum[k])

        @block.tensor
        def _(e):
            run("pe", e)

        @block.scalar
        def _(e):
            run("act", e)

        @block.vector
        def _(e):
            run("dve", e)

        @block.gpsimd
        def _(e):
            run("pool", e)

        @block.sync
        def _(e):
            run("sp", e)


def build_program(depth=DEPTH, taps=None, stop_after=None, ne_decl=NE, dbg_stage=99):
    nc = bass.Bass("TRN2", target_bir_lowering=False)
    es = ExitStack()
    P = Plan(nc)
    taps = taps or {}
    tap_out = {}

    def din(name, shape):
        return nc.dram_tensor(name, list(shape), F32, kind="ExternalInput").ap()

    x_in = din("x", [NLAT, D])
    ctx_in = din("ctx", [NCTX, D])
    c_in = din("c", [1, D])
    cctx_in = din("c_ctx", [1, D])
    w_ada = din("w_ada", [DEPTH, D, 6 * D])
    b_ada = din("b_ada", [DEPTH, 6 * D])
    g_mix = din("g_mix", [DEPTH, D])
    g_ffn = din("g_ffn", [DEPTH, D])
    g_final = din("g_final", [1, D])
    w_in = din("w_in", [DEPTH, D, N_IN])
    dn_conv = din("dn_conv", [DEPTH, 3, 1536])
    dn_a_log = din("dn_a_log", [DEPTH, 8])
    dn_dt_bias = din("dn_dt_bias", [DEPTH, 8])
    dn_norm_g = din("dn_norm_g", [DEPTH, 128])
    hg_lb_logits = din("hg_lb_logits", [DEPTH, 512])
    hg_norm_g = din("hg_norm_g", [DEPTH, 128])
    w_br_dn = din("w_br_dn", [DEPTH, 512, D])
    w_br_hg = din("w_br_hg", [DEPTH, 512, D])
    w_out = din("w_out", [DEPTH, D, D])
    w_rg = din("w_router_grp", [DEPTH, D, 4])
    b_rg = din("b_router_grp", [DEPTH, 4])
    w_re = din("w_router_exp", [DEPTH, D, 32])
    b_re = din("b_router_exp", [DEPTH, 32])
    w_eg = din("w_exp_gate", [DEPTH, ne_decl, D, DEXP])
    w_eu = din("w_exp_up", [DEPTH, ne_decl, D, DEXP])
    w_ed = din("w_exp_down", [DEPTH, ne_decl, DEXP, D])
    out = nc.dram_tensor("out", [NLAT, D], F32, kind="ExternalOutput").ap()
    for nm, shp in taps.items():
        if shp is None:
            continue
        tap_out[nm] = nc.dram_tensor("tap_" + nm, list(shp), F32, kind="ExternalOutput").ap()

    Xd = nc.dram_tensor("Xd", [NTOK, D], F32).ap()
    modrow_d = nc.dram_tensor("modrow_d", [2, 6 * D], F32).ap()

    def sb(name, shape, dt=F32):
        return es.enter_context(nc.sbuf_tensor(name, list(shape), dt))

    hT = sb("hT", [128, KC, NTOK], BF16)
    ident_f = sb("ident_f", [128, 128], F32)
    ident_b = sb("ident_b", [128, 128], BF16)
    ones_f = sb("ones_f", [128, 128], F32)
    eps_c = sb("eps_c", [128, 1], F32)
    sT = sb("sT", [128, KC, 2], F32)
    modfm = sb("modfm", [128, 2, 6, KC], F32)
    gmixT = sb("gmixT", [128, KC], F32)
    gffnT = sb("gffnT", [128, KC], F32)
    AB = sb("AB", [128, 2, 2, KC], F32)
    ARENA = 42400
    A = sb("A", [128, ARENA], F32)
    WORK0 = 16384
    YMIX0 = ARENA - 9216
    wst = A[:, 0:8192].rearrange("p (s n) -> p s n", s=2)
    work = A[:, WORK0:ARENA]
    ymix = A[:, YMIX0:ARENA].bitcast(BF16).rearrange("p (b h n) -> p b h n", b=2, h=4)
    U_f = sb("U_f", [128, 128], F32)
    U_b = sb("U_b", [128, 128], F32)
    SU_f = sb("SU_f", [128, 128], F32)
    SU_b = sb("SU_b", [128, 128], F32)
    UBf = sb("UBf", [128, 128], F32)
    UBb = sb("UBb", [128, 128], F32)
    DBf = sb("DBf", [128, 128], F32)
    DBb = sb("DBb", [128, 128], F32)
    _consts = {"UBf": UBf, "UBb": UBb, "DBf": DBf, "DBb": DBb}

    def sb_const(n):
        return _consts[n]
    lgG = A[:, 41600:41672].rearrange("p (t n) -> p t n", t=NT)
    lgE = A[:, 41672:42248].rearrange("p (t n) -> p t n", t=NT)
    one_c = sb("one_c", [128, 1], F32)
    eps128_c = sb("eps128_c", [128, 1], F32)
    psum = es.enter_context(nc.psum_tensor("psum", [128, 8, 512], F32))

    V, S_, G, T_, SPQ = "dve", "act", "pool", "pe", "sp"

    P.op(G, lambda e: e.memset(ident_f[:], 0.0), writes=["ident_f"])
    P.op(G, lambda e: e.affine_select(out=ident_f[:], in_=ident_f[:], pattern=[[-1, 128]], compare_op=ALU.not_equal,
                                      fill=1.0, base=0, channel_multiplier=1), reads=["ident_f"], writes=["ident_f"])
    P.op(G, lambda e: e.tensor_copy(out=ident_b[:], in_=ident_f[:]), reads=["ident_f"], writes=["ident_b"])
    P.op(G, lambda e: e.memset(ones_f[:], 1.0), writes=["ones_f"])
    P.op(G, lambda e: e.memset(eps_c[:], EPS), writes=["eps_c"])
    P.op(G, lambda e: e.memset(one_c[:], 1.0), writes=["one_c"])
    P.op(G, lambda e: e.memset(eps128_c[:], 128.0 * EPS), writes=["eps128_c"])
    for (m_, cm, op_, nm) in ((U_f, -1, ALU.is_ge, "U_f"), (U_b, 1, ALU.is_ge, "U_b"), (SU_f, -1, ALU.is_gt, "SU_f"), (SU_b, 1, ALU.is_gt, "SU_b")):
        P.op(G, lambda e, m_=m_: e.memset(m_[:], 1.0), writes=[nm])
        P.op(G, lambda e, m_=m_, cm=cm, op_=op_: e.affine_select(out=m_[:], in_=m_[:], pattern=[[-cm, 128]], compare_op=op_, fill=0.0,
                                                               base=0, channel_multiplier=cm), reads=[nm], writes=[nm])

    for (dst_, src_, nm) in ((UBf, U_f, "U_f"), (UBb, U_b, "U_b"), (DBf, SU_b, "SU_b"), (DBb, SU_f, "SU_f")):
        P.op(G, lambda e, dst_=dst_, src_=src_: e.tensor_copy(out=dst_[:], in_=src_[:]), reads=[nm], writes=[nm + "B"])
        P.op(G, lambda e, dst_=dst_: e.memset(dst_[0:64, 64:128], 0.0), reads=[nm + "B"], writes=[nm + "B"])
        P.op(G, lambda e, dst_=dst_: e.memset(dst_[64:128, 0:64], 0.0), reads=[nm + "B"], writes=[nm + "B"])
    crow = work[0:2, 0:D]
    P.op(SPQ, lambda e: e.dma_start(out=work[0:1, 0:D], in_=c_in[:, :]), writes=["crow0"], dma=True)
    P.op(SPQ, lambda e: e.dma_start(out=work[1:2, 0:D], in_=cctx_in[:, :]), writes=["crow1"], dma=True)
    P.op(S_, lambda e: e.activation(out=crow, in_=crow, func=AF.Silu), reads=["crow0", "crow1"], writes=["crow"])
    for k in range(KC):
        P.op(T_, lambda e, k=k: e.transpose(psum[:, 0, 2 * k:2 * k + 2], crow[:, k * 128:(k + 1) * 128], ident_f[0:2, 0:2]),
             reads=["crow", "ident_f"], writes=[("ps", 0)])
    P.op(V, lambda e: e.tensor_copy(out=sT[:].rearrange("p k v -> p (k v)"), in_=psum[:, 0, 0:2 * KC]), reads=[("ps", 0)], writes=["sT"])

    def tap(name, ap_sb, key, dst=None):
        if name in tap_out:
            d = tap_out[name] if dst is None else dst
            P.op(SPQ, lambda e: e.dma_start(out=d, in_=ap_sb), reads=[key], writes=["tap_" + name], dma=True)

    def phase_mod(l):
        modrow = work[0:2, 0:6 * D]
        brow = work[0:2, 6 * D:12 * D]
        P.op(SPQ, lambda e: e.dma_start(out=brow, in_=b_ada[l:l + 1, :].partition_broadcast(2)), writes=["brow"], dma=True)
        NB = 24
        for nb in range(NB):
            slot = nb % 2
            P.op(SPQ, lambda e, nb=nb, slot=slot: e.dma_start(
                out=wst[:, slot, 0:KC * 256].rearrange("p (k n) -> p k n", k=KC),
                in_=w_ada[l, :, nb * 256:(nb + 1) * 256].rearrange("(k p) n -> p k n", p=128)),
                writes=[("wst", slot)], dma=True)
            pb = psum[0:2, nb % 2, 0:256]
            for k in range(KC):
                P.op(T_, lambda e, k=k, slot=slot, pb=pb: e.matmul(pb, lhsT=sT[:, k, :], rhs=wst[:, slot, k * 256:(k + 1) * 256],
                                                                  start=(k == 0), stop=(k == KC - 1)),
                     reads=[("wst", slot), "sT"], writes=[("ps", nb % 2)])
            P.op(V, lambda e, nb=nb, pb=pb: e.tensor_tensor(out=modrow[:, nb * 256:(nb + 1) * 256], in0=pb,
                                                            in1=brow[:, nb * 256:(nb + 1) * 256], op=ALU.add),
                 reads=[("ps", nb % 2), "brow"], writes=["modrow"])
        P.op(SPQ, lambda e: e.dma_start(out=modrow_d[:, :], in_=modrow), reads=["modrow"], writes=["modrow_d"], dma=True)
        tap(f"modrow{l}", modrow, "modrow")
        for j in range(48):
            P.op(T_, lambda e, j=j: e.transpose(psum[:, 2, 2 * j:2 * j + 2], modrow[:, j * 128:(j + 1) * 128], ident_f[0:2, 0:2]),
                 reads=["modrow", "ident_f"], writes=[("ps", 2)])
        P.op(V, lambda e: e.tensor_copy(out=modfm[:].rearrange("p v w k -> p v (w k)"),
                                        in_=psum[:, 2, 0:96].rearrange("p (j v) -> p v j", v=2)),
             reads=[("ps", 2)], writes=[("modfm", 0), ("modfm", 1)])
        grow = work[0:2, 12 * D:13 * D]
        P.op(SPQ, lambda e: e.dma_start(out=work[0:1, 12 * D:13 * D], in_=g_mix[l:l + 1, :]), writes=["grow0"], dma=True)
        P.op(SPQ, lambda e: e.dma_start(out=work[1:2, 12 * D:13 * D], in_=g_ffn[l:l + 1, :]), writes=["grow1"], dma=True)
        for k in range(KC):
            P.op(T_, lambda e, k=k: e.transpose(psum[:, 3, 2 * k:2 * k + 2], grow[:, k * 128:(k + 1) * 128], ident_f[0:2, 0:2]),
                 reads=["grow0", "grow1", "ident_f"], writes=[("ps", 3)])
        P.op(V, lambda e: e.tensor_copy(out=gmixT[:], in_=psum[:, 3, 0:2 * KC].rearrange("p (k v) -> p v k", v=2)[:, 0, :]),
             reads=[("ps", 3)], writes=["gmixT"])
        P.op(V, lambda e: e.tensor_copy(out=gffnT[:], in_=psum[:, 3, 0:2 * KC].rearrange("p (k v) -> p v k", v=2)[:, 1, :]),
             reads=[("ps", 3)], writes=["gffnT"])

    def set_AB(which):
        sh, sc = (0, 1) if which == 0 else (3, 4)
        g = gmixT if which == 0 else gffnT
        gk = "gmixT" if which == 0 else "gffnT"
        for v in range(2):
            P.op(V, lambda e, v=v: e.scalar_tensor_tensor(out=AB[:, v, 0, :], in0=modfm[:, v, sc, :], scalar=1.0, in1=g[:],
                                                          op0=ALU.add, op1=ALU.mult),
                 reads=[("modfm", v), gk], writes=[("AB", v)])
            P.op(V, lambda e, v=v: e.tensor_copy(out=AB[:, v, 1, :], in_=modfm[:, v, sh, :]),
                 reads=[("modfm", v)], writes=[("AB", v)])

    def x_tile_src(l, first_read, t):
        if l == 0 and first_read:
            return ctx_in[t * 128:(t + 1) * 128, :] if t < 2 else x_in[(t - 2) * 128:(t - 1) * 128, :]
        return Xd[t * 128:(t + 1) * 128, :]

    def phase_norm(l, which, tiles):
        set_AB(which)
        xt = [work[:, j * D:(j + 1) * D] for j in range(4)]
        xn = [work[:, 4 * D:5 * D], work[:, 5 * D:6 * D]]
        t32 = [work[:, 6 * D:7 * D], work[:, 7 * D:8 * D]]
        lo = [work[:, 8 * D:8 * D + D // 2].bitcast(BF16), work[:, 8 * D + D // 2:9 * D].bitcast(BF16)]
        junk = work[:, 9 * D:9 * D + D // 2].bitcast(BF16)
        st = work[:, 10 * D:10 * D + 64]
        wr_f = work[:, 11 * D:11 * D + 288]
        wr_hi = work[:, 11 * D + 288:11 * D + 432].bitcast(BF16)
        wr_lo = work[:, 11 * D + 432:11 * D + 576].bitcast(BF16)
        wr_t = work[:, 11 * D + 576:11 * D + 864]
        brow = work[:, 11 * D + 864:11 * D + 900]
        if which == 1:
            P.op(SPQ, lambda e: e.dma_start(out=wr_f.rearrange("p (k n) -> p k n", k=KC)[:, :, 0:4],
                                            in_=w_rg[l].rearrange("(k p) n -> p k n", p=128)), writes=["wr_f0"], dma=True)
            P.op(SPQ, lambda e: e.dma_start(out=wr_f.rearrange("p (k n) -> p k n", k=KC)[:, :, 4:36],
                                            in_=w_re[l].rearrange("(k p) n -> p k n", p=128)), writes=["wr_f1"], dma=True)
            P.op(SPQ, lambda e: e.dma_start(out=brow[:, 0:4], in_=b_rg[l:l + 1, :].partition_broadcast(128)), writes=["brow0"], dma=True)
            P.op(SPQ, lambda e: e.dma_start(out=brow[:, 4:36], in_=b_re[l:l + 1, :].partition_broadcast(128)), writes=["brow1"], dma=True)
            P.op(V, lambda e: e.tensor_copy(out=wr_hi, in_=wr_f), reads=["wr_f0", "wr_f1"], writes=["wr_hi"])
            P.op(V, lambda e: e.tensor_tensor(out=wr_t, in0=wr_f, in1=wr_hi, op=ALU.subtract), reads=["wr_hi", "wr_f0", "wr_f1"], writes=["wr_t"])
            P.op(V, lambda e: e.tensor_copy(out=wr_lo, in_=wr_t), reads=["wr_t"], writes=["wr_lo"])
        def stage1(i, t):
            b = i % 2
            xb = i % 4
            v = 1 if t < 2 else 0
            src = x_tile_src(l, which == 0, t)
            P.op(SPQ, lambda e, xb=xb, src=src: e.dma_start(out=xt[xb], in_=src), reads=[("Xd", t)],
                 writes=[("xt", xb)], dma=True)
            P.op(S_, lambda e, b=b, xb=xb: e.activation(out=junk, in_=xt[xb], func=AF.Square, accum_out=st[:, 2 * b:2 * b + 1]),
                 reads=[("xt", xb)], writes=["junk", ("st", b)])
            P.op(S_, lambda e, b=b: e.activation(out=st[:, 2 * b + 1:2 * b + 2], in_=st[:, 2 * b:2 * b + 1], func=AF.Ln, scale=1.0 / D, bias=eps_c[:]),
                 reads=[("st", b), "eps_c"], writes=[("st2", b)])
            P.op(S_, lambda e, b=b: e.activation(out=st[:, 2 * b + 1:2 * b + 2], in_=st[:, 2 * b + 1:2 * b + 2], func=AF.Exp, scale=-0.5),
                 reads=[("st2", b)], writes=[("st2", b)])
            P.op(G, lambda e, b=b, xb=xb: e.tensor_scalar(out=xn[b], in0=xt[xb], scalar1=st[:, 2 * b + 1:2 * b + 2], scalar2=None, op0=ALU.mult),
                 reads=[("xt", xb), ("st2", b)], writes=[("xn", b)])

        def stage2(i, t):
            b = i % 2
            v = 1 if t < 2 else 0
            banks = (2 * b, 2 * b + 1)
            for k in range(KC):
                bk = banks[k // 4]
                P.op(T_, lambda e, b=b, k=k, bk=bk: e.transpose(psum[:, bk, (k % 4) * 128:(k % 4 + 1) * 128], xn[b][:, k * 128:(k + 1) * 128], ident_f[:]),
                     reads=[("xn", b), "ident_f"], writes=[("ps", bk)])
            for k in range(KC):
                bk = banks[k // 4]
                src_ps = psum[:, bk, (k % 4) * 128:(k % 4 + 1) * 128]
                dst = t32[b][:, k * 128:(k + 1) * 128] if which == 1 else hT[:, k, t * 128:(t + 1) * 128]
                wk = [("t32", b, k // 4)] if which == 1 else [("hT", k, t)]
                if k < 4:
                    P.op(S_, lambda e, k=k, v=v, src_ps=src_ps, dst=dst: e.activation(
                        out=dst, in_=src_ps, func=AF.Identity, scale=AB[:, v, 0, k:k + 1], bias=AB[:, v, 1, k:k + 1]),
                        reads=[("ps", bk), ("AB", v)], writes=wk)
                else:
                    P.op(V, lambda e, k=k, v=v, src_ps=src_ps, dst=dst: e.tensor_scalar(
                        out=dst, in0=src_ps, scalar1=AB[:, v, 0, k:k + 1], scalar2=AB[:, v, 1, k:k + 1], op0=ALU.mult, op1=ALU.add),
                        reads=[("ps", bk), ("AB", v)], writes=wk)
            if which == 1:
                t3 = t32[b].rearrange("p (k n) -> p k n", k=KC)
                l3 = lo[b].rearrange("p (k n) -> p k n", k=KC)
                hview = hT[:, :, t * 128:(t + 1) * 128]
                P.op(G, lambda e, t3=t3, hview=hview: e.tensor_copy(out=hview, in_=t3), reads=[("t32", b, 0), ("t32", b, 1)],
                     writes=[("hT", k, t) for k in range(KC)])
                P.op(V, lambda e, t3=t3, l3=l3, hview=hview: e.tensor_tensor(out=l3, in0=t3, in1=hview, op=ALU.subtract),
                     reads=[("t32", b, 0), ("t32", b, 1)] + [("hT", k, t) for k in range(KC)], writes=[("lo", b)])
                rb = 4 + b
                n_mm = 3 * KC
                j = 0
                for k in range(KC):
                    for (lh, rw, kk) in ((hT[:, k, t * 128:(t + 1) * 128], wr_hi, ("hT", k, t)), (lo[b][:, k * 128:(k + 1) * 128], wr_hi, ("lo", b)),
                                         (hT[:, k, t * 128:(t + 1) * 128], wr_lo, ("hT", k, t))):
                        P.op(T_, lambda e, lh=lh, rw=rw, k=k, j=j, rb=rb: e.matmul(psum[:, rb, 0:36], lhsT=lh, rhs=rw[:, k * 36:(k + 1) * 36],
                                                                                  start=(j == 0), stop=(j == n_mm - 1)),
                             reads=[kk, "wr_hi", "wr_lo"], writes=[("ps", rb)])
                        j += 1
                P.op(V, lambda e, t=t, rb=rb: e.tensor_tensor(out=lgG[:, t, :], in0=psum[:, rb, 0:4], in1=brow[:, 0:4], op=ALU.add),
                     reads=[("ps", rb), "brow0"], writes=[("lgG", t)])
                P.op(V, lambda e, t=t, rb=rb: e.tensor_tensor(out=lgE[:, t, :], in0=psum[:, rb, 4:36], in1=brow[:, 4:36], op=ALU.add),
                     reads=[("ps", rb), "brow1"], writes=[("lgE", t)])

        for i in range(len(tiles) + 1):
            if i < len(tiles):
                stage1(i, tiles[i])
            if i >= 1:
                stage2(i - 1, tiles[i - 1])

    PADW = 2307
    TBLK = [(0, 256)] + [(256 + 512 * i, 512) for i in range(4)]

    def pcol(n):
        return n + 1 if n < NCTX else n + 2

    bank_rr = [0]

    def next_bank():
        b = bank_rr[0]
        bank_rr[0] = (b + 1) % 8
        return b

    evac_rr = [0]

    def evac_eng():
        evac_rr[0] ^= 1
        return S_ if evac_rr[0] else V

    def copy_op(eng, out, in_, reads, writes):
        if eng == S_:
            P.op(S_, lambda e: e.activation(out=out, in_=in_, func=AF.Copy), reads=reads, writes=writes)
        else:
            P.op(eng, lambda e: e.tensor_copy(out=out, in_=in_), reads=reads, writes=writes)

    def load_w_cols(l, slot, groups, key):
        res = []
        off = 0
        for (c0, n) in groups:
            P.op(SPQ, lambda e, c0=c0, n=n, off=off: e.dma_start(
                out=wst[:, slot, off:off + KC * n].rearrange("p (k n) -> p k n", k=KC),
                in_=w_in[l, :, c0:c0 + n].rearrange("(k p) n -> p k n", p=128)),
                writes=[("wst", slot)], dma=True)
            res.append((off, n))
            off += KC * n
        return res

    def interleave(gens):
        active = []
        pending = list(gens)
        while active or pending:
            while pending and len(active) < 4:
                active.append(pending.pop(0))
            nxt = []
            for g in active:
                try:
                    next(g)
                    nxt.append(g)
                except StopIteration:
                    pass
            active = nxt

    def phase_dn(l, ctx_out):
        W0 = 10240
        wbf_dn = A[:, 8192:10240].bitcast(BF16)
        pre = A[:, W0:W0 + PADW]
        yq = A[:, W0 + 2308:W0 + 2308 + PADW]
        yk = A[:, W0 + 4616:W0 + 4616 + PADW]
        yv = A[:, W0 + 6924:W0 + 6924 + PADW]
        o = W0 + 9232
        ktok = A[:, o:o + 2304].rearrange("p (t n) -> p t n", t=NT); o += 2304
        vtok = A[:, o:o + 2304].rearrange("p (t n) -> p t n", t=NT); o += 2304
        oacc = A[:, o:o + 2304].rearrange("p (t n) -> p t n", t=NT); o += 2304
        zs = A[:, o:o + 1152].bitcast(BF16).rearrange("p (t n) -> p t n", t=NT); o += 1152
        ba_raw = A[:, o:o + 288].rearrange("p (t n) -> p t n", t=NT); o += 288
        beta = A[:, o:o + 144].rearrange("p (d t h) -> p d t h", d=2, t=NT); o += 144
        gg = A[:, o:o + 144].rearrange("p (d t h) -> p d t h", d=2, t=NT); o += 144
        gc = A[:, o:o + 144].rearrange("p (d t h) -> p d t h", d=2, t=NT); o += 144
        egc = A[:, o:o + 144].rearrange("p (d t h) -> p d t h", d=2, t=NT); o += 144
        dl = A[:, o:o + 144].rearrange("p (d t h) -> p d t h", d=2, t=NT); o += 144
        egl = A[:, o:o + 144].rearrange("p (d t h) -> p d t h", d=2, t=NT); o += 144
        negA = A[:, o:o + 8]; o += 8
        dtb = A[:, o:o + 8]; o += 8
        cw = A[:, o:o + 36].rearrange("p (c k) -> p c k", k=3); o += 36
        gnb = A[:, o:o + 128]; o += 128
        sq = A[:, o:o + 512]; o += 512
        rn = A[:, o:o + 512]; o += 512
        stat = A[:, o:o + 64]; o += 64
        wba_f = A[:, o:o + 128]; o += 128
        wba_b = A[:, o:o + 64].bitcast(BF16); o += 64
        Sbuf = [A[:, o + 128 * i:o + 128 * (i + 1)] for i in range(4)]; o += 512
        xslots_extra = o
        o += 16 * 128
        assert o <= YMIX0, (o, YMIX0)
        NSLOT = 13
        def uslot(u, i):
            if u >= 4:
                base = (u - 4) * NSLOT * 128
                return A[:, base + 128 * i:base + 128 * (i + 1)]
            if u < 2:
                base = W0 + (0 if u == 0 else 6924)
            else:
                base = xslots_extra if u == 2 else None
            if u == 3:
                if i < 3:
                    base = xslots_extra + 13 * 128
                    return A[:, base + 128 * i:base + 128 * (i + 1)]
                j = i - 3
                if j < 5:
                    base = W0 + 13 * 128
                    return A[:, base + 128 * j:base + 128 * (j + 1)]
                base = W0 + 6924 + 13 * 128
                return A[:, base + 128 * (j - 5):base + 128 * (j - 4)]
            return A[:, base + 128 * i:base + 128 * (i + 1)]

        def uslot2(u, i):
            a = uslot(u, i)
            b = uslot(u, i + 1)
            return a, b

        cwrow = A[0:3, W0:W0 + 1536]
        P.op(SPQ, lambda e: e.dma_start(out=cwrow, in_=dn_conv[l]), writes=["cwrow"], dma=True)
        bk = next_bank()
        for c in range(12):
            P.op(T_, lambda e, c=c, bk=bk: e.transpose(psum[:, bk, 3 * c:3 * c + 3], cwrow[:, c * 128:(c + 1) * 128], ident_f[0:3, 0:3]),
                 reads=["cwrow", "ident_f"], writes=[("ps", bk)])
        P.op(V, lambda e, bk=bk: e.tensor_copy(out=cw[:].rearrange("p c k -> p (c k)"), in_=psum[:, bk, 0:36]), reads=[("ps", bk)], writes=["cw"])
        P.op(SPQ, lambda e: e.dma_start(out=negA, in_=dn_a_log[l:l + 1, :].partition_broadcast(128)), writes=["negA"], dma=True)
        P.op(SPQ, lambda e: e.dma_start(out=dtb, in_=dn_dt_bias[l:l + 1, :].partition_broadcast(128)), writes=["dtb"], dma=True)
        P.op(SPQ, lambda e: e.dma_start(out=gnb, in_=dn_norm_g[l:l + 1, :].partition_broadcast(128)), writes=["gnb"], dma=True)
        P.op(S_, lambda e: e.activation(out=negA, in_=negA, func=AF.Exp), reads=["negA"], writes=["negA"])
        P.op(V, lambda e: e.tensor_scalar(out=negA, in0=negA, scalar1=-1.0, scalar2=None, op0=ALU.mult), reads=["negA"], writes=["negA"])
        P.op(SPQ, lambda e: e.dma_start(out=wba_f.rearrange("p (k n) -> p k n", k=KC),
                                        in_=w_in[l, :, OFF_DNB:OFF_DNB + 16].rearrange("(k p) n -> p k n", p=128)),
             writes=["wba_f"], dma=True)
        P.op(V, lambda e: e.tensor_copy(out=wba_b, in_=wba_f), reads=["wba_f"], writes=["wba_b"])
        bk = next_bank()
        pba = psum[:, bk, 0:288].rearrange("p (t n) -> p t n", t=NT)
        for t in range(NT):
            for k in range(KC):
                P.op(T_, lambda e, t=t, k=k: e.matmul(pba[:, t, :], lhsT=hT[:, k, t * 128:(t + 1) * 128], rhs=wba_b[:, k * 16:(k + 1) * 16],
                                                      start=(k == 0), stop=(k == KC - 1)),
                     reads=["wba_b"] + [("hT", k, t)], writes=[("ps", bk)])
        P.op(V, lambda e: e.tensor_copy(out=ba_raw, in_=pba), reads=[("ps", bk)], writes=["ba_raw"])
        for d in range(2):
            P.op(S_, lambda e, d=d: e.activation(out=beta[:, d, :, :], in_=ba_raw[:, :, 4 * d:4 * d + 4], func=AF.Sigmoid),
                 reads=["ba_raw"], writes=[("beta", d)])
            P.op(V, lambda e, d=d: e.tensor_tensor(out=gg[:, d, :, :], in0=ba_raw[:, :, 8 + 4 * d:12 + 4 * d],
                                                   in1=dtb[:, 4 * d:4 * d + 4].unsqueeze(1).to_broadcast([128, NT, 4]), op=ALU.add),
                 reads=["ba_raw", "dtb"], writes=[("gg", d)])
            P.op(S_, lambda e, d=d: e.activation(out=gg[:, d, :, :], in_=gg[:, d, :, :], func=AF.Exp), reads=[("gg", d)], writes=[("gg", d)])
            P.op(S_, lambda e, d=d: e.activation(out=gg[:, d, :, :], in_=gg[:, d, :, :], func=AF.Ln, bias=one_c[:]), reads=[("gg", d), "one_c"],
                 writes=[("gg", d)])
            P.op(V, lambda e, d=d: e.tensor_tensor(out=gg[:, d, :, :], in0=gg[:, d, :, :],
                                                   in1=negA[:, 4 * d:4 * d + 4].unsqueeze(1).to_broadcast([128, NT, 4]), op=ALU.mult),
                 reads=[("gg", d), "negA"], writes=[("gg", d)])
        bk = next_bank()
        for d in range(2):
            P.op(T_, lambda e, d=d: e.matmul(psum[:, bk, 72 * d:72 * d + 72], lhsT=(U_f if d == 0 else U_b)[:],
                                             rhs=gg[:, d, :, :].rearrange("p t h -> p (t h)"), start=True, stop=True),
                 reads=[("gg", d), "U_f", "U_b"], writes=[("ps", bk)])
        P.op(T_, lambda e: e.matmul(psum[:, bk, 144:288], lhsT=ones_f[:], rhs=gg[:].rearrange("p d t h -> p (d t h)"), start=True, stop=True),
             reads=[("gg", 0), ("gg", 1), "ones_f"], writes=[("ps", bk)])
        flat = lambda a: a[:].rearrange("p d t h -> p (d t h)")
        P.op(V, lambda e: e.tensor_copy(out=flat(gc), in_=psum[:, bk, 0:144]), reads=[("ps", bk)], writes=["gc"])
        P.op(V, lambda e: e.tensor_tensor(out=flat(dl), in0=psum[:, bk, 144:288], in1=flat(gc), op=ALU.subtract), reads=[("ps", bk), "gc"],
             writes=["dl"])
        P.op(S_, lambda e: e.activation(out=flat(egl), in_=psum[:, bk, 144:288], func=AF.Exp), reads=[("ps", bk)], writes=["egl"])
        P.op(S_, lambda e: e.activation(out=flat(dl), in_=flat(dl), func=AF.Exp), reads=["dl"], writes=["dl"])
        P.op(S_, lambda e: e.activation(out=flat(egc), in_=flat(gc), func=AF.Exp), reads=["gc"], writes=["egc"])
        if "dn_gc" in tap_out:
            tap("dn_gc", flat(gc), "gc")
            tap("dn_beta", flat(beta), ("beta", 0))

        tiles_f = list(range(NT))
        tiles_b = [1, 0] + list(range(NT - 1, 1, -1))

        for h in range(4):
            P.barrier()
            ws = h % 2
            grp = load_w_cols(l, ws, [(OFF_DNQ + 128 * h, 128), (OFF_DNK + 128 * h, 128), (OFF_DNV + 128 * h, 128), (OFF_DNZ + 128 * h, 128)],
                              "dnw")
            wb = wbf_dn[:, 0:4096]
            if h == 0 and "dn_wst" in tap_out:
                tap("dn_wst", wst[:, ws, 0:4096], ("wst", ws))
            for j in range(4):
                eng = (G, V, G, V)[j]
                P.op(eng, lambda e, j=j: e.tensor_copy(out=wb[:, j * 1024:(j + 1) * 1024], in_=wst[:, ws, j * 1024:(j + 1) * 1024]),
                     reads=[("wst", ws)], writes=[("wb", ws, j)])
            for pc in (0, 257, 2306):
                P.op(G, lambda e, pc=pc: e.memset(pre[:, pc:pc + 1], 0.0), writes=[("prepad", pc)])
            PREK = [("pre", t0) for (t0, tn) in TBLK] + [("prepad", pc) for pc in (0, 257, 2306)]
            ydst = (yq, yk, yv)
            for j in range(3):
                for (t0, tn) in TBLK:
                    bk = next_bank()
                    for k in range(KC):
                        P.op(T_, lambda e, j=j, k=k, t0=t0, tn=tn, bk=bk: e.matmul(
                            psum[:, bk, 0:tn], lhsT=wb[:, j * 1024 + k * 128:j * 1024 + (k + 1) * 128], rhs=hT[:, k, t0:t0 + tn],
                            start=(k == 0), stop=(k == KC - 1)),
                            reads=[("wb", ws, j)] + [("hT", k, t) for t in range(t0 // 128, (t0 + tn) // 128)], writes=[("ps", bk)])
                    c0 = pcol(t0)
                    copy_op(evac_eng(), pre[:, c0:c0 + tn], psum[:, bk, 0:tn], [("ps", bk)], [("pre", t0)])
                y = ydst[j]
                cj = 4 * j + h
                if h == 0 and j == 2 and "dn_pre" in tap_out:
                    tap("dn_pre", pre[:, 0:PADW], ("pre", 0))
                    tap("dn_cw", cw[:].rearrange("p c k -> p (c k)"), "cw")
                P.op(S_, lambda e, y=y, cj=cj: e.activation(out=y[:, 1:2306], in_=pre[:, 1:2306], func=AF.Copy, scale=cw[:, cj, 1:2]),
                     reads=PREK + ["cw"], writes=[("y", j)])
                P.op(V, lambda e, y=y, cj=cj: e.scalar_tensor_tensor(out=y[:, 1:2306], in0=pre[:, 0:2305], scalar=cw[:, cj, 0:1], in1=y[:, 1:2306],
                                                                     op0=ALU.mult, op1=ALU.add), reads=PREK + ["cw", ("y", j)], writes=[("y", j)])
                P.op(V, lambda e, y=y, cj=cj: e.scalar_tensor_tensor(out=y[:, 1:2306], in0=pre[:, 2:2307], scalar=cw[:, cj, 2:3], in1=y[:, 1:2306],
                                                                     op0=ALU.mult, op1=ALU.add), reads=PREK + ["cw", ("y", j)], writes=[("y", j)])
                P.op(S_, lambda e, y=y: e.activation(out=y[:, 1:2306], in_=y[:, 1:2306], func=AF.Silu), reads=[("y", j)], writes=[("y", j)])
                if j < 2:
                    for (t0, tn) in TBLK:
                        c0 = pcol(t0)
                        bk = next_bank()
                        P.op(S_, lambda e, y=y, c0=c0, tn=tn: e.activation(out=sq[:, 0:tn], in_=y[:, c0:c0 + tn], func=AF.Square),
                             reads=[("y", j)], writes=["sq"])
                        P.op(T_, lambda e, bk=bk, tn=tn: e.matmul(psum[:, bk, 0:tn], lhsT=ones_f[:], rhs=sq[:, 0:tn], start=True, stop=True),
                             reads=["sq", "ones_f"], writes=[("ps", bk)])
                        if j == 0:
                            P.op(S_, lambda e, bk=bk, tn=tn: e.activation(out=rn[:, 0:tn], in_=psum[:, bk, 0:tn], func=AF.Ln, scale=128.0,
                                                                          bias=eps128_c[:]), reads=[("ps", bk), "eps128_c"], writes=["rn"])
                        else:
                            P.op(S_, lambda e, bk=bk, tn=tn: e.activation(out=rn[:, 0:tn], in_=psum[:, bk, 0:tn], func=AF.Ln, bias=eps_c[:]),
                                 reads=[("ps", bk), "eps_c"], writes=["rn"])
                        P.op(S_, lambda e, tn=tn: e.activation(out=rn[:, 0:tn], in_=rn[:, 0:tn], func=AF.Exp, scale=-0.5), reads=["rn"],
                             writes=["rn"])
                        P.op(V, lambda e, y=y, c0=c0, tn=tn: e.tensor_tensor(out=y[:, c0:c0 + tn], in0=y[:, c0:c0 + tn], in1=rn[:, 0:tn],
                                                                             op=ALU.mult), reads=["rn", ("y", j)], writes=[("y", j)])
            for t4 in range(0, NT, 4):
                nt4 = min(4, NT - t4)
                bk = next_bank()
                for ti in range(nt4):
                    t = t4 + ti
                    for k in range(KC):
                        P.op(T_, lambda e, t=t, ti=ti, k=k, bk=bk: e.matmul(
                            psum[:, bk, ti * 128:(ti + 1) * 128], lhsT=hT[:, k, t * 128:(t + 1) * 128],
                            rhs=wb[:, 3 * 1024 + k * 128:3 * 1024 + (k + 1) * 128], start=(k == 0), stop=(k == KC - 1)),
                            reads=[("wb", ws, 3), ("hT", k, t)], writes=[("ps", bk)])
                P.op(S_, lambda e, t4=t4, nt4=nt4, bk=bk: e.activation(out=sq[:, 0:nt4 * 128], in_=psum[:, bk, 0:nt4 * 128], func=AF.Silu),
                     reads=[("ps", bk)], writes=["sq"])
                P.op(G, lambda e, t4=t4, nt4=nt4: e.tensor_tensor(out=zs[:, t4:t4 + nt4, :], in0=sq[:, 0:nt4 * 128].rearrange("p (t n) -> p t n", n=128),
                                                                  in1=gnb.unsqueeze(1).to_broadcast([128, nt4, 128]), op=ALU.mult),
                     reads=["sq", "gnb"], writes=[("zs", t4)])
            for (src, dst, nm, j) in ((yk, ktok, "ktok", 1), (yv, vtok, "vtok", 2)):
                for t4 in range(0, NT, 4):
                    nt4 = min(4, NT - t4)
                    bk = next_bank()
                    for ti in range(nt4):
                        t = t4 + ti
                        c0 = pcol(t * 128)
                        P.op(T_, lambda e, src=src, c0=c0, ti=ti, bk=bk: e.transpose(psum[:, bk, ti * 128:(ti + 1) * 128], src[:, c0:c0 + 128],
                                                                                    ident_f[:]),
                             reads=[("y", j), "ident_f"], writes=[("ps", bk)])
                    copy_op(evac_eng(), dst[:, t4:t4 + nt4, :], psum[:, bk, 0:nt4 * 128].rearrange("p (t n) -> p t n", n=128),
                            [("ps", bk)], [(nm, t4)])
            if h == 0 and "dn_qT" in tap_out:
                tap("dn_qT", yq[:, 0:PADW], ("y", 0))
                tap("dn_kT", yk[:, 0:PADW], ("y", 1))
                tap("dn_vtok", vtok[:].rearrange("p t n -> p (t n)"), ("vtok", 0))
            P.barrier()
            for d in range(2):
                P.op(G, lambda e, d=d: e.memset(Sbuf[2 * d][:], 0.0), writes=[("S", d, 0)])

            oacc_written = set()

            def unit(ui, us, d, t, first, pp):
                kq = ("u", us)
                bA = bB = us
                sl = lambda i: uslot(us, i)
                K = lambda n: ("u", us, n)
                c0 = pcol(t * 128)
                Um = U_f if d == 0 else U_b
                MI = U_f if d == 0 else U_b
                MS = SU_f if d == 0 else SU_b
                col = lambda a: a[:, d, t, h:h + 1]
                gb, E, Ei, M, PTt, MT, Xa, Xb, EG, qgT, kg, kdec, nwT = [sl(i) for i in (0, 1, 2, 3, 4, 5, 6, 7, 8, 9, 10, 11, 12)]
                P.op(G, lambda e: e.tensor_scalar(out=gb, in0=ones_f[:], scalar1=col(gg), scalar2=None, op0=ALU.mult),
                     reads=[("gg", d), "ones_f"], writes=[K("gb")])
                yield
                P.op(T_, lambda e: e.matmul(psum[:, bA, 0:128], lhsT=gb, rhs=Um[:], start=True, stop=True), reads=[K("gb")], writes=[("ps", bA)])
                P.op(T_, lambda e: e.matmul(psum[:, bA, 128:256], lhsT=yk[:, c0:c0 + 128], rhs=yk[:, c0:c0 + 128], start=True, stop=True),
                     reads=[("y", 1)], writes=[("ps", bA)])
                P.op(T_, lambda e: e.matmul(psum[:, bA, 256:384], lhsT=yk[:, c0:c0 + 128], rhs=yq[:, c0:c0 + 128], start=True, stop=True),
                     reads=[("y", 1), ("y", 0)], writes=[("ps", bA)])
                yield
                P.op(V, lambda e: e.tensor_scalar(out=E, in0=psum[:, bA, 0:128], scalar1=col(gc), scalar2=0.0, op0=ALU.subtract, op1=ALU.min),
                     reads=[("ps", bA), "gc"], writes=[K("E")])
                yield
                P.op(S_, lambda e: e.activation(out=E, in_=E, func=AF.Exp), reads=[K("E")], writes=[K("E")])
                P.op(S_, lambda e: e.activation(out=EG, in_=psum[:, bA, 0:128], func=AF.Exp), reads=[("ps", bA)], writes=[K("EG")])
                yield
                P.op(G, lambda e: e.tensor_tensor(out=Ei, in0=E, in1=MI[:], op=ALU.mult), reads=[K("E")], writes=[K("Ei")])
                P.op(G, lambda e: e.tensor_tensor(out=qgT, in0=yq[:, c0:c0 + 128], in1=EG, op=ALU.mult), reads=[K("EG"), ("y", 0)], writes=[K("qgT")])
                yield
                P.op(V, lambda e: e.tensor_tensor(out=PTt, in0=psum[:, bA, 256:384], in1=Ei, op=ALU.mult), reads=[("ps", bA), K("Ei")], writes=[K("PT")])
                P.op(V, lambda e: e.scalar_tensor_tensor(out=M, in0=psum[:, bA, 128:256], scalar=col(beta), in1=Ei, op0=ALU.mult, op1=ALU.mult),
                     reads=[("ps", bA), K("Ei"), ("beta", d)], writes=[K("M")])
                yield
                P.op(G, lambda e: e.tensor_tensor(out=M, in0=M, in1=MS[:], op=ALU.mult), reads=[K("M")], writes=[K("M")])
                P.op(G, lambda e: e.tensor_tensor(out=Xa, in0=ident_f[:], in1=M, op=ALU.subtract), reads=[K("M")], writes=[K("Xa")])
                P.op(S_, lambda e: e.activation(out=kg, in_=ktok[:, t, :], func=AF.Copy, scale=col(egc)), reads=["egc", ("ktok", (t // 4) * 4)],
                     writes=[K("kg")])
                P.op(S_, lambda e: e.activation(out=kdec, in_=ktok[:, t, :], func=AF.Copy, scale=col(dl)), reads=["dl", ("ktok", (t // 4) * 4)],
                     writes=[K("kdec")])
                yield
                P.op(T_, lambda e: e.transpose(psum[:, bB, 0:128], M, ident_f[:]), reads=[K("M")], writes=[("ps", bB)])
                yield
                P.op(S_, lambda e: e.activation(out=MT, in_=psum[:, bB, 0:128], func=AF.Copy), reads=[("ps", bB)], writes=[K("MT")])
                yield
                Pprev, PTprev, kP, kPT = M, MT, K("M"), K("MT")
                Xcur, Xnxt, kX, kXn = Xa, Xb, K("Xa"), K("Xb")
                for st in range(1, 7):
                    pa, pb_ = (sl(0), sl(1)) if st % 2 == 1 else (sl(2), sl(3))
                    kpa, kpb = (K("gb"), K("E")) if st % 2 == 1 else (K("Ei"), K("M"))
                    if st < 6:
                        P.op(T_, lambda e, PTprev=PTprev, Pprev=Pprev: e.matmul(psum[:, bB, 0:128], lhsT=PTprev, rhs=Pprev, start=True, stop=True),
                             reads=[kP, kPT], writes=[("ps", bB)])
                    P.op(T_, lambda e, PTprev=PTprev, Pprev=Pprev: e.matmul(psum[:, bB, 128:256], lhsT=Pprev, rhs=PTprev, start=True, stop=True),
                         reads=[kP, kPT], writes=[("ps", bB)])
                    yield
                    eng = S_ if st % 2 == 1 else V
                    if st < 6:
                        copy_op(eng, pa, psum[:, bB, 0:128], [("ps", bB)], [kpa])
                    copy_op(eng, pb_, psum[:, bB, 128:256], [("ps", bB)], [kpb])
                    yield
                    P.op(T_, lambda e, pb_=pb_, Xcur=Xcur: e.matmul(psum[:, bB, 256:384], lhsT=pb_, rhs=Xcur, start=True, stop=True),
                         reads=[kpb, kX], writes=[("ps", bB)])
                    yield
                    P.op(V, lambda e, Xcur=Xcur, Xnxt=Xnxt: e.tensor_tensor(out=Xnxt, in0=psum[:, bB, 256:384], in1=Xcur, op=ALU.add),
                         reads=[("ps", bB), kX], writes=[kXn])
                    yield
                    Pprev, PTprev, kP, kPT = pa, pb_, kpa, kpb
                    Xcur, Xnxt, kX, kXn = Xnxt, Xcur, kXn, kX
                X, kXf = Xcur, kX
                dbg = h == 0 and ((d == 0 and t == 0) or (d == 1 and t == 1))
                pf = "u%d_" % d
                if dbg and (pf + "X") in tap_out:
                    tap(pf + "X", X, kXf)
                    tap(pf + "PT", PTt, K("PT"))
                    tap(pf + "MT", MT, K("MT"))
                    tap(pf + "kg", kg, K("kg"))
                    tap(pf + "qgT", qgT, K("qgT"))
                    tap(pf + "kdec", kdec, K("kdec"))
                P.op(T_, lambda e: e.matmul(psum[:, bB, 384:512], lhsT=kg, rhs=X, start=True, stop=True), reads=[K("kg"), kXf], writes=[("ps", bB)])
                yield
                P.op(S_, lambda e: e.activation(out=nwT, in_=psum[:, bB, 384:512], func=AF.Copy, scale=-1.0), reads=[("ps", bB)], writes=[K("nwT")])
                yield
                yield "B"
                Sin, Sout = Sbuf[2 * d + pp], Sbuf[2 * d + (1 - pp)]
                kSin, kSout = ("S", d, pp), ("S", d, 1 - pp)
                vnew = EG
                P.op(T_, lambda e: e.matmul(psum[:, bA, 0:128], lhsT=X, rhs=vtok[:, t, :], start=True, stop=False),
                     reads=[kXf, ("vtok", (t // 4) * 4)], writes=[("ps", bA)])
                P.op(T_, lambda e: e.matmul(psum[:, bA, 0:128], lhsT=nwT, rhs=Sin, start=False, stop=True), reads=[K("nwT"), kSin], writes=[("ps", bA)])
                yield
                P.op(S_, lambda e: e.activation(out=vnew, in_=psum[:, bA, 0:128], func=AF.Copy, scale=col(beta)), reads=[("ps", bA), ("beta", d)],
                     writes=[K("EG")])
                yield
                need_o = ctx_out or t >= 2
                if dbg and (pf + "vnew") in tap_out:
                    tap(pf + "vnew", vnew, K("EG"))
                if need_o:
                    P.op(T_, lambda e: e.matmul(psum[:, bA, 128:256], lhsT=qgT, rhs=Sin, start=True, stop=False), reads=[K("qgT"), kSin],
                         writes=[("ps", bA)])
                    P.op(T_, lambda e: e.matmul(psum[:, bA, 128:256], lhsT=PTt, rhs=vnew, start=False, stop=True), reads=[K("PT"), K("EG")],
                         writes=[("ps", bA)])
                P.op(T_, lambda e: e.matmul(psum[:, bA, 256:384], lhsT=kdec, rhs=vnew, start=True, stop=True), reads=[K("kdec"), K("EG")],
                     writes=[("ps", bA)])
                yield
                if need_o:
                    if t not in oacc_written:
                        oacc_written.add(t)
                        P.op(V, lambda e: e.tensor_copy(out=oacc[:, t, :], in_=psum[:, bA, 128:256]), reads=[("ps", bA)], writes=[("oacc", t)])
                    else:
                        P.op(V, lambda e: e.tensor_tensor(out=oacc[:, t, :], in0=psum[:, bA, 128:256], in1=oacc[:, t, :], op=ALU.add),
                             reads=[("ps", bA), ("oacc", t)], writes=[("oacc", t)])
                P.op(V, lambda e: e.scalar_tensor_tensor(out=Sout, in0=Sin, scalar=col(egl), in1=psum[:, bA, 256:384], op0=ALU.mult, op1=ALU.add),
                     reads=[("ps", bA), kSin, "egl"], writes=[kSout])
                if h == 0 and d == 0 and t in (0, 1) and ("S%d" % t) in tap_out:
                    tap("S%d" % t, Sout, kSout)
                if h == 0 and d == 0 and t == 1 and "vnew1" in tap_out:
                    tap("vnew1", vnew, K("EG"))
                yield

            def mixed(b_gens, a_gens):
                act = [(g_, "B") for g_ in b_gens] + [(g_, "A") for g_ in a_gens]
                while act:
                    nxt = []
                    for g_, m_ in act:
                        try:
                            r_ = next(g_)
                        except StopIteration:
                            continue
                        if m_ == "A" and r_ == "B":
                            continue
                        nxt.append((g_, m_))
                    act = nxt

            dirs = [(0, tiles_f), (1, tiles_b)]
            if "dn_only_fwd" in taps:
                dirs = dirs[:1]
            if "dn_only_bwd" in taps:
                dirs = dirs[1:]
            def seq_pairs(pairs):
                for pair in pairs:
                    act_ = list(pair)
                    while act_:
                        nxt_ = []
                        for g_ in act_:
                            try:
                                next(g_)
                                nxt_.append(g_)
                            except StopIteration:
                                pass
                        act_ = nxt_
                        yield

            groups = []
            for gi in range(NT):
                groups.append([unit(0, 4 * ((gi // 2) % 2) + 2 * (gi % 2) + d, d, tl[gi], gi == 0, gi % 2) for d, tl in dirs])
            batches = [groups[i:i + 2] for i in range(0, NT, 2)]
            prev = []
            for bt in batches:
                mixed([seq_pairs(prev)] if prev else [], [g_ for grp in bt for g_ in grp])
                prev = bt
            mixed([seq_pairs(prev)], [])
            P.barrier()
            if h == 0 and "dn_oacc" in tap_out:
                tap("dn_oacc", oacc[:].rearrange("p t n -> p (t n)"), ("oacc", 0))
                P.barrier()
            t_start = 0 if ctx_out else 2
            for t in range(t_start, NT):
                b2 = t % 2
                junk = sq[:, 0:128]
                yb = rn[:, 64 * b2:64 * b2 + 64].bitcast(BF16)
                P.op(S_, lambda e, t=t, b2=b2: e.activation(out=junk, in_=oacc[:, t, :], func=AF.Square, accum_out=stat[:, 2 * b2:2 * b2 + 1]),
                     writes=["junk", ("stat", b2)])
                P.op(S_, lambda e, b2=b2: e.activation(out=stat[:, 2 * b2 + 1:2 * b2 + 2], in_=stat[:, 2 * b2:2 * b2 + 1], func=AF.Ln, scale=1.0 / 128,
                                                       bias=eps_c[:]), reads=[("stat", b2)], writes=[("stat2", b2)])
                P.op(S_, lambda e, b2=b2: e.activation(out=stat[:, 2 * b2 + 1:2 * b2 + 2], in_=stat[:, 2 * b2 + 1:2 * b2 + 2], func=AF.Exp, scale=-0.5),
                     reads=[("stat2", b2)], writes=[("stat2", b2)])
                P.op(V, lambda e, t=t, b2=b2, yb=yb: e.scalar_tensor_tensor(out=yb, in0=oacc[:, t, :], scalar=stat[:, 2 * b2 + 1:2 * b2 + 2],
                                                                            in1=zs[:, t, :], op0=ALU.mult, op1=ALU.mult),
                     reads=[("stat2", b2)], writes=[("yb", b2)])
                bk = next_bank()
                pt = psum[:, bk, 0:64].bitcast(BF16)
                P.op(T_, lambda e, yb=yb, pt=pt: e.transpose(pt, yb, ident_b[:]), reads=[("yb", b2)], writes=[("ps", bk)])
                copy_op(evac_eng(), ymix[:, 0, h, t * 128:(t + 1) * 128], pt, [("ps", bk)], [("ymix", 0, h, t)])
        P.barrier()


    def phase_hg(l, ctx_out):
        W0 = 10240
        wst_hg = A[:, 0:5120]
        wb = A[:, 5120:7680].bitcast(BF16)
        o = W0
        qT = A[:, o:o + NTOK]; o += NTOK
        vtok = A[:, o:o + 1152].bitcast(BF16).rearrange("p (t n) -> p t n", t=NT); o += 1152
        oT = A[:, o:o + NTOK]; o += NTOK
        lbb = A[:, o:o + 128]; o += 128
        omlb = A[:, o:o + 128]; o += 128
        gcol = A[:, o:o + 1]; o += 4
        lbT = A[:, o:o + 4]; o += 4
        lbl = A[0:4, o:o + 512]; o += 512
        Sb = [A[:, o + 128 * i:o + 128 * (i + 1)] for i in range(4)]; o += 512
        sq = A[:, o:o + 512]; o += 512
        rn = A[:, o:o + 512]; o += 512
        NS = 12
        slots0 = o
        o += 4 * NS * 128
        hTs = A[:, o:o + 8192].bitcast(BF16).rearrange("p (k n) -> p k n", k=KC)
        o += 8192
        assert o <= YMIX0, (o, YMIX0)
        for k in range(KC):
            P.op((V, G, S_)[k % 3] if False else (V, G)[k % 2], lambda e, k=k: e.tensor_copy(
                out=hTs[:, k, :].rearrange("p (c r) -> p c r", r=32),
                in_=hT[:, k, NCTX:NTOK].rearrange("p (r c) -> p c r", c=64)), writes=[("hTs", k)])

        def hT_s(k, st):
            if st < 2:
                return hT[:, k, st * 128:(st + 1) * 128]
            return hTs[:, k, (st - 2) * 128:(st - 1) * 128]

        def sl(us, i):
            if us >= 4:
                b = ((us - 4) * NS * 128 if us < 7 else 7680) + i * 128
                return A[:, b:b + 128]
            b = slots0 + (us * NS + i) * 128
            return A[:, b:b + 128]

        UbF = A[:, o - 128:o] if False else None
        P.op(SPQ, lambda e: e.dma_start(out=lbl, in_=hg_lb_logits[:, :]), writes=["lbl"], dma=True)
        bk = next_bank()
        for hc in range(4):
            P.op(T_, lambda e, hc=hc: e.transpose(psum[:, bk, 4 * hc:4 * hc + 4], lbl[:, hc * 128:(hc + 1) * 128], ident_f[0:4, 0:4]),
                 reads=["lbl", "ident_f"], writes=[("ps", bk)])
        lgt = sq[:, 0:16].rearrange("p (h l) -> p h l", l=4)
        P.op(S_, lambda e: e.activation(out=sq[:, 0:16], in_=psum[:, bk, 0:16], func=AF.Exp), reads=[("ps", bk)], writes=["lgt"])
        P.op(V, lambda e: e.tensor_reduce(out=sq[:, 16:20], in_=lgt, axis=AX.X, op=ALU.add), reads=["lgt"], writes=["lsum"])
        P.op(V, lambda e: e.reciprocal(out=sq[:, 16:20], in_=sq[:, 16:20]), reads=["lsum"], writes=["lsum"])
        if l == 0:
            P.op(V, lambda e: e.memset(lbT, 0.0), writes=["lbT"])
        else:
            P.op(V, lambda e: e.tensor_reduce(out=sq[:, 20:24], in_=lgt[:, :, 1:l + 1], axis=AX.X, op=ALU.add), reads=["lgt"], writes=["lpart"])
            P.op(V, lambda e: e.tensor_tensor(out=lbT, in0=sq[:, 20:24], in1=sq[:, 16:20], op=ALU.mult), reads=["lpart", "lsum"], writes=["lbT"])
        P.op(SPQ, lambda e: e.dma_start(out=gcol, in_=hg_norm_g[l:l + 1, :].rearrange("o p -> p o"), allow_slow_non_contiguous=True),
             writes=["gcol"], dma=True)
        UB = [sb_const("UBf"), sb_const("UBb"), sb_const("DBf"), sb_const("DBb")]
        tiles_f = list(range(NT))
        tiles_b = [1, 0] + list(range(NT - 1, 1, -1))

        for h in range(4):
            P.barrier()
            P.op(V, lambda e: e.tensor_scalar(out=sq[:, 0:128], in0=ones_f[:], scalar1=lbT[:, h:h + 1], scalar2=None, op0=ALU.mult),
                 reads=["lbT"], writes=["lbc"])
            bk = next_bank()
            P.op(T_, lambda e: e.transpose(psum[:, bk, 0:128], sq[:, 0:128], ident_f[:]), reads=["lbc"], writes=[("ps", bk)])
            P.op(V, lambda e: e.tensor_copy(out=lbb, in_=psum[:, bk, 0:128]), reads=[("ps", bk)], writes=["lbb"])
            P.op(V, lambda e: e.tensor_scalar(out=omlb, in0=lbb, scalar1=-1.0, scalar2=1.0, op0=ALU.mult, op1=ALU.add), reads=["lbb"], writes=["omlb"])
            cols = [OFF_HGQ + 128 * h, OFF_HGF + 128 * h, OFF_HGF + 512 + 128 * h, OFF_HGI + 128 * h, OFF_HGG + 128 * h]
            for j, c0 in enumerate(cols):
                P.op(SPQ, lambda e, j=j, c0=c0: e.dma_start(out=wst_hg[:, j * 1024:(j + 1) * 1024].rearrange("p (k n) -> p k n", k=KC),
                                                             in_=w_in[l, :, c0:c0 + 128].rearrange("(k p) n -> p k n", p=128)),
                     writes=[("wsth", j)], dma=True)
                P.op((G, V)[j % 2], lambda e, j=j: e.tensor_copy(out=wb[:, j * 1024:(j + 1) * 1024], in_=wst_hg[:, j * 1024:(j + 1) * 1024]),
                     reads=[("wsth", j)], writes=[("wbh", j)])
            wq = lambda k: wb[:, 0 * 1024 + k * 128:0 * 1024 + (k + 1) * 128]
            wf = lambda d, k: wb[:, (1 + d) * 1024 + k * 128:(1 + d) * 1024 + (k + 1) * 128]
            wi = lambda k: wb[:, 3 * 1024 + k * 128:3 * 1024 + (k + 1) * 128]
            wg = lambda k: wb[:, 4 * 1024 + k * 128:4 * 1024 + (k + 1) * 128]
            for st in range(NT):
                bk = next_bank()
                for k in range(KC):
                    P.op(T_, lambda e, st=st, k=k, bk=bk: e.matmul(psum[:, bk, 0:128], lhsT=hT_s(k, st), rhs=wi(k), start=(k == 0), stop=(k == KC - 1)),
                         reads=[("wbh", 3)], writes=[("ps", bk)])
                for k in range(KC):
                    P.op(T_, lambda e, st=st, k=k, bk=bk: e.matmul(psum[:, bk, 128:256], lhsT=wq(k), rhs=hT_s(k, st), start=(k == 0), stop=(k == KC - 1)),
                         reads=[("wbh", 0)], writes=[("ps", bk)])
                P.op(V, lambda e, st=st, bk=bk: e.tensor_copy(out=vtok[:, st, :], in_=psum[:, bk, 0:128]), reads=[("ps", bk)], writes=[("vtok", st)])
                P.op(S_, lambda e, st=st, bk=bk: e.activation(out=qT[:, st * 128:(st + 1) * 128], in_=psum[:, bk, 128:256], func=AF.Silu),
                     reads=[("ps", bk)], writes=[("qT", st)])
            for d in range(2):
                P.op(G, lambda e, d=d: e.memset(Sb[2 * d][:], 0.0), writes=[("S", d, 0)])
            oT_written = set()

            P.barrier()

            def unit(us, d, st, gi):
                bA = bB = us
                K = lambda n: ("u", us, n)
                F, KT, GC, EK, EQ, QG, EGq, ATT, EXD = [sl(us, i) for i in range(9)]
                KTt = sl(us, 9).bitcast(BF16)[:, 0:128]
                QTt = sl(us, 9).bitcast(BF16)[:, 128:256]
                KDEC = sl(us, 10).bitcast(BF16)[:, 0:128]
                ATTb = sl(us, 10).bitcast(BF16)[:, 128:256]
                NEGM = sl(us, 11)[:, 0:2]
                cs = slice(st * 128, (st + 1) * 128)
                for k in range(KC):
                    P.op(T_, lambda e, k=k: e.matmul(psum[:, bA, 0:128], lhsT=hT_s(k, st), rhs=wf(d, k), start=(k == 0), stop=(k == KC - 1)),
                         reads=[("wbh", 1 + d)], writes=[("ps", bA)])
                yield
                P.op(S_, lambda e: e.activation(out=F, in_=psum[:, bA, 0:128], func=AF.Sigmoid), reads=[("ps", bA)], writes=[K("F")])
                yield
                P.op(V, lambda e: e.tensor_tensor(out=F, in0=F, in1=omlb, op=ALU.mult), reads=[K("F"), "omlb"], writes=[K("F")])
                P.op(V, lambda e: e.tensor_tensor(out=F, in0=F, in1=lbb, op=ALU.add), reads=[K("F"), "lbb"], writes=[K("F")])
                yield
                P.op(G, lambda e: e.tensor_scalar(out=F, in0=F, scalar1=1e-30, scalar2=None, op0=ALU.max), reads=[K("F")], writes=[K("F")])
                P.op(G, lambda e: e.tensor_scalar(out=KT, in0=F, scalar1=-1.0, scalar2=1.0, op0=ALU.mult, op1=ALU.add), reads=[K("F")], writes=[K("KT")])
                yield
                P.op(S_, lambda e: e.activation(out=F, in_=F, func=AF.Ln), reads=[K("F")], writes=[K("F")])
                yield
                P.op(T_, lambda e: e.matmul(psum[:, bA, 128:256], lhsT=F, rhs=UB[d][:], start=True, stop=True), reads=[K("F")], writes=[("ps", bA)])
                P.op(T_, lambda e: e.matmul(psum[:, bA, 256:384], lhsT=UB[2 + d][:], rhs=F, start=True, stop=True), reads=[K("F")], writes=[("ps", bA)])
                P.op(T_, lambda e: e.transpose(psum[:, bA, 384:512], KT, ident_f[:]), reads=[K("KT")], writes=[("ps", bA)])
                yield
                P.op(S_, lambda e: e.activation(out=GC, in_=psum[:, bA, 128:256], func=AF.Copy), reads=[("ps", bA)], writes=[K("GC")])
                P.op(S_, lambda e: e.activation(out=EXD, in_=psum[:, bA, 256:384], func=AF.Exp), reads=[("ps", bA)], writes=[K("EXD")])
                yield
                mid = GC.rearrange("p (c n) -> p c n", n=64)[:, :, 32]
                P.op(G, lambda e: e.tensor_scalar(out=NEGM, in0=mid, scalar1=-1.0, scalar2=None, op0=ALU.mult), reads=[K("GC")], writes=[K("NEGM")])
                P.op(S_, lambda e: e.activation(out=EGq, in_=GC, func=AF.Exp), reads=[K("GC")], writes=[K("EGq")])
                yield
                for ch in range(2):
                    c64 = slice(ch * 64, (ch + 1) * 64)
                    P.op(S_, lambda e, ch=ch, c64=c64: e.activation(out=EK[:, c64], in_=GC[:, c64], func=AF.Exp, scale=-1.0, bias=GC[:, ch * 64 + 32:ch * 64 + 33]),
                         reads=[K("GC")], writes=[K("EK")])
                    P.op(S_, lambda e, ch=ch, c64=c64: e.activation(out=EQ[:, c64], in_=GC[:, c64], func=AF.Exp, bias=NEGM[:, ch:ch + 1]),
                         reads=[K("GC"), K("NEGM")], writes=[K("EQ")])
                yield
                P.op(V, lambda e: e.tensor_tensor(out=KTt, in0=psum[:, bA, 384:512], in1=EK, op=ALU.mult), reads=[("ps", bA), K("EK")], writes=[K("KTt")])
                P.op(G, lambda e: e.tensor_tensor(out=QTt, in0=qT[:, cs], in1=EQ, op=ALU.mult), reads=[K("EQ"), ("qT", st)], writes=[K("QTt")])
                P.op(G, lambda e: e.tensor_tensor(out=QG, in0=qT[:, cs], in1=EGq, op=ALU.mult), reads=[K("EGq"), ("qT", st)], writes=[K("QG")])
                P.op(V, lambda e: e.tensor_tensor(out=KDEC, in0=KT, in1=EXD, op=ALU.mult), reads=[K("KT"), K("EXD")], writes=[K("KDEC")])
                yield
                P.op(T_, lambda e: e.matmul(psum[:, bB, 0:128], lhsT=KTt, rhs=QTt, start=True, stop=True), reads=[K("KTt"), K("QTt")], writes=[("ps", bB)])
                yield
                P.op(V, lambda e: e.tensor_copy(out=ATT, in_=psum[:, bB, 0:128]), reads=[("ps", bB)], writes=[K("ATT")])
                yield
                for hf in range(2):
                    c64 = slice(64 * hf, 64 * hf + 64)
                    if d == 0:
                        P.op(G, lambda e, c64=c64, hf=hf: e.affine_select(out=ATTb[:, c64], in_=ATT[:, c64], pattern=[[1, 64]], compare_op=ALU.is_ge,
                                                                          fill=0.0, base=64 * hf, channel_multiplier=-1),
                             reads=[K("ATT")], writes=[K("ATTb%d" % hf)])
                    else:
                        P.op(G, lambda e, c64=c64, hf=hf: e.affine_select(out=ATTb[:, c64], in_=ATT[:, c64], pattern=[[-1, 64]], compare_op=ALU.is_ge,
                                                                          fill=0.0, base=-64 * hf, channel_multiplier=1),
                             reads=[K("ATT")], writes=[K("ATTb%d" % hf)])
                yield "B"
                for ci, ch in enumerate((0, 1) if d == 0 else (1, 0)):
                    pp = (2 * gi + ci) % 2
                    Sin, Sout = Sb[2 * d + pp], Sb[2 * d + 1 - pp]
                    kSin, kSout = ("S", d, pp), ("S", d, 1 - pp)
                    r64 = slice(ch * 64, (ch + 1) * 64)
                    need_o = ctx_out or st >= 2
                    pso = psum[:, bB, 128 + 64 * ch:128 + 64 * (ch + 1)]
                    if need_o:
                        P.op(T_, lambda e, r64=r64, Sin=Sin, pso=pso: e.matmul(pso, lhsT=Sin, rhs=QG[:, r64], start=True, stop=False),
                             reads=[kSin, K("QG")], writes=[("ps", bB)])
                        P.op(T_, lambda e, r64=r64, pso=pso: e.matmul(pso, lhsT=vtok[r64, st, :], rhs=ATTb[r64, r64], start=False, stop=True),
                             reads=[("vtok", st), K("ATTb0"), K("ATTb1")], writes=[("ps", bB)])
                    psS = psum[:, bB, 256 + 128 * ci:256 + 128 * (ci + 1)]
                    P.op(T_, lambda e, r64=r64, psS=psS: e.matmul(psS, lhsT=KDEC[r64, :], rhs=vtok[r64, st, :], start=True, stop=True),
                         reads=[K("KDEC"), ("vtok", st)], writes=[("ps", bB)])
                    yield
                    oc = slice(st * 128 + ch * 64, st * 128 + (ch + 1) * 64)
                    if need_o:
                        key = (st, ch)
                        if key not in oT_written:
                            oT_written.add(key)
                            P.op(V, lambda e, oc=oc, pso=pso: e.tensor_copy(out=oT[:, oc], in_=pso), reads=[("ps", bB)], writes=[("oT", st, ch)])
                        else:
                            P.op(V, lambda e, oc=oc, pso=pso: e.tensor_tensor(out=oT[:, oc], in0=pso, in1=oT[:, oc], op=ALU.add),
                                 reads=[("ps", bB), ("oT", st, ch)], writes=[("oT", st, ch)])
                    gl_col = ch * 64 + (63 if d == 0 else 0)
                    P.op(V, lambda e, Sin=Sin, Sout=Sout, psS=psS, gl_col=gl_col: e.scalar_tensor_tensor(
                        out=Sout, in0=Sin, scalar=EGq[:, gl_col:gl_col + 1], in1=psS, op0=ALU.mult, op1=ALU.add),
                        reads=[("ps", bB), kSin, K("EGq")], writes=[kSout])
                    yield

            def mixed(b_gens, a_gens):
                act = [(g_, "B") for g_ in b_gens] + [(g_, "A") for g_ in a_gens]
                while act:
                    nxt = []
                    for g_, m_ in act:
                        try:
                            r_ = next(g_)
                        except StopIteration:
                            continue
                        if m_ == "A" and r_ == "B":
                            continue
                        nxt.append((g_, m_))
                    act = nxt

            def seq_pairs(pairs):
                for pair in pairs:
                    act_ = list(pair)
                    while act_:
                        nxt_ = []
                        for g_ in act_:
                            try:
                                next(g_)
                                nxt_.append(g_)
                            except StopIteration:
                                pass
                        act_ = nxt_
                        yield

            dirs = [(0, tiles_f), (1, tiles_b)]
            groups = [[unit(4 * ((gi // 2) % 2) + 2 * (gi % 2) + d, d, tl[gi], gi) for d, tl in dirs] for gi in range(NT)]
            batches = [groups[i:i + 2] for i in range(0, NT, 2)]
            prev = []
            for bt in batches:
                mixed([seq_pairs(prev)] if prev else [], [g_ for grp in bt for g_ in grp])
                prev = bt
            mixed([seq_pairs(prev)], [])
            P.barrier()
            if h == 0 and "hg_oT" in tap_out:
                tap("hg_oT", oT[:, 0:NTOK], ("oT", 0, 0))
                P.barrier()
            blocks = TBLK if ctx_out else TBLK[1:]
            for (t0, tn) in blocks:
                bk = next_bank()
                P.op(S_, lambda e, t0=t0, tn=tn: e.activation(out=sq[:, 0:tn], in_=oT[:, t0:t0 + tn], func=AF.Square), writes=["sq"])
                P.op(T_, lambda e, bk=bk, tn=tn: e.matmul(psum[:, bk, 0:tn], lhsT=ones_f[:], rhs=sq[:, 0:tn], start=True, stop=True),
                     reads=["sq"], writes=[("ps", bk)])
                P.op(S_, lambda e, bk=bk, tn=tn: e.activation(out=rn[:, 0:tn], in_=psum[:, bk, 0:tn], func=AF.Ln, scale=1.0 / 128, bias=eps_c[:]),
                     reads=[("ps", bk)], writes=["rn"])
                P.op(S_, lambda e, tn=tn: e.activation(out=rn[:, 0:tn], in_=rn[:, 0:tn], func=AF.Exp, scale=-0.5), reads=["rn"], writes=["rn"])
                P.op(V, lambda e, t0=t0, tn=tn: e.scalar_tensor_tensor(out=oT[:, t0:t0 + tn], in0=oT[:, t0:t0 + tn], scalar=gcol[:, 0:1], in1=rn[:, 0:tn],
                                                                       op0=ALU.mult, op1=ALU.mult), reads=["rn", "gcol"], writes=[("oTn", t0)])
            for bi, (t0, tn) in enumerate(TBLK):
                if not ctx_out and bi == 0:
                    continue
                bk = next_bank()
                for k in range(KC):
                    P.op(T_, lambda e, k=k, bk=bk, t0=t0, tn=tn: e.matmul(psum[:, bk, 0:tn], lhsT=wg(k), rhs=hT[:, k, t0:t0 + tn],
                                                                         start=(k == 0), stop=(k == KC - 1)), reads=[("wbh", 4)], writes=[("ps", bk)])
                P.op(S_, lambda e, bk=bk, tn=tn: e.activation(out=sq[:, 0:tn], in_=psum[:, bk, 0:tn], func=AF.Silu), reads=[("ps", bk)], writes=["sq"])
                if bi == 0:
                    src = oT[:, 0:NCTX]
                    dst = ymix[:, 1, h, 0:NCTX]
                    s2 = sq[:, 0:tn]
                else:
                    b4 = bi - 1
                    src = oT[:, NCTX:NTOK].rearrange("p (c r) -> p r c", r=32)[:, 8 * b4:8 * b4 + 8, :]
                    dst = ymix[:, 1, h, t0:t0 + tn].rearrange("p (r c) -> p r c", c=64)
                    s2 = sq[:, 0:tn].rearrange("p (r c) -> p r c", c=64)
                P.op(V, lambda e, src=src, dst=dst, s2=s2: e.tensor_tensor(out=dst, in0=src, in1=s2, op=ALU.mult),
                     reads=["sq"] + [("oTn", x[0]) for x in TBLK], writes=[("ymix", 1, h, bi)])
        P.barrier()


    def phase_merge(l, ctx_out):
        mergedT = A[:, 10240:19456].bitcast(BF16).rearrange("p (k n) -> p k n", k=KC)
        wbr = A[:, 19456:23552].bitcast(BF16).rearrange("p (b h n) -> p b h n", b=2, h=4)
        wout_b = A[:, 23552:27648].bitcast(BF16).rearrange("p (k n) -> p k n", k=KC)
        wg_b = A[:, 27648:28672].bitcast(BF16)
        sg = [A[:, 28672:29184], A[:, 29184:29696]]
        gateb = A[:, 29696:31744].rearrange("p (v n) -> p v n", v=2)
        xt = [A[:, 31744:32768], A[:, 8192:9216]]
        tmp = A[:, 9216:10240]
        assert 32768 <= YMIX0
        for b, src in enumerate((w_br_dn, w_br_hg)):
            P.op(SPQ, lambda e, b=b, src=src: e.dma_start(out=wst[:, b, :].rearrange("p (h n) -> p h n", h=4),
                                                         in_=src[l].rearrange("(h p) n -> p h n", p=128)), writes=[("wst", b)], dma=True)
            P.op((G, V)[b], lambda e, b=b: e.tensor_copy(out=wbr[:, b, :, :], in_=wst[:, b, :].rearrange("p (h n) -> p h n", h=4)),
                 reads=[("wst", b)], writes=[("wbr", b)])
        for v in range(2):
            P.op(SPQ, lambda e, v=v: e.dma_start(out=gateb[:, v, :], in_=modrow_d[v:v + 1, 2 * D:3 * D].partition_broadcast(128)),
                 writes=[("gateb", v)], dma=True)
        blocks = list(enumerate(TBLK)) if ctx_out else list(enumerate(TBLK))[1:]
        for dc in range(KC):
            slot = dc % 2
            for gi_, c0 in enumerate((OFF_GDN + dc * 128, OFF_GHG + dc * 128)):
                P.op(SPQ, lambda e, gi_=gi_, c0=c0, slot=slot: e.dma_start(
                    out=wst[:, slot, gi_ * 1024:(gi_ + 1) * 1024].rearrange("p (k n) -> p k n", k=KC),
                    in_=w_in[l, :, c0:c0 + 128].rearrange("(k p) n -> p k n", p=128)), writes=[("wst", slot)], dma=True)
            P.op(G, lambda e, slot=slot: e.tensor_copy(out=wg_b, in_=wst[:, slot, 0:2048]), reads=[("wst", slot)], writes=["wg_b"])
            for bi, (t0, tn) in blocks:
                bks = [next_bank() for _ in range(4)]
                for br in range(2):
                    for hc in range(4):
                        P.op(T_, lambda e, br=br, hc=hc, t0=t0, tn=tn, bks=bks: e.matmul(
                            psum[:, bks[br], 0:tn], lhsT=wbr[:, br, hc, dc * 128:(dc + 1) * 128], rhs=ymix[:, br, hc, t0:t0 + tn],
                            start=(hc == 0), stop=(hc == 3)), reads=[("wbr", br)], writes=[("ps", bks[br])])
                    for k in range(KC):
                        P.op(T_, lambda e, br=br, k=k, t0=t0, tn=tn, bks=bks: e.matmul(
                            psum[:, bks[2 + br], 0:tn], lhsT=wg_b[:, br * 1024 + k * 128:br * 1024 + (k + 1) * 128], rhs=hT[:, k, t0:t0 + tn],
                            start=(k == 0), stop=(k == KC - 1)), reads=["wg_b"], writes=[("ps", bks[2 + br])])
                for br in range(2):
                    P.op(S_, lambda e, br=br, tn=tn, bks=bks: e.activation(out=sg[br][:, 0:tn], in_=psum[:, bks[2 + br], 0:tn], func=AF.Sigmoid),
                         reads=[("ps", bks[2 + br])], writes=[("sg", br)])
                    P.op(V, lambda e, br=br, tn=tn, bks=bks: e.tensor_tensor(out=sg[br][:, 0:tn], in0=psum[:, bks[br], 0:tn], in1=sg[br][:, 0:tn], op=ALU.mult),
                         reads=[("ps", bks[br]), ("sg", br)], writes=[("sg", br)])
                P.op(G, lambda e, t0=t0, tn=tn: e.tensor_tensor(out=mergedT[:, dc, t0:t0 + tn], in0=sg[0][:, 0:tn], in1=sg[1][:, 0:tn], op=ALU.add),
                     reads=[("sg", 0), ("sg", 1)], writes=[("mergedT", dc, bi)])
        for half in range(2):
            P.op(SPQ, lambda e, half=half: e.dma_start(out=wst[:, half, :].rearrange("p (k n) -> p k n", k=4),
                                                       in_=w_out[l, half * 512:(half + 1) * 512, :].rearrange("(k p) n -> p k n", p=128)),
                 writes=[("wst", half)], dma=True)
            P.op((G, V)[half], lambda e, half=half: e.tensor_copy(out=wout_b[:, 4 * half:4 * half + 4, :],
                                                                 in_=wst[:, half, :].rearrange("p (k n) -> p k n", k=4)),
                 reads=[("wst", half)], writes=[("wout_b", half)])
        tiles = list(range(NT)) if ctx_out else list(range(2, NT))
        def ld_x(i_):
            P.op(SPQ, lambda e, i_=i_: e.dma_start(out=xt[i_ % 2], in_=x_tile_src(l, True, tiles[i_])), writes=[("xt", i_ % 2)], dma=True)

        ld_x(0)
        for i, t in enumerate(tiles):
            b = i % 2
            v = 1 if t < 2 else 0
            if i + 1 < len(tiles):
                ld_x(i + 1)
            for half in range(2):
                bk = next_bank()
                for dc in range(KC):
                    P.op(T_, lambda e, dc=dc, t=t, half=half, bk=bk: e.matmul(
                        psum[:, bk, :], lhsT=mergedT[:, dc, t * 128:(t + 1) * 128], rhs=wout_b[:, dc, half * 512:(half + 1) * 512],
                        start=(dc == 0), stop=(dc == KC - 1)),
                        reads=[("wout_b", 0), ("wout_b", 1)] + [("mergedT", dc, bi) for bi in range(5)], writes=[("ps", bk)])
                hs = slice(half * 512, (half + 1) * 512)
                P.op(V, lambda e, bk=bk, v=v, hs=hs: e.tensor_tensor(out=tmp[:, hs], in0=psum[:, bk, :], in1=gateb[:, v, hs], op=ALU.mult),
                     reads=[("ps", bk), ("gateb", v)], writes=[("tmp", half)])
                P.op(G, lambda e, b=b, hs=hs: e.tensor_tensor(out=xt[b][:, hs], in0=xt[b][:, hs], in1=tmp[:, hs], op=ALU.add),
                     reads=[("tmp", half), ("xt", b)], writes=[("xt", b)])
            P.op(SPQ, lambda e, b=b, t=t: e.dma_start(out=Xd[t * 128:(t + 1) * 128, :], in_=xt[b]), reads=[("xt", b)], writes=[("Xd", t)], dma=True)
        if not ctx_out:
            pass
        P.barrier()
        for nm_ in ("x1", "xmid%d" % l):
            if nm_ in tap_out:
                P.op(SPQ, lambda e, nm_=nm_: e.dma_start(out=tap_out[nm_], in_=Xd[:, :]), writes=["tap_" + nm_], dma=True)
                P.barrier()

    def phase_moe(l, last):
        o = 8192
        wgb = A[:, o:o + 2048].bitcast(BF16).rearrange("p (k n) -> p k n", k=KC); o += 2048
        wub = A[:, o:o + 2048].bitcast(BF16).rearrange("p (k n) -> p k n", k=KC); o += 2048
        wdb = A[:, o:o + 2048].bitcast(BF16).rearrange("p (k n) -> p k n", k=4); o += 2048
        acc = A[:, o:o + 18432].rearrange("p (t n) -> p t n", t=NT); o += 18432
        actT = A[:, o:o + 4608].bitcast(BF16).rearrange("p (f n) -> p f n", f=4); o += 4608
        gw = A[:, o:o + 576].rearrange("p (t n) -> p t n", t=NT); o += 576
        gsel = A[:, o:o + 72].rearrange("p (t n) -> p t n", t=NT); o += 72
        pen = A[:, o:o + 72].rearrange("p (t n) -> p t n", t=NT); o += 72
        oh1 = A[:, o:o + 576].rearrange("p (t n) -> p t n", t=NT); o += 576
        oh2 = A[:, o:o + 576].rearrange("p (t n) -> p t n", t=NT); o += 576
        sm = A[:, o:o + 18 * 8].rearrange("p (j t) -> p j t", j=8); o += 144
        brow = A[:, o:o + 36]; o += 36
        wr_f = A[:, o:o + 288]; o += 288
        wr_b = A[:, o:o + 144].bitcast(BF16); o += 144
        sgs = [A[:, o:o + 512], A[:, o + 512:o + 1024]]; o += 1024
        assert o <= ARENA, (o, ARENA)
        tiles = list(range(NT)) if not last else list(range(2, NT))
        blocks = TBLK if not last else TBLK[1:]
        T0 = tiles[0]
        NTl = len(tiles)
        ts = slice(T0, NT)
        gmax, gsum, m1, m2, p2, g1, g2 = [sm[:, j, ts] for j in range(7)]
        bc = lambda a, n: a.unsqueeze(2).to_broadcast([128, NTl, n])
        ops = []
        Vop = lambda f, **kw: P.op(V, f, reads=["moe_chain"], writes=["moe_chain"])
        Sop = lambda f, **kw: P.op(S_, f, reads=["moe_chain"], writes=["moe_chain"])
        lG, lE = lgG[:, ts, :], lgE[:, ts, :]
        Vop(lambda e: e.tensor_reduce(out=gmax, in_=lG, axis=AX.X, op=ALU.max))
        Vop(lambda e: e.tensor_tensor(out=gsel[:, ts, :], in0=lG, in1=bc(gmax, 4), op=ALU.is_equal))
        Vop(lambda e: e.tensor_tensor(out=pen[:, ts, :], in0=lG, in1=bc(gmax, 4), op=ALU.subtract))
        Sop(lambda e: e.activation(out=pen[:, ts, :], in_=pen[:, ts, :], func=AF.Exp))
        Vop(lambda e: e.tensor_reduce(out=gsum, in_=pen[:, ts, :], axis=AX.X, op=ALU.add))
        Vop(lambda e: e.reciprocal(out=gsum, in_=gsum))
        Vop(lambda e: e.tensor_scalar(out=pen[:, ts, :], in0=gsel[:, ts, :], scalar1=-1.0, scalar2=1e30, op0=ALU.add, op1=ALU.mult))
        lE4 = lgE[:, ts, :].rearrange("p t (g j) -> p t g j", g=4)
        Vop(lambda e: e.tensor_tensor(out=lE4, in0=lE4, in1=gsel[:, ts, :].unsqueeze(3).to_broadcast([128, NTl, 4, 8]), op=ALU.mult))
        Vop(lambda e: e.tensor_tensor(out=lE4, in0=lE4, in1=pen[:, ts, :].unsqueeze(3).to_broadcast([128, NTl, 4, 8]), op=ALU.add))
        Vop(lambda e: e.tensor_reduce(out=m1, in_=lE, axis=AX.X, op=ALU.max))
        Vop(lambda e: e.tensor_tensor(out=oh1[:, ts, :], in0=lE, in1=bc(m1, 32), op=ALU.is_equal))
        Vop(lambda e: e.scalar_tensor_tensor(out=lE, in0=oh1[:, ts, :], scalar=-1e30, in1=lE, op0=ALU.mult, op1=ALU.add))
        Vop(lambda e: e.tensor_reduce(out=m2, in_=lE, axis=AX.X, op=ALU.max))
        Vop(lambda e: e.tensor_tensor(out=oh2[:, ts, :], in0=lE, in1=bc(m2, 32), op=ALU.is_equal))
        Vop(lambda e: e.tensor_tensor(out=p2, in0=m2, in1=m1, op=ALU.subtract))
        Sop(lambda e: e.activation(out=p2, in_=p2, func=AF.Exp))
        Vop(lambda e: e.tensor_scalar(out=g1, in0=p2, scalar1=1.0, scalar2=None, op0=ALU.add))
        Vop(lambda e: e.reciprocal(out=g1, in_=g1))
        Vop(lambda e: e.tensor_tensor(out=g1, in0=g1, in1=gsum, op=ALU.mult))
        Vop(lambda e: e.tensor_tensor(out=g2, in0=g1, in1=p2, op=ALU.mult))
        Vop(lambda e: e.tensor_tensor(out=gw[:, ts, :], in0=oh1[:, ts, :], in1=bc(g1, 32), op=ALU.mult))
        Vop(lambda e: e.tensor_tensor(out=oh2[:, ts, :], in0=oh2[:, ts, :], in1=bc(g2, 32), op=ALU.mult))
        Vop(lambda e: e.tensor_tensor(out=gw[:, ts, :], in0=gw[:, ts, :], in1=oh2[:, ts, :], op=ALU.add))
        P.barrier()
        if "gw" in tap_out:
            tap("gw", gw[:].rearrange("p t n -> p (t n)"), "moe_chain")
            P.barrier()
        ne_run = min(NE, ne_decl) if "moe_ne" not in taps else taps["moe_ne_n"]
        for ex in range(ne_run):
            srcs = (w_eg[l, ex].rearrange("(k p) n -> p k n", p=128), w_eu[l, ex].rearrange("(k p) n -> p k n", p=128),
                    w_ed[l, ex].rearrange("(k p) n -> p k n", p=128))
            dsts = (wgb, wub, wdb)
            for j in range(3):
                slot = (3 * ex + j) % 2
                kk = KC if j < 2 else 4
                P.op(SPQ, lambda e, j=j, slot=slot, kk=kk: e.dma_start(out=wst[:, slot, :].rearrange("p (k n) -> p k n", k=kk), in_=srcs[j]),
                     writes=[("wst", slot)], dma=True)
                copy_op((G, S_, G)[j], dsts[j][:], wst[:, slot, :].rearrange("p (k n) -> p k n", k=kk), [("wst", slot)], [("wexp", j)])
            for bi, (t0, tn) in enumerate(blocks):
                for fc in range(4):
                    bg, bu = next_bank(), next_bank()
                    for k in range(KC):
                        P.op(T_, lambda e, k=k, fc=fc, t0=t0, tn=tn, bg=bg: e.matmul(psum[:, bg, 0:tn], lhsT=wgb[:, k, fc * 128:(fc + 1) * 128],
                                                                                   rhs=hT[:, k, t0:t0 + tn], start=(k == 0), stop=(k == KC - 1)),
                             reads=[("wexp", 0)], writes=[("ps", bg)])
                    for k in range(KC):
                        P.op(T_, lambda e, k=k, fc=fc, t0=t0, tn=tn, bu=bu: e.matmul(psum[:, bu, 0:tn], lhsT=wub[:, k, fc * 128:(fc + 1) * 128],
                                                                                   rhs=hT[:, k, t0:t0 + tn], start=(k == 0), stop=(k == KC - 1)),
                             reads=[("wexp", 1)], writes=[("ps", bu)])
                    sb_ = (bi * 4 + fc) % 2
                    P.op(S_, lambda e, tn=tn, bg=bg, sb_=sb_: e.activation(out=sgs[sb_][:, 0:tn], in_=psum[:, bg, 0:tn], func=AF.Silu),
                         reads=[("ps", bg)], writes=[("sgs", sb_)])
                    P.op(V, lambda e, fc=fc, t0=t0, tn=tn, bu=bu, sb_=sb_: e.tensor_tensor(out=actT[:, fc, t0:t0 + tn], in0=psum[:, bu, 0:tn],
                                                                                          in1=sgs[sb_][:, 0:tn], op=ALU.mult),
                         reads=[("ps", bu), ("sgs", sb_)], writes=[("actT", fc, bi)])
            for t in tiles:
                bi = 0 if t < 2 else (t - 2) // 4 + 1
                if last:
                    bi = (t - 2) // 4
                for half in range(2):
                    bk = next_bank()
                    for fc in range(4):
                        P.op(T_, lambda e, fc=fc, t=t, half=half, bk=bk: e.matmul(psum[:, bk, :], lhsT=actT[:, fc, t * 128:(t + 1) * 128],
                                                                                 rhs=wdb[:, fc, half * 512:(half + 1) * 512], start=(fc == 0), stop=(fc == 3)),
                             reads=[("wexp", 2)] + [("actT", fc, bi)], writes=[("ps", bk)])
                    hs = slice(half * 512, (half + 1) * 512)
                    if ex == 0:
                        P.op(V, lambda e, t=t, hs=hs, bk=bk: e.tensor_scalar(out=acc[:, t, hs], in0=psum[:, bk, :], scalar1=gw[:, t, ex:ex + 1], scalar2=None,
                                                                            op0=ALU.mult), reads=[("ps", bk)], writes=[("acc", t, half)])
                    else:
                        P.op(V, lambda e, t=t, hs=hs, bk=bk, ex=ex: e.scalar_tensor_tensor(out=acc[:, t, hs], in0=psum[:, bk, :], scalar=gw[:, t, ex:ex + 1],
                                                                                          in1=acc[:, t, hs], op0=ALU.mult, op1=ALU.add),
                             reads=[("ps", bk), ("acc", t, half)], writes=[("acc", t, half)])
        P.barrier()
        gate5 = A[:, 0:2048].rearrange("p (v n) -> p v n", v=2)
        xt = [A[:, 2048:3072], A[:, 3072:4096]]
        gfin = A[:, 4096:5120]
        junk = A[:, 5120:5632].bitcast(BF16)
        st = A[:, 5632:5696]
        for v in range(2):
            P.op(SPQ, lambda e, v=v: e.dma_start(out=gate5[:, v, :], in_=modrow_d[v:v + 1, 5 * D:6 * D].partition_broadcast(128)),
                 writes=[("gate5", v)], dma=True)
        if last:
            P.op(SPQ, lambda e: e.dma_start(out=gfin, in_=g_final[0:1, :].partition_broadcast(128)), writes=["gfin"], dma=True)
        def ld_x2(i_):
            t_ = tiles[i_]
            P.op(SPQ, lambda e, i_=i_, t_=t_: e.dma_start(out=xt[i_ % 2], in_=Xd[t_ * 128:(t_ + 1) * 128, :]), writes=[("xt", i_ % 2)], dma=True)

        ld_x2(0)
        for i, t in enumerate(tiles):
            b = i % 2
            v = 1 if t < 2 else 0
            if i + 1 < len(tiles):
                ld_x2(i + 1)
            P.op(G, lambda e, t=t, v=v: e.tensor_tensor(out=acc[:, t, :], in0=acc[:, t, :], in1=gate5[:, v, :], op=ALU.mult),
                 reads=[("gate5", v)], writes=[("accg", t)])
            P.op(V, lambda e, b=b, t=t: e.tensor_tensor(out=xt[b], in0=xt[b], in1=acc[:, t, :], op=ALU.add), reads=[("xt", b), ("accg", t)],
                 writes=[("xt", b)])
            if not last:
                P.op(SPQ, lambda e, b=b, t=t: e.dma_start(out=Xd[t * 128:(t + 1) * 128, :], in_=xt[b]), reads=[("xt", b)], writes=[("Xd", t)], dma=True)
            else:
                P.op(S_, lambda e, b=b: e.activation(out=junk, in_=xt[b], func=AF.Square, accum_out=st[:, 2 * b:2 * b + 1]), reads=[("xt", b)],
                     writes=["junk", ("st", b)])
                P.op(S_, lambda e, b=b: e.activation(out=st[:, 2 * b + 1:2 * b + 2], in_=st[:, 2 * b:2 * b + 1], func=AF.Ln, scale=1.0 / D, bias=eps_c[:]),
                     reads=[("st", b)], writes=[("st2", b)])
                P.op(S_, lambda e, b=b: e.activation(out=st[:, 2 * b + 1:2 * b + 2], in_=st[:, 2 * b + 1:2 * b + 2], func=AF.Exp, scale=-0.5),
                     reads=[("st2", b)], writes=[("st2", b)])
                P.op(V, lambda e, b=b: e.scalar_tensor_tensor(out=xt[b], in0=xt[b], scalar=st[:, 2 * b + 1:2 * b + 2], in1=gfin, op0=ALU.mult, op1=ALU.mult),
                     reads=[("xt", b), ("st2", b), "gfin"], writes=[("xt", b)])
                P.op(SPQ, lambda e, b=b, t=t: e.dma_start(out=out[(t - 2) * 128:(t - 1) * 128, :], in_=xt[b]), reads=[("xt", b)], writes=[("out", t)],
                     dma=True)
        P.barrier()
        if ("xend%d" % l) in tap_out:
            P.op(SPQ, lambda e: e.dma_start(out=tap_out["xend%d" % l], in_=Xd[:, :]), writes=["tap_xend"], dma=True)
            P.barrier()

    for l in range(depth):
        phase_mod(l)
        P.barrier()
        if stop_after == "mod":
            break
        last = (l == depth - 1)
        phase_norm(l, 0, list(range(NT)))
        P.barrier()
        if stop_after == "norm0":
            break
        if "skip_dn" not in taps:
            phase_dn(l, not last)
        if stop_after == "dn":
            break
        phase_hg(l, not last)
        if stop_after == "hg":
            break
        phase_merge(l, not last)
        if stop_after == "merge":
            break
        phase_norm(l, 1, list(range(NT)) if not last else list(range(2, NT)))
        P.barrier()
        phase_moe(l, last)

    if "hT" in tap_out:
        for k in range(KC):
            stg = work[:, 0:NTOK]
            P.op(V, lambda e, k=k: e.tensor_copy(out=stg, in_=hT[:, k, :]), reads=[("hT", k, t) for t in range(NT)], writes=["stg"])
            P.op(SPQ, lambda e, k=k: e.dma_start(out=tap_out["hT"][k * 128:(k + 1) * 128, :], in_=stg), reads=["stg"],
                 writes=[("tap_hT", k)], dma=True)

    P.prepare(es)
    with es, nc.Block() as block:
        P.emit(block)
    return nc


_NC_CACHE = {}


def kernel(**inputs):
    f32 = lambda a: np.ascontiguousarray(np.asarray(a, dtype=np.float32))
    shared = {k: f32(v) for k, v in inputs.items() if k not in ("x", "c", "ctx", "c_ctx", "g_final", "dn_a_log", "dn_dt_bias")}
    shared["c_ctx"] = f32(inputs["c_ctx"]).reshape(1, D)
    shared["g_final"] = f32(inputs["g_final"]).reshape(1, D)
    shared["dn_a_log"] = f32(inputs["dn_a_log"]).reshape(DEPTH, 8)
    shared["dn_dt_bias"] = f32(inputs["dn_dt_bias"]).reshape(DEPTH, 8)
    x = f32(inputs["x"])
    c = f32(inputs["c"])
    ctx = f32(inputs["ctx"])
    nb = x.shape[0]
    if "nc" not in _NC_CACHE:
        _NC_CACHE["nc"] = build_program()
    nc = _NC_CACHE["nc"]
    in_maps = []
    for b in range(nb):
        m = dict(shared)
        m["x"] = np.ascontiguousarray(x[b])
        m["ctx"] = np.ascontiguousarray(ctx[b])
        m["c"] = np.ascontiguousarray(c[b:b + 1])
        in_maps.append(m)
    res = run_bass_kernel_spmd(nc, in_maps, core_ids=list(range(nb)))
    return np.stack([np.asarray(r["out"], dtype=np.float32) for r in res.results], axis=0)
```
